# Optimizing a Trainium2 kernel written in Bass

```python
import math
import jax, jax.numpy as jnp
from jax import lax
import numpy as np

D_MODEL = 1024
BATCH = 4
SEQ = 4096
DEPTH = 2

BRANCH_WIDTH = D_MODEL // 2
NORM_EPS = 1e-6

RET_HEADS = 4
RET_DK = BRANCH_WIDTH // (2 * RET_HEADS)
RET_DV = BRANCH_WIDTH // RET_HEADS
RET_CHUNK = 128
ROPE_BASE = 10000.0
MAX_POS_OFFSET = 1024

GLA_HEADS = 4
GLA_DK = BRANCH_WIDTH // (2 * GLA_HEADS)
GLA_DV = BRANCH_WIDTH // GLA_HEADS
GLA_GATE_RANK = 16
GLA_GATE_NORMALIZER = 16.0
GLA_CHUNK = 64

SSD_HEAD_DIM = 64
SSD_HEADS = BRANCH_WIDTH // SSD_HEAD_DIM
SSD_GROUPS = 2
SSD_STATE = 128
SSD_CONV = 4
SSD_CHUNK = 128
SSD_CONV_CH = BRANCH_WIDTH + 2 * SSD_GROUPS * SSD_STATE

RWKV_HEAD_DIM = 64
RWKV_HEADS = BRANCH_WIDTH // RWKV_HEAD_DIM
RWKV_DECAY_RANK = 64
RWKV_ICL_RANK = 64
RWKV_SHIFT_WIDTH = 3 * BRANCH_WIDTH + RWKV_DECAY_RANK + RWKV_ICL_RANK
RWKV_LN_EPS = 64e-5

MEM_TOKENS = 256
MEM_HEADS = 4
MEM_HEAD_DIM = 64
MEM_WIDTH = MEM_HEADS * MEM_HEAD_DIM

N_BRANCHES = 5
IN_SIZES = (
    RET_HEADS * RET_DK, RET_HEADS * RET_DK, BRANCH_WIDTH, BRANCH_WIDTH,
    GLA_HEADS * GLA_DK, GLA_HEADS * GLA_DK, BRANCH_WIDTH, GLA_GATE_RANK, BRANCH_WIDTH,
    SSD_CONV_CH, SSD_HEADS, BRANCH_WIDTH,
    RWKV_SHIFT_WIDTH, BRANCH_WIDTH,
    MEM_WIDTH,
    N_BRANCHES * D_MODEL,
)
IN_TOTAL = sum(IN_SIZES)

kernel_name = 'hybrid_ret_gla_ssd_rwkv7_mem'


def split_cols(p, sizes):
    out, start = [], 0
    for s in sizes:
        out.append(p[..., start:start + s])
        start += s
    return out


def to_heads(t, n_heads):
    b, s, _ = t.shape
    return t.reshape(b, s, n_heads, -1).transpose(0, 2, 1, 3)


def from_heads(t):
    b, h, s, d = t.shape
    return t.transpose(0, 2, 1, 3).reshape(b, s, h * d)


def rms_norm(x, g):
    xf = x.astype(jnp.float32)
    y = xf * lax.rsqrt(jnp.mean(xf * xf, axis=-1, keepdims=True) + NORM_EPS)
    return (y * g.astype(jnp.float32)).astype(x.dtype)


def rms_unit(xf):
    return xf * lax.rsqrt(jnp.mean(xf * xf, axis=-1, keepdims=True) + NORM_EPS)


def rotate_interleaved(x, positions):
    dk = x.shape[-1]
    inv = 1.0 / (ROPE_BASE ** jnp.linspace(0.0, 1.0, dk // 2, dtype=jnp.float32))
    ang = positions.astype(jnp.float32)[:, None, :, None] * inv
    sin, cos = jnp.sin(ang), jnp.cos(ang)
    xp = x.reshape(*x.shape[:-1], dk // 2, 2)
    x1, x2 = xp[..., 0], xp[..., 1]
    return jnp.stack([x1 * cos - x2 * sin, x2 * cos + x1 * sin], axis=-1).reshape(x.shape)


def causal_depthwise_conv(x, w, bias):
    k, c = w.shape
    y = lax.conv_general_dilated(x, w[:, None, :], window_strides=(1,), padding=[(k - 1, 0)],
                                 dimension_numbers=('NWC', 'WIO', 'NWC'), feature_group_count=c)
    return y + bias


def chunked_scalar_decay(q, k, v, log_a, chunk):
    f32 = jnp.float32
    b, h, t, dk = q.shape
    dv = v.shape[-1]
    n = t // chunk
    qc = q.astype(f32).reshape(b, h, n, chunk, dk)
    kc = k.astype(f32).reshape(b, h, n, chunk, dk)
    vc = v.astype(f32).reshape(b, h, n, chunk, dv)
    cum = jnp.cumsum(log_a.astype(f32).reshape(b, h, n, chunk), axis=-1)
    causal = jnp.tril(jnp.ones((chunk, chunk), dtype=bool))
    seg = jnp.exp(jnp.where(causal, cum[..., :, None] - cum[..., None, :], -jnp.inf))
    scores = jnp.einsum('bhntd,bhnsd->bhnts', qc, kc) * seg
    y_intra = jnp.einsum('bhnts,bhnsv->bhntv', scores, vc)
    last = cum[..., -1:]
    chunk_state = jnp.einsum('bhnsd,bhnsv->bhndv', kc * jnp.exp(last - cum)[..., None], vc)
    chunk_decay = jnp.exp(last[..., 0])

    def step(state, inp):
        dec, cs = inp
        return state * dec[..., None, None] + cs, state

    init = jnp.zeros((b, h, dk, dv), f32)
    _, prev = lax.scan(step, init, (jnp.moveaxis(chunk_decay, 2, 0), jnp.moveaxis(chunk_state, 2, 0)))
    prev = jnp.moveaxis(prev, 0, 2)
    y_inter = jnp.einsum('bhntd,bhndv->bhntv', qc * jnp.exp(cum)[..., None], prev)
    return (y_intra + y_inter).reshape(b, h, t, dv)


def chunked_vector_decay(q, k, v, log_g, chunk):
    f32 = jnp.float32
    b, h, t, dk = q.shape
    dv = v.shape[-1]
    n = t // chunk
    qc = q.astype(f32).reshape(b, h, n, chunk, dk)
    kc = k.astype(f32).reshape(b, h, n, chunk, dk)
    vc = v.astype(f32).reshape(b, h, n, chunk, dv)
    cum = jnp.cumsum(log_g.astype(f32).reshape(b, h, n, chunk, dk), axis=3)
    ref = cum[:, :, :, chunk // 2:chunk // 2 + 1, :]
    q_in = qc * jnp.exp(cum - ref)
    k_in = kc * jnp.exp(ref - cum)
    causal = jnp.tril(jnp.ones((chunk, chunk), dtype=bool))
    scores = jnp.where(causal, jnp.einsum('bhntd,bhnsd->bhnts', q_in, k_in), 0.0)
    y_intra = jnp.einsum('bhnts,bhnsv->bhntv', scores, vc)
    last = cum[:, :, :, -1:, :]
    chunk_state = jnp.einsum('bhnsd,bhnsv->bhndv', kc * jnp.exp(last - cum), vc)
    chunk_decay = jnp.exp(last[:, :, :, 0, :])

    def step(state, inp):
        dec, cs = inp
        return state * dec[..., None] + cs, state

    init = jnp.zeros((b, h, dk, dv), f32)
    _, prev = lax.scan(step, init, (jnp.moveaxis(chunk_decay, 2, 0), jnp.moveaxis(chunk_state, 2, 0)))
    prev = jnp.moveaxis(prev, 0, 2)
    y_inter = jnp.einsum('bhntd,bhndv->bhntv', qc * jnp.exp(cum), prev)
    return (y_intra + y_inter).reshape(b, h, t, dv)


def retention_branch(q, k, v, positions):
    f32 = jnp.float32
    b, t, _ = q.shape
    qh = rotate_interleaved(to_heads(q.astype(f32), RET_HEADS), positions)
    kh = rotate_interleaved(to_heads(k.astype(f32), RET_HEADS) * RET_DK ** -0.5, positions)
    vh = to_heads(v.astype(f32), RET_HEADS)
    log_gamma = jnp.log(1.0 - 2.0 ** (-5.0 - jnp.arange(RET_HEADS, dtype=f32)))
    log_a = jnp.broadcast_to(log_gamma[None, :, None], (b, RET_HEADS, t))
    o = chunked_scalar_decay(qh, kh, vh, log_a, RET_CHUNK)
    return from_heads(rms_unit(o))


def gla_branch(q, k, v, gk_low, gk_w2, gk_b, norm_g):
    f32 = jnp.float32
    log_g = jax.nn.log_sigmoid((gk_low @ gk_w2 + gk_b).astype(f32)) / GLA_GATE_NORMALIZER
    qh = to_heads(q.astype(f32), GLA_HEADS) * GLA_DK ** -0.5
    kh = to_heads(k.astype(f32), GLA_HEADS)
    vh = to_heads(v.astype(f32), GLA_HEADS)
    o = chunked_vector_decay(qh, kh, vh, to_heads(log_g, GLA_HEADS), GLA_CHUNK)
    return from_heads(rms_unit(o) * norm_g.astype(f32))


def ssd_branch(xbc, dt_raw, z, conv_w, conv_b, dt_bias, a_log, d_skip, norm_g):
    f32 = jnp.float32
    xbc = jax.nn.silu(causal_depthwise_conv(xbc.astype(f32), conv_w.astype(f32), conv_b.astype(f32)))
    xs, bm, cm = split_cols(xbc, (BRANCH_WIDTH, SSD_GROUPS * SSD_STATE, SSD_GROUPS * SSD_STATE))
    rep = SSD_HEADS // SSD_GROUPS
    xh = to_heads(xs, SSD_HEADS)
    bh = jnp.repeat(to_heads(bm, SSD_GROUPS), rep, axis=1)
    ch = jnp.repeat(to_heads(cm, SSD_GROUPS), rep, axis=1)
    dt = jax.nn.softplus(dt_raw.astype(f32) + dt_bias.astype(f32)).transpose(0, 2, 1)
    a = -jnp.exp(a_log.astype(f32))
    y = chunked_scalar_decay(ch, bh, xh * dt[..., None], dt * a[None, :, None], SSD_CHUNK)
    y = from_heads(y + d_skip.astype(f32)[None, :, None, None] * xh)
    y = y * jax.nn.silu(z.astype(f32))
    b, t, _ = y.shape
    y = rms_unit(y.reshape(b, t, SSD_GROUPS, -1)).reshape(b, t, BRANCH_WIDTH)
    return y * norm_g.astype(f32)


def rwkv7_branch(u, mu, w0, w2, a0, a2, k_k, k_a, r_k, ln_g, ln_b):
    f32 = jnp.float32
    b, t, _ = u.shape
    u = u.astype(f32)
    u_prev = jnp.pad(u, ((0, 0), (1, 0), (0, 0)))[:, :-1]
    xs = u + (u_prev - u) * mu.astype(f32)
    r, k, v, wl, al = split_cols(xs, (BRANCH_WIDTH, BRANCH_WIDTH, BRANCH_WIDTH, RWKV_DECAY_RANK, RWKV_ICL_RANK))
    log_w = -jax.nn.softplus(-(w0.astype(f32) + jnp.tanh(wl) @ w2.astype(f32))) - 0.5
    decay = jnp.exp(-jnp.exp(log_w))
    a = jax.nn.sigmoid(a0.astype(f32) + al @ a2.astype(f32))
    kk = k * k_k.astype(f32)
    k = k * (1.0 + (a - 1.0) * k_a.astype(f32))
    shp = (b, t, RWKV_HEADS, RWKV_HEAD_DIM)
    r, decay, k, v, a, kk = [z_.reshape(shp) for z_ in (r, decay, k, v, a, kk)]
    kk = kk / jnp.maximum(jnp.sqrt(jnp.sum(kk * kk, axis=-1, keepdims=True)), 1e-12)

    def step(state, inp):
        r_t, w_t, k_t, v_t, a_t, b_t = inp
        sa = jnp.einsum('bhvk,bhk->bhv', state, a_t)
        state = (state * w_t[:, :, None, :] + sa[..., None] * b_t[:, :, None, :]
                 + v_t[..., None] * k_t[:, :, None, :])
        return state, jnp.einsum('bhvk,bhk->bhv', state, r_t)

    init = jnp.zeros((b, RWKV_HEADS, RWKV_HEAD_DIM, RWKV_HEAD_DIM), f32)
    xs_seq = tuple(jnp.moveaxis(z_, 1, 0) for z_ in (r, decay, k, v, -kk, kk * a))
    _, y = lax.scan(step, init, xs_seq)
    y = jnp.moveaxis(y, 0, 1)
    mean = jnp.mean(y, axis=-1, keepdims=True)
    var = jnp.mean(jnp.square(y - mean), axis=-1, keepdims=True)
    y = ((y - mean) * lax.rsqrt(var + RWKV_LN_EPS)).reshape(b, t, BRANCH_WIDTH)
    y = y * ln_g.astype(f32) + ln_b.astype(f32)
    bonus = jnp.sum(r * k * r_k.astype(f32), axis=-1, keepdims=True) * v
    return y + bonus.reshape(b, t, BRANCH_WIDTH)


def memory_branch(q, mem_n, w_kv):
    f32 = jnp.float32
    km, vm = split_cols(mem_n @ w_kv, (MEM_WIDTH, MEM_WIDTH))
    qh = to_heads(q.astype(f32), MEM_HEADS) * MEM_HEAD_DIM ** -0.5
    kh = to_heads(km.astype(f32), MEM_HEADS)
    vh = to_heads(vm.astype(f32), MEM_HEADS)
    p = jax.nn.softmax(jnp.einsum('bhtd,bhmd->bhtm', qh, kh), axis=-1)
    return from_heads(jnp.einsum('bhtm,bhmd->bhtd', p, vh))


def setup_inputs(seed: int = 0) -> dict:
    key = jax.random.key(seed)
    f32 = jnp.float32
    counter = [0]

    def nk():
        counter[0] += 1
        return jax.random.fold_in(key, counter[0])

    def nrm(shape, scale):
        return jax.random.normal(nk(), shape, f32) * scale

    def gain(shape):
        return 1.0 + nrm(shape, 0.05)

    L, D, W = DEPTH, D_MODEL, BRANCH_WIDTH
    x = nrm((BATCH, SEQ, D), 1.0)
    mem = nrm((BATCH, MEM_TOKENS, D), 1.0)
    offset = jax.random.randint(nk(), (BATCH, 1), 0, MAX_POS_OFFSET, dtype=jnp.int32)
    positions = offset + jnp.arange(SEQ, dtype=jnp.int32)[None, :]
    dt0 = jnp.exp(jax.random.uniform(nk(), (L, SSD_HEADS), f32, math.log(1e-3), math.log(1e-1)))
    return {
        'x': x,
        'mem': mem,
        'positions': positions,
        'norm_g': gain((L, D)),
        'w_in': nrm((L, D, IN_TOTAL), D ** -0.5),
        'gla_gk_w2': nrm((L, GLA_GATE_RANK, GLA_HEADS * GLA_DK), GLA_GATE_RANK ** -0.5),
        'gla_gk_b': nrm((L, GLA_HEADS * GLA_DK), 0.1),
        'gla_norm_g': gain((L, GLA_DV)),
        'ssd_conv_w': nrm((L, SSD_CONV, SSD_CONV_CH), SSD_CONV ** -0.5),
        'ssd_conv_b': nrm((L, SSD_CONV_CH), 0.02),
        'ssd_dt_bias': dt0 + jnp.log(-jnp.expm1(-dt0)),
        'ssd_a_log': jnp.log(jax.random.uniform(nk(), (L, SSD_HEADS), f32, 1.0, 16.0)),
        'ssd_d': gain((L, SSD_HEADS)),
        'ssd_norm_g': gain((L, W)),
        'rwkv_mu': jax.random.uniform(nk(), (L, RWKV_SHIFT_WIDTH), f32),
        'rwkv_w0': jnp.linspace(-6.0, -1.0, W, dtype=f32)[None, :] + nrm((L, W), 0.1),
        'rwkv_w2': nrm((L, RWKV_DECAY_RANK, W), 0.5 * RWKV_DECAY_RANK ** -0.5),
        'rwkv_a0': nrm((L, W), 0.1),
        'rwkv_a2': nrm((L, RWKV_ICL_RANK, W), 0.5 * RWKV_ICL_RANK ** -0.5),
        'rwkv_k_k': 0.85 + nrm((L, W), 0.05),
        'rwkv_k_a': gain((L, W)),
        'rwkv_r_k': nrm((L, RWKV_HEADS, RWKV_HEAD_DIM), 0.1),
        'rwkv_ln_g': gain((L, W)),
        'rwkv_ln_b': nrm((L, W), 0.02),
        'mem_norm_g': gain((L, D)),
        'w_mem_kv': nrm((L, D, 2 * MEM_WIDTH), D ** -0.5),
        'w_up_ret': nrm((L, W, D), W ** -0.5),
        'w_up_gla': nrm((L, W, D), W ** -0.5),
        'w_up_ssd': nrm((L, W, D), W ** -0.5),
        'w_up_rwkv': nrm((L, W, D), W ** -0.5),
        'w_up_mem': nrm((L, MEM_WIDTH, D), MEM_WIDTH ** -0.5),
        'w_out': nrm((L, D, D), D ** -0.5),
        'final_norm_g': gain((D,)),
    }


def reference(x, mem, positions, norm_g, w_in, gla_gk_w2, gla_gk_b, gla_norm_g,
              ssd_conv_w, ssd_conv_b, ssd_dt_bias, ssd_a_log, ssd_d, ssd_norm_g,
              rwkv_mu, rwkv_w0, rwkv_w2, rwkv_a0, rwkv_a2, rwkv_k_k, rwkv_k_a, rwkv_r_k,
              rwkv_ln_g, rwkv_ln_b, mem_norm_g, w_mem_kv,
              w_up_ret, w_up_gla, w_up_ssd, w_up_rwkv, w_up_mem, w_out, final_norm_g):
    f32 = jnp.float32
    b, t, d = x.shape
    for l in range(DEPTH):
        h = rms_norm(x, norm_g[l])
        p = h @ w_in[l]
        (ret_q, ret_k, ret_v, ret_g, gla_q, gla_k, gla_v, gla_gk, gla_g,
         ssd_xbc, ssd_dt, ssd_z, rwkv_in, rwkv_g, mem_q, gates) = split_cols(p, IN_SIZES)

        o_ret = retention_branch(ret_q, ret_k, ret_v, positions) * jax.nn.silu(ret_g.astype(f32))
        o_gla = gla_branch(gla_q, gla_k, gla_v, gla_gk, gla_gk_w2[l], gla_gk_b[l],
                           gla_norm_g[l]) * jax.nn.silu(gla_g.astype(f32))
        o_ssd = ssd_branch(ssd_xbc, ssd_dt, ssd_z, ssd_conv_w[l], ssd_conv_b[l], ssd_dt_bias[l],
                           ssd_a_log[l], ssd_d[l], ssd_norm_g[l])
        o_rwkv = rwkv7_branch(rwkv_in, rwkv_mu[l], rwkv_w0[l], rwkv_w2[l], rwkv_a0[l], rwkv_a2[l],
                              rwkv_k_k[l], rwkv_k_a[l], rwkv_r_k[l], rwkv_ln_g[l],
                              rwkv_ln_b[l]) * jax.nn.silu(rwkv_g.astype(f32))
        o_mem = memory_branch(mem_q, rms_norm(mem, mem_norm_g[l]), w_mem_kv[l])

        g = jax.nn.sigmoid(gates.astype(f32)).reshape(b, t, N_BRANCHES, d)
        branches = (o_ret.astype(x.dtype) @ w_up_ret[l], o_gla.astype(x.dtype) @ w_up_gla[l],
                    o_ssd.astype(x.dtype) @ w_up_ssd[l], o_rwkv.astype(x.dtype) @ w_up_rwkv[l],
                    o_mem.astype(x.dtype) @ w_up_mem[l])
        merged = g[:, :, 0] * branches[0].astype(f32)
        for i in range(1, N_BRANCHES):
            merged = merged + g[:, :, i] * branches[i].astype(f32)
        x = x + merged.astype(x.dtype) @ w_out[l]
    return rms_norm(x, final_norm_g)
```

```python
import contextlib
import math
import numpy as np
import concourse.bass as bass
import concourse.mybir as mybir
from concourse.bass_utils import run_bass_kernel_spmd

F32 = mybir.dt.float32
I32 = mybir.dt.int32
AF = mybir.ActivationFunctionType
ALU = mybir.AluOpType
AX = mybir.AxisListType

SEQ = 4096
D = 1024
T = 128
BLK = 256
NJ = BLK // T
IN_TOTAL = 12184
C_RET_Q, C_RET_K, C_RET_V, C_RET_G = 0, 256, 512, 1024
C_GLA_Q, C_GLA_K, C_GLA_V, C_GLA_GK, C_GLA_G = 1536, 1792, 2048, 2560, 2576
C_SSD_XBC, C_SSD_DT, C_SSD_Z = 3088, 4112, 4120
C_RWKV_IN, C_RWKV_G, C_MEM_Q, C_GATES = 4632, 6296, 6808, 7064
SEM_LIMIT = 30000
TWO_PI = 2.0 * math.pi


class Buf:
    __slots__ = ("w", "r")

    def __init__(self):
        self.w = None
        self.r = {}


class Tl:
    def __init__(self, t):
        self.t = t
        self.b = Buf()

    def __getitem__(self, k):
        return self.t[k]


def _bufs(lst):
    out = []
    for x in lst:
        if x is None:
            continue
        out.append(x.b if isinstance(x, Tl) else x)
    return out


class KB:
    def __init__(self, nc):
        self.nc = nc
        self.es = contextlib.ExitStack()
        self.eng = {"pe": nc.tensor, "dve": nc.vector, "act": nc.scalar, "pool": nc.gpsimd, "sp": nc.sync}
        self.nsem = 0
        self.sem = {}
        self.cnt = {}
        for e in ("pe", "dve", "act", "pool"):
            self.sem[e] = self.newsem(e)
            self.cnt[e] = 0
        self.seen = {e: {} for e in self.eng}
        self.NS = 8
        self.dsem = {q: [self.newsem("d" + q) for _ in range(self.NS)] for q in ("sp", "pool")}
        self.dval = {q: [0] * self.NS for q in ("sp", "pool")}
        self.drr = {q: 0 for q in ("sp", "pool")}
        self.ntile = 0
        self.ninst = 0

    def newsem(self, name):
        self.nsem += 1
        return self.es.enter_context(self.nc.semaphore(f"{name}_{self.nsem}"))

    def sb(self, shape, dtype=F32, name=None):
        self.ntile += 1
        return Tl(self.es.enter_context(self.nc.sbuf_tensor(f"{name or 'sb'}_{self.ntile}", list(shape), dtype)))

    def ps(self, shape=(128, 512), dtype=F32, name=None):
        self.ntile += 1
        return Tl(self.es.enter_context(self.nc.psum_tensor(f"{name or 'ps'}_{self.ntile}", list(shape), dtype)))

    def _collect(self, e, reads, writes):
        need = {}

        def add(tok):
            if tok is None:
                return
            sem, val, te = tok
            if te == "pe" and e == "pe":
                return
            if self.seen[e].get(sem, 0) >= val:
                return
            if need.get(sem, 0) < val:
                need[sem] = val

        for b in reads:
            add(b.w)
        for b in writes:
            add(b.w)
            for tok in b.r.values():
                add(tok)
        return need

    def _mark(self, tok, reads, writes, e):
        for b in reads:
            b.r[e] = tok
        for b in writes:
            b.w = tok
            b.r = {}

    def op(self, e, emit, r=(), w=()):
        reads, writes = _bufs(r), _bufs(w)
        need = self._collect(e, reads, writes)
        items = list(need.items())
        eng = self.eng[e]
        for sem, val in items[:-1]:
            eng.wait_ge(sem, val)
            self.seen[e][sem] = val
        ins = emit()
        if items:
            sem, val = items[-1]
            ins._wait_ge(sem, val)
            self.seen[e][sem] = val
        if self.cnt[e] >= SEM_LIMIT:
            self.sem[e] = self.newsem(e)
            self.cnt[e] = 0
        self.cnt[e] += 1
        ins.then_inc(self.sem[e], 1)
        self.ninst += 1
        self._mark((self.sem[e], self.cnt[e], e), reads, writes, e)
        return ins

    def dma(self, q, out, in_, r=(), w=(), **kw):
        reads, writes = _bufs(r), _bufs(w)
        need = self._collect(q, reads, writes)
        slot = self.drr[q] % self.NS
        self.drr[q] += 1
        sem = self.dsem[q][slot]
        prev = self.dval[q][slot]
        if prev > 0 and self.seen[q].get(sem, 0) < prev:
            need[sem] = max(need.get(sem, 0), prev)
        eng = self.eng[q]
        for s, v in need.items():
            eng.wait_ge(s, v)
            self.seen[q][s] = v
        eng.dma_start(out=out, in_=in_, **kw).then_inc(sem, 16)
        self.dval[q][slot] = prev + 16
        self.ninst += 1
        self._mark((sem, prev + 16, "dma"), reads, writes, "dma_" + q + str(slot))

    def finish(self):
        sp = self.nc.sync
        for q in ("sp", "pool"):
            for s, v in zip(self.dsem[q], self.dval[q]):
                if v > 0:
                    sp.wait_ge(s, v)
        for e in ("pe", "dve", "act", "pool"):
            if self.cnt[e] > 0:
                sp.wait_ge(self.sem[e], self.cnt[e])

    def mm(self, out, lhsT, rhs, start=True, stop=True, r=(), w=()):
        nc = self.nc
        return self.op("pe", lambda: nc.tensor.matmul(out, lhsT, rhs, start=start, stop=stop), r, w)

    def V(self, fn, r=(), w=()):
        return self.op("dve", fn, r, w)

    def A(self, fn, r=(), w=()):
        return self.op("act", fn, r, w)

    def G(self, fn, r=(), w=()):
        return self.op("pool", fn, r, w)


class Ring:
    def __init__(self, tiles):
        self.tiles = tiles
        self.i = 0

    def next(self):
        t = self.tiles[self.i % len(self.tiles)]
        self.i += 1
        return t


def make_consts():
    cols = {}
    parts = []
    off = [0]

    def add(name, arr):
        arr = np.asarray(arr, np.float32)
        assert arr.shape[0] == 128
        arr = arr.reshape(128, -1)
        cols[name] = (off[0], arr.shape[1])
        parts.append(arr)
        off[0] += arr.shape[1]

    i = np.arange(128)
    add("ident", np.eye(128))
    add("maskT", (i[:, None] <= i[None, :]))
    add("maskS", (i[:, None] < i[None, :]))
    add("maskL", (i[:, None] > i[None, :]))
    add("ones", np.ones((128, 128)))
    add("negm", np.where(i[:, None] > i[None, :], -30000.0, 0.0))
    inv = 1.0 / (10000.0 ** np.linspace(0.0, 1.0, 32, dtype=np.float32))
    invrow = np.zeros((128, 64), np.float32)
    invrow[0, :] = np.repeat(inv.astype(np.float32), 2)
    add("invrow", invrow)
    rot = np.zeros((128, 64), np.float32)
    for p in range(32):
        rot[2 * p + 1, 2 * p] = -1.0
        rot[2 * p, 2 * p + 1] = 1.0
    add("rot", rot)
    lg = np.log(1.0 - 2.0 ** (-5.0 - np.arange(4, dtype=np.float64)))
    dec = np.zeros((128, 4, 128))
    for h in range(4):
        dec[:, h, :] = np.where(i[:, None] <= i[None, :], np.exp(lg[h] * (i[None, :] - i[:, None])), 0.0)
    add("retdec", dec)
    qs = np.zeros((128, 4, 128))
    for h in range(4):
        qs[:, h, :] = np.exp(lg[h] * (i[None, :] + 1))
    add("retqs", qs)
    ks = np.zeros((128, 4))
    for h in range(4):
        ks[:, h] = np.exp(lg[h] * (127 - i))
    add("retks", ks)
    bo = np.zeros((128, 128))
    bo[:64, :64] = 1
    bo[64:, 64:] = 1
    add("blk64", bo)
    hs = np.zeros((128, 2))
    hs[:64, 0] = 1
    hs[64:, 1] = 1
    add("headsel", hs)
    return np.concatenate(parts, axis=1), cols, [float(np.exp(lg[h] * 128)) for h in range(4)]


CST, CCOL, RET_SDEC = make_consts()

PARAM_NAMES = ["norm_g", "w_in", "gla_gk_w2", "gla_gk_b", "gla_norm_g", "ssd_conv_w", "ssd_conv_b",
               "ssd_dt_bias", "ssd_a_log", "ssd_d", "ssd_norm_g", "rwkv_mu", "rwkv_w0", "rwkv_w2",
               "rwkv_a0", "rwkv_a2", "rwkv_k_k", "rwkv_k_a", "rwkv_r_k", "rwkv_ln_g", "rwkv_ln_b",
               "mem_norm_g", "w_mem_kv", "w_up_ret", "w_up_gla", "w_up_ssd", "w_up_rwkv", "w_up_mem",
               "w_out", "final_norm_g"]
SHAPES = {
    "x": [SEQ, D], "mem": [256, D], "positions": [1, SEQ],
    "norm_g": [2, D], "w_in": [2, D, IN_TOTAL], "gla_gk_w2": [2, 16, 256], "gla_gk_b": [2, 256],
    "gla_norm_g": [2, 128], "ssd_conv_w": [2, 4, 1024], "ssd_conv_b": [2, 1024], "ssd_dt_bias": [2, 8],
    "ssd_a_log": [2, 8], "ssd_d": [2, 8], "ssd_norm_g": [2, 512], "rwkv_mu": [2, 1664],
    "rwkv_w0": [2, 512], "rwkv_w2": [2, 64, 512], "rwkv_a0": [2, 512], "rwkv_a2": [2, 64, 512],
    "rwkv_k_k": [2, 512], "rwkv_k_a": [2, 512], "rwkv_r_k": [2, 512], "rwkv_ln_g": [2, 512],
    "rwkv_ln_b": [2, 512], "mem_norm_g": [2, D], "w_mem_kv": [2, D, 512], "w_up_ret": [2, 512, D],
    "w_up_gla": [2, 512, D], "w_up_ssd": [2, 512, D], "w_up_rwkv": [2, 512, D], "w_up_mem": [2, 256, D],
    "w_out": [2, D, D], "final_norm_g": [1, D],
}


def build(nblk=SEQ // BLK, nlayers=2, debug=None, branches=("ret", "gla", "ssd", "rwkv", "mem"), dbg_what=0, dbg_layer=0, do_merge=True):
    nc = bass.Bass("TRN2", target_bir_lowering=False)
    k = KB(nc)
    dr = {}
    ntok_all = nblk * BLK
    for n, shp in SHAPES.items():
        shp = list(shp)
        if n in ("x",):
            shp[0] = ntok_all
        elif n == "positions":
            shp[1] = ntok_all
        elif n not in ("mem", "final_norm_g"):
            shp[0] = nlayers
        dr[n] = nc.dram_tensor(n, shp, I32 if n == "positions" else F32, kind="ExternalInput").ap()
    cst_d = nc.dram_tensor("cst", list(CST.shape), F32, kind="ExternalInput").ap()
    out_d = nc.dram_tensor("out", [ntok_all, D], F32, kind="ExternalOutput").ap()
    x1_d = nc.dram_tensor("x1s", [ntok_all, D], F32, kind="Internal").ap()
    x1_b = [Buf() for _ in range(SEQ // BLK)]
    dbg_d = None
    if debug is not None:
        dbg_d = nc.dram_tensor("dbg", [ntok_all, debug], F32, kind="ExternalOutput").ap()

    cst = k.sb([128, CST.shape[1]], name="cst")
    k.dma("sp", cst[:, :], cst_d[:, :], w=[cst])

    def C(name, p0=0, p1=128):
        o, n = CCOL[name]
        return cst[p0:p1, o:o + n]

    ident = C("ident")


    psr = Ring([k.ps() for _ in range(6)])
    pacc = [k.ps(name='pacc') for _ in range(2)]
    ntok = nblk * BLK
    NCH = SEQ // BLK

    def bc(ap_row, n):
        return ap_row.to_broadcast([128, n])

    xb = k.sb([128, NJ, D], name="xb")
    hT = k.sb([128, 8, BLK], name="hT")
    xn = k.sb([128, D], name="xn")
    junk = k.sb([128, D], name="junk")
    ss = k.sb([128, 16], name="ss")
    wst = Ring([k.sb([128, 8, 512], name="wst") for _ in range(2)])
    TK = [k.sb([128, NJ, 512], name="TK") for _ in range(4)]
    FM = [k.sb([128, BLK], name="FM") for _ in range(16)]
    sq = Ring([k.sb([128, 128], name="sq") for _ in range(16)])
    sq2 = Ring([k.sb([128, 256], name="sq2") for _ in range(6)])
    stt = Ring([k.sb([128, 32], name="st") for _ in range(8)])
    oT = [k.sb([128, 4, BLK], name="oT") for _ in range(4)] + [k.sb([128, 2, BLK], name="oTm")]
    obr = Ring([k.sb([128, 512], name="obr") for _ in range(2)])
    posi = k.sb([1, BLK], I32, name="posi")
    posf = k.sb([1, BLK], name="posf")
    cosT = k.sb([64, BLK], name="cosT")
    sinT = k.sb([64, BLK], name="sinT")
    rope_qi = k.sb([64, BLK], I32, name="rope_qi")
    rope_qf = k.sb([64, BLK], name="rope_qf")
    pi_c = k.sb([128, 1], name="pi_c")
    k.V(lambda: nc.vector.memset(pi_c[:, :], -math.pi), w=[pi_c])
    gcol = k.sb([128, 8], name="gcol")
    gla_w2 = k.sb([16, 256], name="gla_w2")
    gla_b = k.sb([1, 256], name="gla_b")
    gla_ng = k.sb([128, 128], name="gla_ng")
    ssd_cw = k.sb([128, 8, 4], name="ssd_cw")
    ssd_cb = k.sb([128, 8], name="ssd_cb")
    ssd_sm = k.sb([128, 32], name="ssd_sm")
    ssd_ng = k.sb([128, 512], name="ssd_ng")
    ssd_carry = k.sb([128, 8, 4], name="ssd_carry")
    rw_cols = k.sb([128, 64], name="rw_cols")
    rw_w2a2 = k.sb([128, 512], name="rw_w2a2")
    rw_lng = k.sb([128, 512], name="rw_lng")
    rw_lnb = k.sb([128, 512], name="rw_lnb")
    rw_carry = k.sb([128, 16], name="rw_carry")
    kmT = k.sb([64, 4, 256], name="kmT")
    vm = k.sb([128, 2, 256], name="vm")
    ret_S = [k.sb([64, 128], name="ret_S") for _ in range(4)]
    gla_S = [k.sb([64, 128], name="gla_S") for _ in range(4)]
    ssd_S = k.sb([128, 512], name="ssd_S")
    rw_S = [k.sb([128, 64], name="rw_S") for _ in range(4)]

    def load_w(wd, c0, ncol, rows=8, r0=0):
        wt = wst.next()
        k.dma("sp", wt[:, 0:rows, 0:ncol],
              wd[r0 * 128:(r0 + rows) * 128, :].rearrange("(c p) n -> p c n", p=128)[:, :, c0:c0 + ncol], w=[wt])
        return wt

    def proj_fm(l, c0, ncol, evac):
        wt = load_w(dr["w_in"][l], c0, ncol)
        pa = psr.next()
        for dc in range(8):
            k.mm(pa[0:ncol, 0:BLK], wt[:, dc, 0:ncol], hT[:, dc, :], start=(dc == 0), stop=(dc == 7), r=[wt, hT], w=[pa])
        evac(pa)

    def proj_tok(l, c0, ncol, evac):
        for g0 in range(0, ncol, 512):
            w_ = min(512, ncol - g0)
            wt = load_w(dr["w_in"][l], c0 + g0, w_)
            for j in range(NJ):
                pa = psr.next()
                for dc in range(8):
                    k.mm(pa[:, 0:w_], hT[:, dc, j * T:(j + 1) * T], wt[:, dc, 0:w_], start=(dc == 0), stop=(dc == 7),
                         r=[wt, hT], w=[pa])
                evac(j, g0, w_, pa)

    def tr(dst_ap, dst_tl, src_ap, src_tl, npart, nfree, eng="act"):
        pa = psr.next()
        k.op("pe", lambda: nc.tensor.transpose(pa[0:nfree, 0:npart], src_ap, ident[0:npart, 0:npart]),
             r=[src_tl, cst], w=[pa])
        if eng == "act":
            k.A(lambda: nc.scalar.copy(out=dst_ap, in_=pa[0:nfree, 0:npart]), r=[pa], w=[dst_tl])
        else:
            k.V(lambda: nc.vector.tensor_copy(out=dst_ap, in_=pa[0:nfree, 0:npart]), r=[pa], w=[dst_tl])

    def rstd_groups(y_tl, y_ap_of, n, w, eps, st, c0):
        for g in range(n):
            k.A(lambda: nc.scalar.activation(out=junk[:, 0:w], in_=y_ap_of(g), func=AF.Square,
                                             accum_out=st[:, c0 + g:c0 + g + 1]), r=[y_tl], w=[junk, st])
        k.V(lambda: nc.vector.tensor_scalar(out=st[:, c0 + n:c0 + 2 * n], in0=st[:, c0:c0 + n], scalar1=1.0 / w,
                                            scalar2=eps, op0=ALU.mult, op1=ALU.add), r=[st], w=[st])
        k.A(lambda: nc.scalar.sqrt(out=st[:, c0 + n:c0 + 2 * n], in_=st[:, c0 + n:c0 + 2 * n]), r=[st], w=[st])
        k.V(lambda: nc.vector.reciprocal(out=st[:, c0 + 2 * n:c0 + 3 * n], in_=st[:, c0 + n:c0 + 2 * n]), r=[st], w=[st])

    def softplus_ip(x_ap, x_tl, n):
        a = sq2.next()
        k.A(lambda: nc.scalar.activation(out=a[:, 0:n], in_=x_ap, func=AF.Abs), r=[x_tl], w=[a])
        k.A(lambda: nc.scalar.activation(out=a[:, 0:n], in_=a[:, 0:n], func=AF.Exp, scale=-1.0), r=[a], w=[a])
        k.A(lambda: nc.scalar.activation(out=a[:, 0:n], in_=a[:, 0:n], func=AF.Ln, bias=1.0), r=[a], w=[a])
        k.V(lambda: nc.vector.scalar_tensor_tensor(out=x_ap, in0=x_ap, scalar=0.0, in1=a[:, 0:n], op0=ALU.max,
                                                   op1=ALU.add), r=[x_tl, a], w=[x_tl])

    def emit_out(bi, l, blk, j, o, width):
        t0 = blk * BLK
        for c in range(width // 128):
            tr(oT[bi][:, c, j * T:(j + 1) * T], oT[bi], o[:, c * 128:(c + 1) * 128], o, 128, 128,
               eng=("act" if c % 2 == 0 else "dve"))
        if debug is not None and l == dbg_layer:
            k.dma("pool", dbg_d[t0 + j * T:t0 + (j + 1) * T, bi * 512:bi * 512 + width], o[:, 0:width], r=[o])

    def load_params(l):
        NCg = dict(allow_slow_non_contiguous=True)
        k.dma("sp", gcol[:, :], dr["norm_g"][l].rearrange("(c p) -> p c", p=128), w=[gcol], **NCg)
        k.dma("sp", gla_w2[:, :], dr["gla_gk_w2"][l], w=[gla_w2])
        k.dma("sp", gla_b[:, :], dr["gla_gk_b"][l:l + 1, :], w=[gla_b])
        k.dma("sp", gla_ng[:, :], bc(dr["gla_norm_g"][l:l + 1, :], 128), w=[gla_ng])
        for j_ in range(4):
            k.dma("sp", ssd_cw[:, :, j_], dr["ssd_conv_w"][l, j_].rearrange("(c p) -> p c", p=128), w=[ssd_cw], **NCg)
        k.dma("sp", ssd_cb[:, :], dr["ssd_conv_b"][l].rearrange("(c p) -> p c", p=128), w=[ssd_cb], **NCg)
        k.dma("sp", ssd_sm[:, 0:8], bc(dr["ssd_dt_bias"][l:l + 1, :], 8), w=[ssd_sm])
        k.dma("sp", ssd_sm[:, 8:16], bc(dr["ssd_a_log"][l:l + 1, :], 8), w=[ssd_sm])
        k.dma("sp", ssd_sm[:, 16:24], bc(dr["ssd_d"][l:l + 1, :], 8), w=[ssd_sm])
        k.A(lambda: nc.scalar.activation(out=ssd_sm[:, 8:16], in_=ssd_sm[:, 8:16], func=AF.Exp), r=[ssd_sm], w=[ssd_sm])
        k.V(lambda: nc.vector.tensor_scalar(out=ssd_sm[:, 8:16], in0=ssd_sm[:, 8:16], scalar1=-1.0, scalar2=None,
                                            op0=ALU.mult), r=[ssd_sm], w=[ssd_sm])
        k.dma("sp", ssd_ng[:, :], bc(dr["ssd_norm_g"][l:l + 1, :], 512), w=[ssd_ng])
        k.dma("sp", rw_cols[:, 0:13], dr["rwkv_mu"][l].rearrange("(c p) -> p c", p=128), w=[rw_cols], **NCg)
        for i_, nm in enumerate(["rwkv_w0", "rwkv_a0", "rwkv_k_k", "rwkv_k_a", "rwkv_r_k"]):
            k.dma("sp", rw_cols[:, 16 + 4 * i_:20 + 4 * i_], dr[nm][l].rearrange("(c p) -> p c", p=128), w=[rw_cols], **NCg)
        k.V(lambda: nc.vector.tensor_scalar(out=rw_cols[:, 40:53], in0=rw_cols[:, 0:13], scalar1=-1.0, scalar2=1.0,
                                            op0=ALU.mult, op1=ALU.add), r=[rw_cols], w=[rw_cols])
        k.dma("sp", rw_w2a2[0:64, :], dr["rwkv_w2"][l], w=[rw_w2a2])
        k.dma("sp", rw_w2a2[64:128, :], dr["rwkv_a2"][l], w=[rw_w2a2])
        k.dma("sp", rw_lng[:, :], bc(dr["rwkv_ln_g"][l:l + 1, :], 512), w=[rw_lng])
        k.dma("sp", rw_lnb[:, :], bc(dr["rwkv_ln_b"][l:l + 1, :], 512), w=[rw_lnb])
        mg = stt.next()
        k.dma("sp", mg[:, 0:8], dr["mem_norm_g"][l].rearrange("(c p) -> p c", p=128), w=[mg], **NCg)
        memT = FM[0:8]
        for mt in range(2):
            k.dma("sp", xn[:, :], dr["mem"][mt * 128:(mt + 1) * 128, :], w=[xn])
            st = stt.next()
            k.A(lambda: nc.scalar.activation(out=junk[:, :], in_=xn[:, :], func=AF.Square, accum_out=st[:, 0:1]),
                r=[xn], w=[junk, st])
            k.V(lambda: nc.vector.tensor_scalar(out=st[:, 1:2], in0=st[:, 0:1], scalar1=1.0 / D, scalar2=1e-6,
                                                op0=ALU.mult, op1=ALU.add), r=[st], w=[st])
            k.A(lambda: nc.scalar.sqrt(out=st[:, 1:2], in_=st[:, 1:2]), r=[st], w=[st])
            k.V(lambda: nc.vector.reciprocal(out=st[:, 2:3], in_=st[:, 1:2]), r=[st], w=[st])
            k.V(lambda: nc.vector.tensor_scalar(out=xn[:, :], in0=xn[:, :], scalar1=st[:, 2:3], scalar2=None,
                                                op0=ALU.mult), r=[xn, st], w=[xn])
            for dc in range(8):
                pa = psr.next()
                k.op("pe", lambda: nc.tensor.transpose(pa[:, 0:T], xn[:, dc * T:(dc + 1) * T], ident), r=[xn, cst], w=[pa])
                k.A(lambda: nc.scalar.activation(out=memT[dc][:, mt * T:(mt + 1) * T], in_=pa[:, 0:T], func=AF.Identity,
                                                 scale=mg[:, dc:dc + 1]), r=[pa, mg], w=[memT[dc]])
        wt = load_w(dr["w_mem_kv"][l], 0, 512)
        for h in range(4):
            pa = psr.next()
            for dc in range(8):
                k.mm(pa[0:64, 0:256], wt[:, dc, h * 64:(h + 1) * 64], memT[dc][:, 0:256], start=(dc == 0), stop=(dc == 7),
                     r=[wt, memT[dc]], w=[pa])
            k.A(lambda: nc.scalar.copy(out=kmT[:, h, :], in_=pa[0:64, 0:256]), r=[pa], w=[kmT])
        for mt in range(2):
            pa = psr.next()
            for dc in range(8):
                k.mm(pa[:, 0:256], memT[dc][:, mt * T:(mt + 1) * T], wt[:, dc, 256:512], start=(dc == 0), stop=(dc == 7),
                     r=[wt, memT[dc]], w=[pa])
            k.A(lambda: nc.scalar.copy(out=vm[:, mt, :], in_=pa[:, 0:256]), r=[pa], w=[vm])
        for s_ in ret_S + gla_S + rw_S + [ssd_S]:
            k.V(lambda: nc.vector.memset(s_[:, :], 0.0), w=[s_])
        k.V(lambda: nc.vector.memset(ssd_carry[:, :, :], 0.0), w=[ssd_carry])
        k.V(lambda: nc.vector.memset(rw_carry[:, :], 0.0), w=[rw_carry])

    def la_step(ks_ap, ks_tl, qs_ap, qs_tl, qi_ap, qi_tl, mask_ap, mask_tl, v_ap, v_tl, S, py_ap, py):
        psc = psr.next()
        k.mm(psc[:, 0:T], ks_ap, qs_ap, r=[ks_tl, qs_tl], w=[psc])
        sc = sq.next()
        k.V(lambda: nc.vector.tensor_tensor(out=sc[:, :], in0=psc[:, 0:T], in1=mask_ap, op=ALU.mult),
            r=[psc, mask_tl], w=[sc])
        k.mm(py_ap, sc[:, :], v_ap, start=True, stop=False, r=[sc, v_tl], w=[py])
        k.mm(py_ap, qi_ap, S[:, :], start=False, stop=True, r=[qi_tl, S], w=[py])

    def ret_block(l, blk):
        t0 = blk * BLK
        qT, kT = FM[0:4], FM[4:8]
        vt, gt = TK[0], TK[1]
        for c_base, dst, scale in ((C_RET_Q, qT, 1.0), (C_RET_K, kT, 0.125)):
            for h in range(4):
                def ev(pa, h=h, dst=dst, scale=scale):
                    ta, tb = FM[8], FM[9]
                    k.A(lambda: nc.scalar.activation(out=ta[0:64, :], in_=pa[0:64, 0:BLK], func=AF.Copy, scale=scale),
                        r=[pa], w=[ta])
                    pb = psr.next()
                    k.mm(pb[0:64, 0:BLK], C("rot", 0, 64), ta[0:64, :], r=[cst, ta], w=[pb])
                    k.V(lambda: nc.vector.tensor_tensor(out=tb[0:64, :], in0=pb[0:64, 0:BLK], in1=sinT[:, :], op=ALU.mult),
                        r=[pb, sinT], w=[tb])
                    k.G(lambda: nc.gpsimd.tensor_tensor(out=ta[0:64, :], in0=ta[0:64, :], in1=cosT[:, :], op=ALU.mult),
                        r=[ta, cosT], w=[ta])
                    k.V(lambda: nc.vector.tensor_tensor(out=dst[h][0:64, :], in0=ta[0:64, :], in1=tb[0:64, :], op=ALU.add),
                        r=[ta, tb], w=[dst[h]])
                proj_fm(l, c_base + 64 * h, 64, ev)
        proj_tok(l, C_RET_V, 512, lambda j, g0, w_, pa: k.A(
            lambda: nc.scalar.copy(out=vt[:, j, g0:g0 + w_], in_=pa[:, 0:w_]), r=[pa], w=[vt]))
        proj_tok(l, C_RET_G, 512, lambda j, g0, w_, pa: k.A(
            lambda: nc.scalar.activation(out=gt[:, j, g0:g0 + w_], in_=pa[:, 0:w_], func=AF.Silu), r=[pa], w=[gt]))
        ko, _ = CCOL["retks"]
        for j in range(NJ):
            py = pacc[j % 2]
            for h in range(4):
                qs_ = qT[h][0:64, j * T:(j + 1) * T]
                ks_ = kT[h][0:64, j * T:(j + 1) * T]
                vh = vt[:, j, h * 128:(h + 1) * 128]
                qi = sq.next()
                k.G(lambda: nc.gpsimd.tensor_tensor(out=qi[0:64, :], in0=qs_, in1=C("retqs", 0, 64)[:, h * T:(h + 1) * T],
                                                    op=ALU.mult), r=[qT[h], cst], w=[qi])
                la_step(ks_, kT[h], qs_, qT[h], qi[0:64, :], qi, C("retdec")[:, h * T:(h + 1) * T], cst, vh, vt,
                        ret_S[h], py[:, h * 128:(h + 1) * 128], py)
                kt = sq.next()
                pt = psr.next()
                k.op("pe", lambda: nc.tensor.transpose(pt[:, 0:64], ks_, ident[0:64, 0:64]), r=[kT[h], cst], w=[pt])
                k.A(lambda: nc.scalar.activation(out=kt[:, 0:64], in_=pt[:, 0:64], func=AF.Identity,
                                                 scale=cst[:, ko + h:ko + h + 1]), r=[pt, cst], w=[kt])
                pds = psr.next()
                k.mm(pds[0:64, 0:128], kt[:, 0:64], vh, r=[kt, vt], w=[pds])
                k.V(lambda: nc.vector.scalar_tensor_tensor(out=ret_S[h][:, :], in0=ret_S[h][:, :], scalar=RET_SDEC[h],
                                                           in1=pds[0:64, 0:128], op0=ALU.mult, op1=ALU.add),
                    r=[ret_S[h], pds], w=[ret_S[h]])
            st = stt.next()
            rstd_groups(py, lambda g: py[:, g * 128:(g + 1) * 128], 4, 128, 1e-6, st, 0)
            o = obr.next()
            for h in range(4):
                k.V(lambda: nc.vector.scalar_tensor_tensor(out=o[:, h * 128:(h + 1) * 128], in0=py[:, h * 128:(h + 1) * 128],
                                                           scalar=st[:, 8 + h:9 + h], in1=gt[:, j, h * 128:(h + 1) * 128],
                                                           op0=ALU.mult, op1=ALU.mult), r=[py, st, gt], w=[o])
            emit_out(0, l, blk, j, o, 512)

    def gla_block(l, blk):
        qT, kT = FM[0:4], FM[4:8]
        vt, gt = TK[0], TK[1]
        glow = FM[8]
        for h in range(4):
            proj_fm(l, C_GLA_Q + 64 * h, 64, lambda pa, h=h: k.A(
                lambda: nc.scalar.activation(out=qT[h][0:64, :], in_=pa[0:64, 0:BLK], func=AF.Copy, scale=0.125),
                r=[pa], w=[qT[h]]))
            proj_fm(l, C_GLA_K + 64 * h, 64, lambda pa, h=h: k.A(
                lambda: nc.scalar.copy(out=kT[h][0:64, :], in_=pa[0:64, 0:BLK]), r=[pa], w=[kT[h]]))
        proj_fm(l, C_GLA_GK, 16, lambda pa: k.A(
            lambda: nc.scalar.copy(out=glow[0:16, :], in_=pa[0:16, 0:BLK]), r=[pa], w=[glow]))
        proj_tok(l, C_GLA_V, 512, lambda j, g0, w_, pa: k.A(
            lambda: nc.scalar.copy(out=vt[:, j, g0:g0 + w_], in_=pa[:, 0:w_]), r=[pa], w=[vt]))
        proj_tok(l, C_GLA_G, 512, lambda j, g0, w_, pa: k.A(
            lambda: nc.scalar.activation(out=gt[:, j, g0:g0 + w_], in_=pa[:, 0:w_], func=AF.Silu), r=[pa], w=[gt]))
        for j in range(NJ):
            pl = psr.next()
            k.mm(pl[:, 0:256], glow[0:16, j * T:(j + 1) * T], gla_w2[:, :], start=True, stop=False, r=[glow, gla_w2], w=[pl])
            k.mm(pl[:, 0:256], C("ones", 0, 1), gla_b[:, :], start=False, stop=True, r=[cst, gla_b], w=[pl])
            lg = sq2.next()
            k.A(lambda: nc.scalar.activation(out=lg[:, :], in_=pl[:, 0:256], func=AF.Copy, scale=-1.0), r=[pl], w=[lg])
            softplus_ip(lg[:, :], lg, 256)
            k.V(lambda: nc.vector.tensor_scalar(out=lg[:, :], in0=lg[:, :], scalar1=-1.0 / 16.0, scalar2=None,
                                                op0=ALU.mult), r=[lg], w=[lg])
            py = pacc[j % 2]
            for h in range(4):
                pc = psr.next()
                k.mm(pc[0:64, 0:T], lg[:, h * 64:(h + 1) * 64], C("maskT"), r=[lg, cst], w=[pc])
                st = stt.next()
                k.V(lambda: nc.vector.tensor_copy(out=st[0:64, 0:1], in_=pc[0:64, 64:65]), r=[pc], w=[st])
                k.V(lambda: nc.vector.tensor_scalar(out=st[0:64, 1:2], in0=pc[0:64, 64:65], scalar1=-1.0, scalar2=None,
                                                    op0=ALU.mult), r=[pc], w=[st])
                ekin, eqin, eq = sq.next(), sq.next(), sq.next()
                k.A(lambda: nc.scalar.activation(out=ekin[0:64, :], in_=pc[0:64, 0:T], func=AF.Exp, scale=-1.0,
                                                 bias=st[0:64, 0:1]), r=[pc, st], w=[ekin])
                k.A(lambda: nc.scalar.activation(out=eqin[0:64, :], in_=pc[0:64, 0:T], func=AF.Exp, scale=1.0,
                                                 bias=st[0:64, 1:2]), r=[pc, st], w=[eqin])
                k.A(lambda: nc.scalar.activation(out=eq[0:64, :], in_=pc[0:64, 0:T], func=AF.Exp), r=[pc], w=[eq])
                k.A(lambda: nc.scalar.activation(out=st[0:64, 2:3], in_=pc[0:64, T - 1:T], func=AF.Exp), r=[pc], w=[st])
                k.A(lambda: nc.scalar.activation(out=st[0:64, 3:4], in_=pc[0:64, T - 1:T], func=AF.Exp, scale=1.0,
                                                 bias=st[0:64, 1:2]), r=[pc, st], w=[st])
                qs_ = qT[h][0:64, j * T:(j + 1) * T]
                ks_ = kT[h][0:64, j * T:(j + 1) * T]
                k.V(lambda: nc.vector.tensor_tensor(out=ekin[0:64, :], in0=ekin[0:64, :], in1=ks_, op=ALU.mult),
                    r=[ekin, kT[h]], w=[ekin])
                k.G(lambda: nc.gpsimd.tensor_tensor(out=eqin[0:64, :], in0=eqin[0:64, :], in1=qs_, op=ALU.mult),
                    r=[eqin, qT[h]], w=[eqin])
                k.G(lambda: nc.gpsimd.tensor_tensor(out=eq[0:64, :], in0=eq[0:64, :], in1=qs_, op=ALU.mult),
                    r=[eq, qT[h]], w=[eq])
                vh = vt[:, j, h * 128:(h + 1) * 128]
                la_step(ekin[0:64, :], ekin, eqin[0:64, :], eqin, eq[0:64, :], eq, C("maskT"), cst, vh, vt,
                        gla_S[h], py[:, h * 128:(h + 1) * 128], py)
                kt = sq.next()
                tr(kt[:, 0:64], kt, ekin[0:64, :], ekin, 64, 128)
                pds = psr.next()
                k.mm(pds[0:64, 0:128], kt[:, 0:64], vh, r=[kt, vt], w=[pds])
                k.V(lambda: nc.vector.tensor_scalar(out=gla_S[h][:, :], in0=gla_S[h][:, :], scalar1=st[0:64, 2:3],
                                                    scalar2=None, op0=ALU.mult), r=[gla_S[h], st], w=[gla_S[h]])
                k.V(lambda: nc.vector.scalar_tensor_tensor(out=gla_S[h][:, :], in0=pds[0:64, 0:128], scalar=st[0:64, 3:4],
                                                           in1=gla_S[h][:, :], op0=ALU.mult, op1=ALU.add),
                    r=[gla_S[h], pds, st], w=[gla_S[h]])
            st = stt.next()
            rstd_groups(py, lambda g: py[:, g * 128:(g + 1) * 128], 4, 128, 1e-6, st, 0)
            o = obr.next()
            for h in range(4):
                k.V(lambda: nc.vector.scalar_tensor_tensor(out=o[:, h * 128:(h + 1) * 128], in0=py[:, h * 128:(h + 1) * 128],
                                                           scalar=st[:, 8 + h:9 + h], in1=gt[:, j, h * 128:(h + 1) * 128],
                                                           op0=ALU.mult, op1=ALU.mult), r=[py, st, gt], w=[o])
                k.G(lambda: nc.gpsimd.tensor_tensor(out=o[:, h * 128:(h + 1) * 128], in0=o[:, h * 128:(h + 1) * 128],
                                                    in1=gla_ng[:, :], op=ALU.mult), r=[o, gla_ng], w=[o])
            emit_out(1, l, blk, j, o, 512)

    def ssd_block(l, blk):
        cv = FM[0:8]
        zt, dtt = TK[0], TK[1]
        raw = FM[8]
        for c in range(8):
            def ev(pa, c=c):
                k.A(lambda: nc.scalar.copy(out=raw[:, :], in_=pa[:, 0:BLK]), r=[pa], w=[raw])
                acc = cv[c]
                k.V(lambda: nc.vector.tensor_scalar(out=acc[:, :], in0=raw[:, :], scalar1=ssd_cw[:, c, 3:4], scalar2=None,
                                                    op0=ALU.mult), r=[raw, ssd_cw], w=[acc])
                for d_ in (1, 2, 3):
                    k.V(lambda: nc.vector.scalar_tensor_tensor(out=acc[:, d_:BLK], in0=raw[:, 0:BLK - d_],
                                                               scalar=ssd_cw[:, c, 3 - d_:4 - d_], in1=acc[:, d_:BLK],
                                                               op0=ALU.mult, op1=ALU.add), r=[raw, ssd_cw, acc], w=[acc])
                    k.V(lambda: nc.vector.scalar_tensor_tensor(out=acc[:, 0:d_], in0=ssd_carry[:, c, 3 - d_:3],
                                                               scalar=ssd_cw[:, c, 3 - d_:4 - d_], in1=acc[:, 0:d_],
                                                               op0=ALU.mult, op1=ALU.add),
                        r=[ssd_carry, ssd_cw, acc], w=[acc])
                k.V(lambda: nc.vector.tensor_copy(out=ssd_carry[:, c, 0:3], in_=raw[:, BLK - 3:BLK]), r=[raw], w=[ssd_carry])
                k.A(lambda: nc.scalar.activation(out=acc[:, :], in_=acc[:, :], func=AF.Silu, bias=ssd_cb[:, c:c + 1],
                                                 scale=1.0), r=[acc, ssd_cb], w=[acc])
            proj_fm(l, C_SSD_XBC + 128 * c, 128, ev)
        proj_tok(l, C_SSD_Z, 512, lambda j, g0, w_, pa: k.A(
            lambda: nc.scalar.activation(out=zt[:, j, g0:g0 + w_], in_=pa[:, 0:w_], func=AF.Silu), r=[pa], w=[zt]))

        def ev_dt(j, g0, w_, pa):
            k.V(lambda: nc.vector.tensor_tensor(out=dtt[:, j, 0:8], in0=pa[:, 0:8], in1=ssd_sm[:, 0:8], op=ALU.add),
                r=[pa, ssd_sm], w=[dtt])
            softplus_ip(dtt[:, j, 0:8], dtt, 8)
        proj_tok(l, C_SSD_DT, 8, ev_dt)
        for j in range(NJ):
            xs = obr.next()
            for c in range(4):
                tr(xs[:, c * 128:(c + 1) * 128], xs, cv[c][:, j * T:(j + 1) * T], cv[c], 128, 128,
                   eng=("act" if c % 2 == 0 else "dve"))
            btok = [sq.next(), sq.next()]
            for g in range(2):
                tr(btok[g][:, :], btok[g], cv[4 + g][:, j * T:(j + 1) * T], cv[4 + g], 128, 128)
            st = stt.next()
            k.V(lambda: nc.vector.tensor_tensor(out=st[:, 0:8], in0=dtt[:, j, 0:8], in1=ssd_sm[:, 8:16], op=ALU.mult),
                r=[dtt, ssd_sm], w=[st])
            pq = psr.next()
            k.mm(pq[:, 0:8], C("maskT"), st[:, 0:8], r=[cst, st], w=[pq])
            k.mm(pq[:, 8:16], C("maskL"), st[:, 0:8], r=[cst, st], w=[pq])
            k.mm(pq[:, 16:24], C("ones"), st[:, 0:8], r=[cst, st], w=[pq])
            k.A(lambda: nc.scalar.activation(out=st[:, 8:32], in_=pq[:, 0:24], func=AF.Exp), r=[pq], w=[st])
            rhsM = [sq2.next() for _ in range(4)]
            for h in range(8):
                k.V(lambda: nc.vector.tensor_scalar(out=rhsM[h // 2][:, (h % 2) * T:(h % 2 + 1) * T], in0=C("maskT"),
                                                    scalar1=st[:, h:h + 1], scalar2=None, op0=ALU.mult),
                    r=[cst, st], w=[rhsM[h // 2]])
            v1 = sq2.next(), sq2.next()
            st2 = stt.next()
            k.V(lambda: nc.vector.tensor_tensor(out=st2[:, 0:8], in0=dtt[:, j, 0:8], in1=st[:, 16:24], op=ALU.mult),
                r=[dtt, st], w=[st2])
            for g in range(2):
                k.V(lambda: nc.vector.tensor_tensor(
                    out=v1[g][:, :].rearrange("p (h d) -> p h d", h=4),
                    in0=xs[:, g * 256:(g + 1) * 256].rearrange("p (h d) -> p h d", h=4),
                    in1=dtt[:, j, g * 4:(g + 1) * 4].unsqueeze(2).to_broadcast([128, 4, 64]), op=ALU.mult),
                    r=[xs, dtt], w=[v1[g]])
            k.V(lambda: nc.vector.tensor_tensor(
                out=junk[:, 0:512].rearrange("p (h d) -> p h d", h=8),
                in0=xs[:, :].rearrange("p (h d) -> p h d", h=8),
                in1=st2[:, 0:8].unsqueeze(2).to_broadcast([128, 8, 64]), op=ALU.mult), r=[xs, st2], w=[junk])
            pyi = pacc[0]
            pye = pacc[1]
            for g in range(2):
                bT = cv[4 + g][:, j * T:(j + 1) * T]
                cT = cv[6 + g][:, j * T:(j + 1) * T]
                psc = psr.next()
                k.mm(psc[:, 0:T], bT, cT, r=[cv[4 + g], cv[6 + g]], w=[psc])
                scg = sq.next()
                k.A(lambda: nc.scalar.copy(out=scg[:, :], in_=psc[:, 0:T]), r=[psc], w=[scg])
                for hh in range(4):
                    h = g * 4 + hh
                    pd = psr.next()
                    k.mm(pd[:, 0:T], C("maskL"), rhsM[h // 2][:, (h % 2) * T:(h % 2 + 1) * T], start=True, stop=False,
                         r=[cst, rhsM[h // 2]], w=[pd])
                    k.mm(pd[:, 0:T], ident, C("negm"), start=False, stop=True, r=[cst], w=[pd])
                    ex = sq.next()
                    k.A(lambda: nc.scalar.activation(out=ex[:, :], in_=pd[:, 0:T], func=AF.Exp), r=[pd], w=[ex])
                    k.V(lambda: nc.vector.tensor_tensor(out=ex[:, :], in0=ex[:, :], in1=scg[:, :], op=ALU.mult),
                        r=[ex, scg], w=[ex])
                    k.mm(pyi[:, h * 64:(h + 1) * 64], ex[:, :], v1[g][:, hh * 64:(hh + 1) * 64], r=[ex, v1[g]], w=[pyi])
                k.mm(pye[:, g * 256:(g + 1) * 256], cT, ssd_S[:, g * 256:(g + 1) * 256], r=[cv[6 + g], ssd_S], w=[pye])
                pds = psr.next()
                k.mm(pds[:, 0:256], btok[g][:, :], junk[:, g * 256:(g + 1) * 256], r=[btok[g], junk], w=[pds])
                sg = ssd_S[:, g * 256:(g + 1) * 256].rearrange("p (h d) -> p h d", h=4)
                k.V(lambda: nc.vector.tensor_tensor(out=sg, in0=sg,
                                                    in1=st[:, 24 + g * 4:28 + g * 4].unsqueeze(2).to_broadcast([128, 4, 64]),
                                                    op=ALU.mult), r=[ssd_S, st], w=[ssd_S])
                k.V(lambda: nc.vector.tensor_tensor(out=ssd_S[:, g * 256:(g + 1) * 256], in0=ssd_S[:, g * 256:(g + 1) * 256],
                                                    in1=pds[:, 0:256], op=ALU.add), r=[ssd_S, pds], w=[ssd_S])
            o = obr.next()
            k.A(lambda: nc.scalar.copy(out=o[:, :], in_=pyi[:, :]), r=[pyi], w=[o])
            y3 = o[:, :].rearrange("p (h d) -> p h d", h=8)
            tmp = junk[:, 512:1024]
            k.V(lambda: nc.vector.tensor_tensor(out=tmp.rearrange("p (h d) -> p h d", h=8),
                                                in0=pye[:, :].rearrange("p (h d) -> p h d", h=8),
                                                in1=st[:, 8:16].unsqueeze(2).to_broadcast([128, 8, 64]), op=ALU.mult),
                r=[pye, st], w=[junk])
            k.V(lambda: nc.vector.tensor_tensor(out=o[:, :], in0=o[:, :], in1=tmp, op=ALU.add), r=[o, junk], w=[o])
            k.V(lambda: nc.vector.tensor_tensor(out=tmp.rearrange("p (h d) -> p h d", h=8),
                                                in0=xs[:, :].rearrange("p (h d) -> p h d", h=8),
                                                in1=ssd_sm[:, 16:24].unsqueeze(2).to_broadcast([128, 8, 64]), op=ALU.mult),
                r=[xs, ssd_sm], w=[junk])
            k.V(lambda: nc.vector.tensor_tensor(out=o[:, :], in0=o[:, :], in1=tmp, op=ALU.add), r=[o, junk], w=[o])
            k.V(lambda: nc.vector.tensor_tensor(out=o[:, :], in0=o[:, :], in1=zt[:, j, :], op=ALU.mult), r=[o, zt], w=[o])
            st3 = stt.next()
            rstd_groups(o, lambda g: o[:, g * 256:(g + 1) * 256], 2, 256, 1e-6, st3, 0)
            for g in range(2):
                k.V(lambda: nc.vector.scalar_tensor_tensor(out=o[:, g * 256:(g + 1) * 256], in0=o[:, g * 256:(g + 1) * 256],
                                                           scalar=st3[:, 4 + g:5 + g], in1=ssd_ng[:, g * 256:(g + 1) * 256],
                                                           op0=ALU.mult, op1=ALU.mult), r=[o, st3, ssd_ng], w=[o])
            emit_out(2, l, blk, j, o, 512)

    def mem_block(l, blk):
        qT = FM[0:4]
        for h in range(4):
            proj_fm(l, C_MEM_Q + 64 * h, 64, lambda pa, h=h: k.A(
                lambda: nc.scalar.activation(out=qT[h][0:64, :], in_=pa[0:64, 0:BLK], func=AF.Copy, scale=0.125),
                r=[pa], w=[qT[h]]))
        for j in range(NJ):
            st = stt.next()
            po = pacc[j % 2]
            o = obr.next()
            for h in range(4):
                psc = psr.next()
                k.mm(psc[:, 0:256], qT[h][0:64, j * T:(j + 1) * T], kmT[:, h, :], r=[qT[h], kmT], w=[psc])
                k.V(lambda: nc.vector.tensor_reduce(out=st[:, h:h + 1], in_=psc[:, 0:256], axis=AX.X, op=ALU.max),
                    r=[psc], w=[st])
                k.V(lambda: nc.vector.tensor_scalar(out=st[:, 4 + h:5 + h], in0=st[:, h:h + 1], scalar1=-1.0, scalar2=None,
                                                    op0=ALU.mult), r=[st], w=[st])
                pe_ = sq2.next()
                k.A(lambda: nc.scalar.activation(out=pe_[:, :], in_=psc[:, 0:256], func=AF.Exp, bias=st[:, 4 + h:5 + h],
                                                 scale=1.0, accum_out=st[:, 8 + h:9 + h]), r=[psc, st], w=[pe_, st])
                pT = [sq.next(), sq.next()]
                for mt in range(2):
                    tr(pT[mt][:, :], pT[mt], pe_[:, mt * T:(mt + 1) * T], pe_, 128, 128, eng=("act" if mt == 0 else "dve"))
                for mt in range(2):
                    k.mm(po[:, h * 64:(h + 1) * 64], pT[mt][:, :], vm[:, mt, h * 64:(h + 1) * 64], start=(mt == 0),
                         stop=(mt == 1), r=[pT[mt], vm], w=[po])
            k.V(lambda: nc.vector.reciprocal(out=st[:, 12:16], in_=st[:, 8:12]), r=[st], w=[st])
            k.V(lambda: nc.vector.tensor_tensor(out=o[:, 0:256].rearrange("p (h d) -> p h d", h=4),
                                                in0=po[:, 0:256].rearrange("p (h d) -> p h d", h=4),
                                                in1=st[:, 12:16].unsqueeze(2).to_broadcast([128, 4, 64]), op=ALU.mult),
                r=[po, st], w=[o])
            emit_out(4, l, blk, j, o, 256)

    rwt = [k.sb([128, 128], name="rwt") for _ in range(26)]
    rw_AR = k.sb([128, 256], name="rw_AR")
    rw_bon = k.sb([128, NJ, 8], name="rw_bon")

    def rwkv_block(l, blk):
        xr, xk, xv, ldt, at, kkt, tmpt, k2t, raw, bvt, prod = FM[0], FM[1], FM[2], FM[3], FM[4], FM[5], FM[6], FM[7], FM[8], FM[9], FM[10]
        waT = FM[12]
        gt, ytok, vtok = TK[0], TK[2], TK[3]

        def shift_ev(dst, cidx):
            def ev(pa):
                k.A(lambda: nc.scalar.copy(out=raw[:, :], in_=pa[:, 0:BLK]), r=[pa], w=[raw])
                k.V(lambda: nc.vector.tensor_scalar(out=dst[:, :], in0=raw[:, :], scalar1=rw_cols[:, 40 + cidx:41 + cidx],
                                                    scalar2=None, op0=ALU.mult), r=[raw, rw_cols], w=[dst])
                k.V(lambda: nc.vector.scalar_tensor_tensor(out=dst[:, 1:BLK], in0=raw[:, 0:BLK - 1],
                                                           scalar=rw_cols[:, cidx:cidx + 1], in1=dst[:, 1:BLK],
                                                           op0=ALU.mult, op1=ALU.add), r=[raw, rw_cols, dst], w=[dst])
                k.V(lambda: nc.vector.scalar_tensor_tensor(out=dst[:, 0:1], in0=rw_carry[:, cidx:cidx + 1],
                                                           scalar=rw_cols[:, cidx:cidx + 1], in1=dst[:, 0:1],
                                                           op0=ALU.mult, op1=ALU.add), r=[rw_carry, rw_cols, dst], w=[dst])
                k.V(lambda: nc.vector.tensor_copy(out=rw_carry[:, cidx:cidx + 1], in_=raw[:, BLK - 1:BLK]),
                    r=[raw], w=[rw_carry])
            return ev

        proj_tok(l, C_RWKV_G, 512, lambda j, g0, w_, pa: k.A(
            lambda: nc.scalar.activation(out=gt[:, j, g0:g0 + w_], in_=pa[:, 0:w_], func=AF.Silu), r=[pa], w=[gt]))
        proj_fm(l, C_RWKV_IN + 12 * 128, 128, shift_ev(waT, 12))
        k.A(lambda: nc.scalar.activation(out=waT[0:64, :], in_=waT[0:64, :], func=AF.Tanh), r=[waT], w=[waT])
        for p in range(4):
            proj_fm(l, C_RWKV_IN + p * 128, 128, shift_ev(xr, p))
            proj_fm(l, C_RWKV_IN + (4 + p) * 128, 128, shift_ev(xk, 4 + p))
            proj_fm(l, C_RWKV_IN + (8 + p) * 128, 128, shift_ev(xv, 8 + p))
            pz = psr.next()
            k.mm(pz[:, 0:BLK], rw_w2a2[0:64, p * 128:(p + 1) * 128], waT[0:64, :], r=[rw_w2a2, waT], w=[pz])
            k.A(lambda: nc.scalar.activation(out=ldt[:, :], in_=pz[:, 0:BLK], func=AF.Sigmoid, bias=rw_cols[:, 16 + p:17 + p],
                                             scale=1.0), r=[pz, rw_cols], w=[ldt])
            k.V(lambda: nc.vector.tensor_scalar(out=ldt[:, :], in0=ldt[:, :], scalar1=-math.exp(-0.5), scalar2=None,
                                                op0=ALU.mult), r=[ldt], w=[ldt])
            pa_ = psr.next()
            k.mm(pa_[:, 0:BLK], rw_w2a2[64:128, p * 128:(p + 1) * 128], waT[64:128, :], r=[rw_w2a2, waT], w=[pa_])
            k.A(lambda: nc.scalar.activation(out=at[:, :], in_=pa_[:, 0:BLK], func=AF.Sigmoid, bias=rw_cols[:, 20 + p:21 + p],
                                             scale=1.0), r=[pa_, rw_cols], w=[at])
            k.V(lambda: nc.vector.tensor_scalar(out=kkt[:, :], in0=xk[:, :], scalar1=rw_cols[:, 24 + p:25 + p], scalar2=None,
                                                op0=ALU.mult), r=[xk, rw_cols], w=[kkt])
            k.G(lambda: nc.gpsimd.tensor_tensor(out=tmpt[:, :], in0=kkt[:, :], in1=kkt[:, :], op=ALU.mult), r=[kkt], w=[tmpt])
            pn = psr.next()
            k.mm(pn[:, 0:BLK], C("blk64"), tmpt[:, :], r=[cst, tmpt], w=[pn])
            k.A(lambda: nc.scalar.sqrt(out=tmpt[:, :], in_=pn[:, 0:BLK]), r=[pn], w=[tmpt])
            k.V(lambda: nc.vector.tensor_scalar(out=tmpt[:, :], in0=tmpt[:, :], scalar1=1e-12, scalar2=None, op0=ALU.max),
                r=[tmpt], w=[tmpt])
            k.V(lambda: nc.vector.reciprocal(out=tmpt[:, :], in_=tmpt[:, :]), r=[tmpt], w=[tmpt])
            k.V(lambda: nc.vector.tensor_tensor(out=kkt[:, :], in0=kkt[:, :], in1=tmpt[:, :], op=ALU.mult), r=[kkt, tmpt], w=[kkt])
            k.V(lambda: nc.vector.tensor_scalar(out=tmpt[:, :], in0=at[:, :], scalar1=rw_cols[:, 28 + p:29 + p], scalar2=-1.0,
                                                op0=ALU.mult, op1=ALU.mult), r=[at, rw_cols], w=[tmpt])
            k.V(lambda: nc.vector.tensor_scalar(out=tmpt[:, :], in0=tmpt[:, :], scalar1=rw_cols[:, 28 + p:29 + p], scalar2=-1.0,
                                                op0=ALU.add, op1=ALU.add), r=[tmpt, rw_cols], w=[tmpt])
            k.V(lambda: nc.vector.scalar_tensor_tensor(out=k2t[:, :], in0=tmpt[:, :], scalar=-1.0, in1=xk[:, :],
                                                       op0=ALU.mult, op1=ALU.mult), r=[tmpt, xk], w=[k2t])
            k.G(lambda: nc.gpsimd.tensor_tensor(out=bvt[:, :], in0=kkt[:, :], in1=at[:, :], op=ALU.mult), r=[kkt, at], w=[bvt])
            k.V(lambda: nc.vector.scalar_tensor_tensor(out=prod[:, :], in0=xr[:, :], scalar=rw_cols[:, 32 + p:33 + p],
                                                       in1=k2t[:, :], op0=ALU.mult, op1=ALU.mult),
                r=[xr, rw_cols, k2t], w=[prod])
            for j in range(NJ):
                js = slice(j * T, (j + 1) * T)
                (cum, epos, eneg, eposx, Bt, Kt, Btok, Ktok, Vt, S0p) = rwt[0:10]
                pb = psr.next()
                k.mm(pb[:, 0:2], prod[:, js], C("headsel"), r=[prod, cst], w=[pb])
                k.A(lambda: nc.scalar.copy(out=rw_bon[:, j, 2 * p:2 * p + 2], in_=pb[:, 0:2]), r=[pb], w=[rw_bon])
                k.V(lambda: nc.vector.tensor_tensor_scan(out=cum[:, :], data0=C("ones"), data1=ldt[:, js], initial=0.0,
                                                         op0=ALU.mult, op1=ALU.add), r=[cst, ldt], w=[cum])
                st = stt.next()
                k.V(lambda: nc.vector.tensor_copy(out=st[:, 0:1], in_=cum[:, 63:64]), r=[cum], w=[st])
                k.V(lambda: nc.vector.tensor_scalar(out=st[:, 1:2], in0=cum[:, 63:64], scalar1=-1.0, scalar2=None,
                                                    op0=ALU.mult), r=[cum], w=[st])
                k.A(lambda: nc.scalar.activation(out=epos[:, :], in_=cum[:, :], func=AF.Exp, bias=st[:, 1:2], scale=1.0),
                    r=[cum, st], w=[epos])
                k.A(lambda: nc.scalar.activation(out=eneg[:, :], in_=cum[:, :], func=AF.Exp, bias=st[:, 0:1], scale=-1.0),
                    r=[cum, st], w=[eneg])
                k.V(lambda: nc.vector.tensor_tensor(out=eposx[:, :], in0=cum[:, :], in1=ldt[:, js], op=ALU.subtract),
                    r=[cum, ldt], w=[eposx])
                k.A(lambda: nc.scalar.activation(out=eposx[:, :], in_=eposx[:, :], func=AF.Exp, bias=st[:, 1:2], scale=1.0),
                    r=[eposx, st], w=[eposx])
                k.A(lambda: nc.scalar.activation(out=st[:, 2:3], in_=st[:, 0:1], func=AF.Exp), r=[st], w=[st])
                k.A(lambda: nc.scalar.activation(out=st[:, 3:4], in_=cum[:, T - 1:T], func=AF.Exp, bias=st[:, 1:2], scale=1.0),
                    r=[cum, st], w=[st])
                k.V(lambda: nc.vector.scalar_tensor_tensor(out=rw_AR[:, 0:T], in0=kkt[:, js], scalar=-1.0, in1=eposx[:, :],
                                                           op0=ALU.mult, op1=ALU.mult), r=[kkt, eposx], w=[rw_AR])
                k.V(lambda: nc.vector.tensor_tensor(out=rw_AR[:, T:2 * T], in0=xr[:, js], in1=epos[:, :], op=ALU.mult),
                    r=[xr, epos], w=[rw_AR])
                k.G(lambda: nc.gpsimd.tensor_tensor(out=Bt[:, :], in0=bvt[:, js], in1=eneg[:, :], op=ALU.mult),
                    r=[bvt, eneg], w=[Bt])
                k.G(lambda: nc.gpsimd.tensor_tensor(out=Kt[:, :], in0=k2t[:, js], in1=eneg[:, :], op=ALU.mult),
                    r=[k2t, eneg], w=[Kt])
                k.V(lambda: nc.vector.tensor_scalar(out=S0p[:, 0:64], in0=rw_S[p][:, :], scalar1=st[:, 2:3], scalar2=None,
                                                    op0=ALU.mult), r=[rw_S[p], st], w=[S0p])
                tr(Btok[:, :], Btok, Bt[:, :], Bt, 128, 128, eng="act")
                tr(Ktok[:, :], Ktok, Kt[:, :], Kt, 128, 128, eng="dve")
                tr(Vt[:, :], Vt, xv[:, js], xv, 128, 128, eng="act")
                k.G(lambda: nc.gpsimd.tensor_copy(out=vtok[:, j, p * 128:(p + 1) * 128], in_=Vt[:, :]), r=[Vt], w=[vtok])
                for hh in range(2):
                    r0 = 64 * hh
                    rs = slice(r0, r0 + 64)
                    (X0, XT0, X1, XT1, Z0, Z1, Mbr, Nak, Mkr) = rwt[10 + 8 * hh:10 + 8 * hh + 8] + [rwt[10 + 16 + hh]] \
                        if False else (rwt[10 + 8 * hh], rwt[11 + 8 * hh], rwt[12 + 8 * hh], rwt[13 + 8 * hh],
                                       rwt[14 + 8 * hh], rwt[15 + 8 * hh], rwt[16 + 8 * hh], rwt[17 + 8 * hh], sq.next())
                    pN = psr.next()
                    k.mm(pN[:, 0:2 * T], Bt[rs, :], rw_AR[rs, :], r=[Bt, rw_AR], w=[pN])
                    pK = psr.next()
                    k.mm(pK[:, 0:2 * T], Kt[rs, :], rw_AR[rs, :], r=[Kt, rw_AR], w=[pK])
                    pX = psr.next()
                    k.mm(pX[:, 0:T], rw_AR[rs, 0:T], Bt[rs, :], r=[Bt, rw_AR], w=[pX])
                    k.V(lambda: nc.vector.tensor_tensor(out=X0[:, :], in0=pN[:, 0:T], in1=C("maskS"), op=ALU.mult),
                        r=[pN, cst], w=[X0])
                    k.V(lambda: nc.vector.tensor_tensor(out=Mbr[:, :], in0=pN[:, T:2 * T], in1=C("maskT"), op=ALU.mult),
                        r=[pN, cst], w=[Mbr])
                    k.V(lambda: nc.vector.tensor_tensor(out=Nak[:, :], in0=pK[:, 0:T], in1=C("maskS"), op=ALU.mult),
                        r=[pK, cst], w=[Nak])
                    k.V(lambda: nc.vector.tensor_tensor(out=Mkr[:, :], in0=pK[:, T:2 * T], in1=C("maskT"), op=ALU.mult),
                        r=[pK, cst], w=[Mkr])
                    k.V(lambda: nc.vector.tensor_tensor(out=XT0[:, :], in0=pX[:, 0:T], in1=C("maskL"), op=ALU.mult),
                        r=[pX, cst], w=[XT0])
                    pW = psr.next()
                    k.mm(pW[:, 0:64], rw_AR[rs, 0:T], S0p[rs, 0:64], start=True, stop=False, r=[rw_AR, S0p], w=[pW])
                    k.mm(pW[:, 0:64], Nak[:, :], Vt[:, rs], start=False, stop=True, r=[Nak, Vt], w=[pW])
                    k.A(lambda: nc.scalar.copy(out=Z0[:, 0:64], in_=pW[:, 0:64]), r=[pW], w=[Z0])
                    X, XT, Xn, XTn, Z, Zn = X0, XT0, X1, XT1, Z0, Z1
                    for lev in range(7):
                        pZ = psr.next()
                        k.mm(pZ[:, 0:64], X[:, :], Z[:, 0:64], r=[X, Z], w=[pZ])
                        k.V(lambda: nc.vector.tensor_tensor(out=Zn[:, 0:64], in0=Z[:, 0:64], in1=pZ[:, 0:64], op=ALU.add),
                            r=[Z, pZ], w=[Zn])
                        Z, Zn = Zn, Z
                        if lev < 6:
                            p1 = psr.next()
                            k.mm(p1[:, 0:T], XT[:, :], X[:, :], r=[XT, X], w=[p1])
                            p2 = psr.next()
                            k.mm(p2[:, 0:T], X[:, :], XT[:, :], r=[XT, X], w=[p2])
                            k.A(lambda: nc.scalar.copy(out=Xn[:, :], in_=p1[:, 0:T]), r=[p1], w=[Xn])
                            k.V(lambda: nc.vector.tensor_copy(out=XTn[:, :], in_=p2[:, 0:T]), r=[p2], w=[XTn])
                            X, Xn = Xn, X
                            XT, XTn = XTn, XT
                    U = Z
                    pY = psr.next()
                    k.mm(pY[:, 0:64], rw_AR[rs, T:2 * T], S0p[rs, 0:64], start=True, stop=False, r=[rw_AR, S0p], w=[pY])
                    k.mm(pY[:, 0:64], Mbr[:, :], U[:, 0:64], start=False, stop=False, r=[Mbr, U], w=[pY])
                    k.mm(pY[:, 0:64], Mkr[:, :], Vt[:, rs], start=False, stop=True, r=[Mkr, Vt], w=[pY])
                    hcol = (2 * p + hh) * 64
                    k.A(lambda: nc.scalar.copy(out=ytok[:, j, hcol:hcol + 64], in_=pY[:, 0:64]), r=[pY], w=[ytok])
                    pS = psr.next()
                    k.mm(pS[:, 0:64], Btok[:, :], U[:, 0:64], start=True, stop=False, r=[Btok, U], w=[pS])
                    k.mm(pS[:, 0:64], Ktok[:, :], Vt[:, rs], start=False, stop=True, r=[Ktok, Vt], w=[pS])
                    k.V(lambda: nc.vector.tensor_tensor(out=S0p[rs, 64:128], in0=S0p[rs, 0:64], in1=pS[rs, 0:64], op=ALU.add),
                        r=[S0p, pS], w=[S0p])
                    k.V(lambda: nc.vector.tensor_scalar(out=rw_S[p][rs, :], in0=S0p[rs, 64:128], scalar1=st[rs, 3:4],
                                                        scalar2=None, op0=ALU.mult), r=[S0p, st], w=[rw_S[p]])
        for j in range(NJ):
            st = stt.next()
            y3 = ytok[:, j, :].rearrange("p (h d) -> p h d", h=8)
            k.V(lambda: nc.vector.tensor_reduce(out=st[:, 0:8], in_=y3, axis=AX.X, op=ALU.add), r=[ytok], w=[st])
            k.A(lambda: nc.scalar.activation(out=junk[:, 0:512], in_=ytok[:, j, :], func=AF.Square), r=[ytok], w=[junk])
            k.V(lambda: nc.vector.tensor_reduce(out=st[:, 8:16], in_=junk[:, 0:512].rearrange("p (h d) -> p h d", h=8),
                                                axis=AX.X, op=ALU.add), r=[junk], w=[st])
            k.V(lambda: nc.vector.tensor_scalar(out=st[:, 0:8], in0=st[:, 0:8], scalar1=1.0 / 64, scalar2=None, op0=ALU.mult),
                r=[st], w=[st])
            k.V(lambda: nc.vector.tensor_tensor(out=st[:, 16:24], in0=st[:, 0:8], in1=st[:, 0:8], op=ALU.mult), r=[st], w=[st])
            k.V(lambda: nc.vector.scalar_tensor_tensor(out=st[:, 8:16], in0=st[:, 8:16], scalar=1.0 / 64, in1=st[:, 16:24],
                                                       op0=ALU.mult, op1=ALU.subtract), r=[st], w=[st])
            k.V(lambda: nc.vector.tensor_scalar(out=st[:, 8:16], in0=st[:, 8:16], scalar1=64e-5, scalar2=None, op0=ALU.add),
                r=[st], w=[st])
            k.A(lambda: nc.scalar.sqrt(out=st[:, 8:16], in_=st[:, 8:16]), r=[st], w=[st])
            k.V(lambda: nc.vector.reciprocal(out=st[:, 24:32], in_=st[:, 8:16]), r=[st], w=[st])
            o = obr.next()
            o3 = o[:, :].rearrange("p (h d) -> p h d", h=8)
            k.V(lambda: nc.vector.tensor_tensor(out=o3, in0=y3, in1=st[:, 0:8].unsqueeze(2).to_broadcast([128, 8, 64]),
                                                op=ALU.subtract), r=[ytok, st], w=[o])
            k.V(lambda: nc.vector.tensor_tensor(out=o3, in0=o3, in1=st[:, 24:32].unsqueeze(2).to_broadcast([128, 8, 64]),
                                                op=ALU.mult), r=[o, st], w=[o])
            k.G(lambda: nc.gpsimd.tensor_tensor(out=o[:, :], in0=o[:, :], in1=rw_lng[:, :], op=ALU.mult), r=[o, rw_lng], w=[o])
            k.G(lambda: nc.gpsimd.tensor_tensor(out=o[:, :], in0=o[:, :], in1=rw_lnb[:, :], op=ALU.add), r=[o, rw_lnb], w=[o])
            k.V(lambda: nc.vector.tensor_tensor(out=junk[:, 0:512].rearrange("p (h d) -> p h d", h=8),
                                                in0=vtok[:, j, :].rearrange("p (h d) -> p h d", h=8),
                                                in1=rw_bon[:, j, :].unsqueeze(2).to_broadcast([128, 8, 64]), op=ALU.mult),
                r=[vtok, rw_bon], w=[junk])
            k.V(lambda: nc.vector.tensor_tensor(out=o[:, :], in0=o[:, :], in1=junk[:, 0:512], op=ALU.add), r=[o, junk], w=[o])
            k.V(lambda: nc.vector.tensor_tensor(out=o[:, :], in0=o[:, :], in1=gt[:, j, :], op=ALU.mult), r=[o, gt], w=[o])
            emit_out(3, l, blk, j, o, 512)

    gsb = Ring([k.sb([128, 512], name="gsb") for _ in range(2)])
    fng = k.sb([128, D], name="fng")
    UP = ["w_up_ret", "w_up_gla", "w_up_ssd", "w_up_rwkv", "w_up_mem"]

    def merge_block(l, blk, last):
        t0 = blk * BLK
        mh = [TK[0], TK[1]]
        first = True
        for bi in range(5):
            if BR[bi] not in branches:
                continue
            nr = 2 if bi == 4 else 4
            for hf in range(2):
                wg = load_w(dr["w_in"][l], C_GATES + bi * 1024 + hf * 512, 512)
                wu = load_w(dr[UP[bi]][l], hf * 512, 512, rows=nr)
                for j in range(NJ):
                    pg = psr.next()
                    for dc in range(8):
                        k.mm(pg[:, :], hT[:, dc, j * T:(j + 1) * T], wg[:, dc, :], start=(dc == 0), stop=(dc == 7),
                             r=[hT, wg], w=[pg])
                    gs = gsb.next()
                    k.A(lambda: nc.scalar.activation(out=gs[:, :], in_=pg[:, :], func=AF.Sigmoid), r=[pg], w=[gs])
                    pu = psr.next()
                    for c in range(nr):
                        k.mm(pu[:, :], oT[bi][:, c, j * T:(j + 1) * T], wu[:, c, :], start=(c == 0), stop=(c == nr - 1),
                             r=[oT[bi], wu], w=[pu])
                    if first:
                        k.V(lambda: nc.vector.tensor_tensor(out=mh[hf][:, j, :], in0=gs[:, :], in1=pu[:, :], op=ALU.mult),
                            r=[gs, pu], w=[mh[hf]])
                    else:
                        k.V(lambda: nc.vector.tensor_tensor(out=gs[:, :], in0=gs[:, :], in1=pu[:, :], op=ALU.mult),
                            r=[gs, pu], w=[gs])
                        k.G(lambda: nc.gpsimd.tensor_tensor(out=mh[hf][:, j, :], in0=mh[hf][:, j, :], in1=gs[:, :], op=ALU.add),
                            r=[gs, mh[hf]], w=[mh[hf]])
            first = False
        if debug is not None and l == dbg_layer:
            for j in range(NJ):
                for hf in range(2):
                    k.dma("pool", dbg_d[t0 + j * T:t0 + (j + 1) * T, 2304 + hf * 512:2304 + (hf + 1) * 512], mh[hf][:, j, :],
                          r=[mh[hf]])
        for j in range(NJ):
            for hf in range(2):
                for c in range(4):
                    tr(hT[:, hf * 4 + c, j * T:(j + 1) * T], hT, mh[hf][:, j, c * 128:(c + 1) * 128], mh[hf], 128, 128,
                       eng=("act" if c % 2 == 0 else "dve"))
        for hf in range(2):
            wo = load_w(dr["w_out"][l], hf * 512, 512)
            for j in range(NJ):
                po = psr.next()
                for dc in range(8):
                    k.mm(po[:, :], hT[:, dc, j * T:(j + 1) * T], wo[:, dc, :], start=(dc == 0), stop=(dc == 7),
                         r=[hT, wo], w=[po])
                k.V(lambda: nc.vector.tensor_tensor(out=xb[:, j, hf * 512:(hf + 1) * 512], in0=xb[:, j, hf * 512:(hf + 1) * 512],
                                                    in1=po[:, :], op=ALU.add), r=[xb, po], w=[xb])
        if debug is not None and l == dbg_layer:
            for j in range(NJ):
                k.dma("pool", dbg_d[t0 + j * T:t0 + (j + 1) * T, 3328:4352], xb[:, j, :], r=[xb])
        if not last:
            k.dma("pool", x1_d[t0:t0 + BLK, :].rearrange("(j p) d -> p j d", p=128), xb[:, :, :], r=[xb], w=[x1_b[blk]])
        else:
            for j in range(NJ):
                st = stt.next()
                k.A(lambda: nc.scalar.activation(out=junk[:, :], in_=xb[:, j, :], func=AF.Square, accum_out=st[:, 0:1]),
                    r=[xb], w=[junk, st])
                k.V(lambda: nc.vector.tensor_scalar(out=st[:, 1:2], in0=st[:, 0:1], scalar1=1.0 / D, scalar2=1e-6,
                                                    op0=ALU.mult, op1=ALU.add), r=[st], w=[st])
                k.A(lambda: nc.scalar.sqrt(out=st[:, 1:2], in_=st[:, 1:2]), r=[st], w=[st])
                k.V(lambda: nc.vector.reciprocal(out=st[:, 2:3], in_=st[:, 1:2]), r=[st], w=[st])
                k.V(lambda: nc.vector.scalar_tensor_tensor(out=xb[:, j, :], in0=xb[:, j, :], scalar=st[:, 2:3], in1=fng[:, :],
                                                           op0=ALU.mult, op1=ALU.mult), r=[xb, st, fng], w=[xb])
            k.dma("pool", out_d[t0:t0 + BLK, :].rearrange("(j p) d -> p j d", p=128), xb[:, :, :], r=[xb])

    BR = ["ret", "gla", "ssd", "rwkv", "mem"]
    k.dma("sp", fng[:, :], bc(dr["final_norm_g"][0:1, :], D), w=[fng])
    for l in range(nlayers):
        load_params(l)
        for blk in range(nblk):
            t0 = blk * BLK
            src = dr["x"] if l == 0 else x1_d
            rb = [] if l == 0 else [x1_b[blk]]
            k.dma("sp", xb[:, :, :], src[t0:t0 + BLK, :].rearrange("(j p) d -> p j d", p=128), r=rb, w=[xb])
            st = ss
            for j in range(NJ):
                k.A(lambda: nc.scalar.activation(out=junk[:, :], in_=xb[:, j, :], func=AF.Square,
                                                 accum_out=st[:, j:j + 1]), r=[xb], w=[junk, st])
            k.V(lambda: nc.vector.tensor_scalar(out=st[:, 4:4 + NJ], in0=st[:, 0:NJ], scalar1=1.0 / D, scalar2=1e-6,
                                                op0=ALU.mult, op1=ALU.add), r=[st], w=[st])
            k.A(lambda: nc.scalar.sqrt(out=st[:, 4:4 + NJ], in_=st[:, 4:4 + NJ]), r=[st], w=[st])
            k.V(lambda: nc.vector.reciprocal(out=st[:, 8:8 + NJ], in_=st[:, 4:4 + NJ]), r=[st], w=[st])
            for j in range(NJ):
                k.V(lambda: nc.vector.tensor_scalar(out=xn[:, :], in0=xb[:, j, :], scalar1=st[:, 8 + j:9 + j],
                                                    scalar2=None, op0=ALU.mult), r=[xb, st], w=[xn])
                for half in range(2):
                    pa = psr.next()
                    for q in range(4):
                        dc = half * 4 + q
                        k.op("pe", lambda: nc.tensor.transpose(pa[:, q * T:(q + 1) * T], xn[:, dc * T:(dc + 1) * T], ident),
                             r=[xn, cst], w=[pa])
                    for q in range(4):
                        dc = half * 4 + q
                        k.A(lambda: nc.scalar.activation(out=hT[:, dc, j * T:(j + 1) * T], in_=pa[:, q * T:(q + 1) * T],
                                                         func=AF.Identity, scale=gcol[:, dc:dc + 1]), r=[pa, gcol], w=[hT])
            k.dma("sp", posi[:, :], dr["positions"][:, t0:t0 + BLK], w=[posi])
            k.V(lambda: nc.vector.tensor_copy(out=posf[:, :], in_=posi[:, :]), r=[posi], w=[posf])
            pa = psr.next()
            k.mm(pa[0:64, 0:BLK], C("invrow", 0, 1), posf[0:1, :], r=[cst, posf], w=[pa])
            for tab, shift in ((sinT, math.pi), (cosT, 1.5 * math.pi)):
                k.V(lambda: nc.vector.tensor_scalar(out=tab[:, :], in0=pa[0:64, 0:BLK], scalar1=shift, scalar2=None,
                                                    op0=ALU.add), r=[pa], w=[tab])
                k.V(lambda: nc.vector.tensor_scalar(out=rope_qi[:, :], in0=tab[:, :], scalar1=1.0 / TWO_PI, scalar2=None,
                                                    op0=ALU.mult), r=[tab], w=[rope_qi])
                k.V(lambda: nc.vector.tensor_copy(out=rope_qf[:, :], in_=rope_qi[:, :]), r=[rope_qi], w=[rope_qf])
                k.V(lambda: nc.vector.scalar_tensor_tensor(out=tab[:, :], in0=rope_qf[:, :], scalar=-TWO_PI, in1=tab[:, :],
                                                           op0=ALU.mult, op1=ALU.add), r=[rope_qf, tab], w=[tab])
                k.V(lambda: nc.vector.tensor_scalar(out=rope_qf[:, :], in0=tab[:, :], scalar1=0.0, scalar2=TWO_PI,
                                                    op0=ALU.is_lt, op1=ALU.mult), r=[tab], w=[rope_qf])
                k.V(lambda: nc.vector.tensor_tensor(out=tab[:, :], in0=tab[:, :], in1=rope_qf[:, :], op=ALU.add),
                    r=[tab, rope_qf], w=[tab])
                k.V(lambda: nc.vector.tensor_scalar(out=tab[:, :], in0=tab[:, :], scalar1=0.0, scalar2=TWO_PI,
                                                    op0=ALU.max, op1=ALU.min), r=[tab], w=[tab])
                k.A(lambda: nc.scalar.activation(out=tab[:, :], in_=tab[:, :], func=AF.Sin, bias=pi_c[0:64, :], scale=1.0),
                    r=[tab, pi_c], w=[tab])
            if "ret" in branches:
                ret_block(l, blk)
            if "gla" in branches:
                gla_block(l, blk)
            if "ssd" in branches:
                ssd_block(l, blk)
            if "rwkv" in branches:
                rwkv_block(l, blk)
            if "mem" in branches:
                mem_block(l, blk)
            if do_merge:
                merge_block(l, blk, last=(l == nlayers - 1))
    k.finish()
    return nc, k


NCORES = 4


def make_in_maps(inputs):
    maps = []
    params = {n: np.ascontiguousarray(np.asarray(inputs[n], np.float32).reshape(SHAPES[n])) for n in PARAM_NAMES}
    for c in range(NCORES):
        b = c % 4
        m = {"x": np.ascontiguousarray(inputs["x"][b]), "mem": np.ascontiguousarray(inputs["mem"][b]),
             "positions": np.ascontiguousarray(inputs["positions"][b:b + 1]).astype(np.int32), "cst": CST}
        m.update(params)
        maps.append(m)
    return maps


def kernel(**inputs):
    nc, k = build()
    maps = make_in_maps(inputs)
    res = run_bass_kernel_spmd(nc, maps, core_ids=list(range(NCORES)))
    out = np.stack([np.asarray(res.results[b]["out"]) for b in range(4)], axis=0)
    return out.astype(np.float32)
```

```python
import contextlib
import math
import numpy as np
import concourse.bass as bass
import concourse.mybir as mybir
from concourse.bass_utils import run_bass_kernel_spmd

F32 = mybir.dt.float32
F32R = mybir.dt.float32r
FAST = True
FAST_RW = True
I32 = mybir.dt.int32
AF = mybir.ActivationFunctionType
ALU = mybir.AluOpType
AX = mybir.AxisListType

SEQ = 4096
D = 1024
T = 128
BLK = 256
NJ = BLK // T
IN_TOTAL = 12184
C_RET_Q, C_RET_K, C_RET_V, C_RET_G = 0, 256, 512, 1024
C_GLA_Q, C_GLA_K, C_GLA_V, C_GLA_GK, C_GLA_G = 1536, 1792, 2048, 2560, 2576
C_SSD_XBC, C_SSD_DT, C_SSD_Z = 3088, 4112, 4120
C_RWKV_IN, C_RWKV_G, C_MEM_Q, C_GATES = 4632, 6296, 6808, 7064
SEM_LIMIT = 30000
TWO_PI = 2.0 * math.pi


class Buf:
    __slots__ = ("w", "r")

    def __init__(self):
        self.w = None
        self.r = {}


class Tl:
    def __init__(self, t):
        self.t = t
        self.b = Buf()

    def __getitem__(self, k):
        return self.t[k]


def _bufs(lst):
    out = []
    for x in lst:
        if x is None:
            continue
        out.append(x.b if isinstance(x, Tl) else x)
    return out


class KB:
    def __init__(self, nc):
        self.nc = nc
        self.es = contextlib.ExitStack()
        self.eng = {"pe": nc.tensor, "dve": nc.vector, "act": nc.scalar, "pool": nc.gpsimd, "sp": nc.sync}
        self.nsem = 0
        self.sem = {}
        self.cnt = {}
        for e in ("pe", "dve", "act", "pool"):
            self.sem[e] = self.newsem(e)
            self.cnt[e] = 0
        self.seen = {e: {} for e in self.eng}
        self.NS = 8
        self.dsem = {q: [self.newsem("d" + q) for _ in range(self.NS)] for q in ("sp", "pool")}
        self.dval = {q: [0] * self.NS for q in ("sp", "pool")}
        self.drr = {q: 0 for q in ("sp", "pool")}
        self.ntile = 0
        self.ninst = 0

    def newsem(self, name):
        self.nsem += 1
        return self.es.enter_context(self.nc.semaphore(f"{name}_{self.nsem}"))

    def sb(self, shape, dtype=F32, name=None):
        self.ntile += 1
        return Tl(self.es.enter_context(self.nc.sbuf_tensor(f"{name or 'sb'}_{self.ntile}", list(shape), dtype)))

    def ps(self, shape=(128, 512), dtype=F32, name=None):
        self.ntile += 1
        return Tl(self.es.enter_context(self.nc.psum_tensor(f"{name or 'ps'}_{self.ntile}", list(shape), dtype)))

    def _collect(self, e, reads, writes):
        need = {}

        def add(tok):
            if tok is None:
                return
            sem, val, te = tok
            if te == "pe" and e == "pe":
                return
            if self.seen[e].get(sem, 0) >= val:
                return
            if need.get(sem, 0) < val:
                need[sem] = val

        for b in reads:
            add(b.w)
        for b in writes:
            add(b.w)
            for tok in b.r.values():
                add(tok)
        return need

    def _mark(self, tok, reads, writes, e):
        for b in reads:
            b.r[e] = tok
        for b in writes:
            b.w = tok
            b.r = {}

    def op(self, e, emit, r=(), w=()):
        reads, writes = _bufs(r), _bufs(w)
        need = self._collect(e, reads, writes)
        items = list(need.items())
        eng = self.eng[e]
        for sem, val in items[:-1]:
            eng.wait_ge(sem, val)
            self.seen[e][sem] = val
        ins = emit()
        if items:
            sem, val = items[-1]
            ins._wait_ge(sem, val)
            self.seen[e][sem] = val
        if self.cnt[e] >= SEM_LIMIT:
            self.sem[e] = self.newsem(e)
            self.cnt[e] = 0
        self.cnt[e] += 1
        ins.then_inc(self.sem[e], 1)
        self.ninst += 1
        self._mark((self.sem[e], self.cnt[e], e), reads, writes, e)
        return ins

    def dma(self, q, out, in_, r=(), w=(), **kw):
        reads, writes = _bufs(r), _bufs(w)
        need = self._collect(q, reads, writes)
        slot = self.drr[q] % self.NS
        self.drr[q] += 1
        sem = self.dsem[q][slot]
        prev = self.dval[q][slot]
        if prev > 0 and self.seen[q].get(sem, 0) < prev:
            need[sem] = max(need.get(sem, 0), prev)
        eng = self.eng[q]
        for s, v in need.items():
            eng.wait_ge(s, v)
            self.seen[q][s] = v
        eng.dma_start(out=out, in_=in_, **kw).then_inc(sem, 16)
        self.dval[q][slot] = prev + 16
        self.ninst += 1
        self._mark((sem, prev + 16, "dma"), reads, writes, "dma_" + q + str(slot))

    def collective(self, in_ap, out_ap, r=(), w=()):
        e = "pool"
        reads, writes = _bufs(r), _bufs(w)
        need = self._collect(e, reads, writes)
        eng = self.eng[e]
        for sem, val in need.items():
            eng.wait_ge(sem, val)
            self.seen[e][sem] = val
        if not hasattr(self, "cc_sem"):
            self.cc_sem = self.newsem("cc")
            self.cc_cnt = 0
        ins = self.nc.gpsimd.collective_compute("AllReduce", ALU.add, replica_groups=[list(range(8))],
                                                ins=[in_ap], outs=[out_ap])
        self.cc_cnt += 1
        ins.then_inc(self.cc_sem)
        self.ninst += 1
        self._mark((self.cc_sem, self.cc_cnt, "cc"), reads, writes, "cc")

    def finish(self):
        sp = self.nc.sync
        for q in ("sp", "pool"):
            for s, v in zip(self.dsem[q], self.dval[q]):
                if v > 0:
                    sp.wait_ge(s, v)
        for e in ("pe", "dve", "act", "pool"):
            if self.cnt[e] > 0:
                sp.wait_ge(self.sem[e], self.cnt[e])
        if hasattr(self, "cc_sem"):
            sp.wait_ge(self.cc_sem, self.cc_cnt)

    def mm(self, out, lhsT, rhs, start=True, stop=True, r=(), w=(), fast=False):
        nc = self.nc
        if fast and FAST:
            assert lhsT.dtype == F32R and rhs.dtype == F32R
        else:
            if lhsT.dtype == F32R:
                lhsT = lhsT.bitcast(F32)
            if rhs.dtype == F32R:
                rhs = rhs.bitcast(F32)
        return self.op("pe", lambda: nc.tensor.matmul(out, lhsT, rhs, start=start, stop=stop), r, w)

    def V(self, fn, r=(), w=()):
        return self.op("dve", fn, r, w)

    def A(self, fn, r=(), w=()):
        return self.op("act", fn, r, w)

    def G(self, fn, r=(), w=()):
        return self.op("pool", fn, r, w)


class Ring:
    def __init__(self, tiles):
        self.tiles = tiles
        self.i = 0

    def next(self):
        t = self.tiles[self.i % len(self.tiles)]
        self.i += 1
        return t


def make_consts():
    cols = {}
    parts = []
    off = [0]

    def add(name, arr):
        arr = np.asarray(arr, np.float32)
        assert arr.shape[0] == 128
        arr = arr.reshape(128, -1)
        cols[name] = (off[0], arr.shape[1])
        parts.append(arr)
        off[0] += arr.shape[1]

    i = np.arange(128)
    add("ident", np.eye(128))
    add("maskT", (i[:, None] <= i[None, :]))
    add("maskS", (i[:, None] < i[None, :]))
    add("maskL", (i[:, None] > i[None, :]))
    add("ones", np.ones((128, 128)))
    add("negm", np.where(i[:, None] > i[None, :], -30000.0, 0.0))
    inv = 1.0 / (10000.0 ** np.linspace(0.0, 1.0, 32, dtype=np.float32))
    invrow = np.zeros((128, 64), np.float32)
    invrow[0, :] = np.repeat(inv.astype(np.float32), 2)
    add("invrow", invrow)
    rot = np.zeros((128, 64), np.float32)
    for p in range(32):
        rot[2 * p + 1, 2 * p] = -1.0
        rot[2 * p, 2 * p + 1] = 1.0
    add("rot", rot)
    lg = np.log(1.0 - 2.0 ** (-5.0 - np.arange(4, dtype=np.float64)))
    dec = np.zeros((128, 4, 128))
    for h in range(4):
        dec[:, h, :] = np.where(i[:, None] <= i[None, :], np.exp(lg[h] * (i[None, :] - i[:, None])), 0.0)
    add("retdec", dec)
    qs = np.zeros((128, 4, 128))
    for h in range(4):
        qs[:, h, :] = np.exp(lg[h] * (i[None, :] + 1))
    add("retqs", qs)
    ks = np.zeros((128, 4))
    for h in range(4):
        ks[:, h] = np.exp(lg[h] * (127 - i))
    add("retks", ks)
    bo = np.zeros((128, 128))
    bo[:64, :64] = 1
    bo[64:, 64:] = 1
    add("blk64", bo)
    hs = np.zeros((128, 2))
    hs[:64, 0] = 1
    hs[64:, 1] = 1
    add("headsel", hs)
    return np.concatenate(parts, axis=1), cols, [float(np.exp(lg[h] * 128)) for h in range(4)]


CST, CCOL, RET_SDEC = make_consts()

PARAM_NAMES = ["norm_g", "w_in", "gla_gk_w2", "gla_gk_b", "gla_norm_g", "ssd_conv_w", "ssd_conv_b",
               "ssd_dt_bias", "ssd_a_log", "ssd_d", "ssd_norm_g", "rwkv_mu", "rwkv_w0", "rwkv_w2",
               "rwkv_a0", "rwkv_a2", "rwkv_k_k", "rwkv_k_a", "rwkv_r_k", "rwkv_ln_g", "rwkv_ln_b",
               "mem_norm_g", "w_mem_kv", "w_up_ret", "w_up_gla", "w_up_ssd", "w_up_rwkv", "w_up_mem",
               "w_out", "final_norm_g"]
SHAPES = {
    "x": [SEQ, D], "mem": [256, D], "positions": [1, SEQ],
    "norm_g": [2, D], "w_in": [2, D, IN_TOTAL], "gla_gk_w2": [2, 16, 256], "gla_gk_b": [2, 256],
    "gla_norm_g": [2, 128], "ssd_conv_w": [2, 4, 1024], "ssd_conv_b": [2, 1024], "ssd_dt_bias": [2, 8],
    "ssd_a_log": [2, 8], "ssd_d": [2, 8], "ssd_norm_g": [2, 512], "rwkv_mu": [2, 1664],
    "rwkv_w0": [2, 512], "rwkv_w2": [2, 64, 512], "rwkv_a0": [2, 512], "rwkv_a2": [2, 64, 512],
    "rwkv_k_k": [2, 512], "rwkv_k_a": [2, 512], "rwkv_r_k": [2, 512], "rwkv_ln_g": [2, 512],
    "rwkv_ln_b": [2, 512], "mem_norm_g": [2, D], "w_mem_kv": [2, D, 512], "w_up_ret": [2, 512, D],
    "w_up_gla": [2, 512, D], "w_up_ssd": [2, 512, D], "w_up_rwkv": [2, 512, D], "w_up_mem": [2, 256, D],
    "w_out": [2, D, D], "final_norm_g": [1, D],
}


def build(nblk=SEQ // BLK, nlayers=2, debug=None, branches=("ret", "gla", "ssd", "rwkv", "mem"), dbg_what=0, dbg_layer=0, do_merge=True, pipe=False):
    nc = bass.Bass("TRN2", target_bir_lowering=False)
    k = KB(nc)
    dr = {}
    ntok_all = nblk * BLK
    for n, shp in SHAPES.items():
        shp = list(shp)
        if n in ("x",):
            shp[0] = ntok_all
        elif n == "positions":
            shp[1] = ntok_all + (BLK if pipe else 0)
        elif n not in ("mem", "final_norm_g"):
            shp[0] = nlayers
        dr[n] = nc.dram_tensor(n, shp, I32 if n == "positions" else F32, kind="ExternalInput").ap()
    cst_d = nc.dram_tensor("cst", list(CST.shape), F32, kind="ExternalInput").ap()
    out_d = nc.dram_tensor("out", [ntok_all, D], F32, kind="ExternalOutput").ap()
    x1_d = nc.dram_tensor("x1s", [ntok_all, D], F32, kind="Internal").ap()
    x1_b = [Buf() for _ in range(SEQ // BLK)]
    if pipe:
        role_d = nc.dram_tensor("role", [128, 16], F32, kind="ExternalInput").ap()
        cin_d = nc.dram_tensor("cin", [4 * BLK, D], F32, kind="Internal").ap()
        cout_d = nc.dram_tensor("cout", [4 * BLK, D], F32, kind="Internal").ap()
        cin_b, cout_b = Buf(), Buf()
        out_b = [Buf() for _ in range(SEQ // BLK)]
    dbg_d = None
    if debug is not None:
        dbg_d = nc.dram_tensor("dbg", [ntok_all, debug], F32, kind="ExternalOutput").ap()

    cst = k.sb([128, CST.shape[1]], name="cst")
    k.dma("sp", cst[:, :], cst_d[:, :], w=[cst])

    def C(name, p0=0, p1=128):
        o, n = CCOL[name]
        return cst[p0:p1, o:o + n]

    ident = C("ident")


    psr = Ring([k.ps() for _ in range(6)])
    pacc = [k.ps(name='pacc') for _ in range(2)]
    ntok = nblk * BLK
    NCH = SEQ // BLK

    def bc(ap_row, n):
        return ap_row.to_broadcast([128, n])

    xb = k.sb([128, NJ, D], name="xb")
    hT = k.sb([128, 8, BLK], F32R if FAST else F32, name="hT")
    xn = k.sb([128, D], name="xn")
    junk = k.sb([128, D], name="junk")
    ss = k.sb([128, 16], name="ss")
    wst = Ring([k.sb([128, 8, 512], F32R if FAST else F32, name="wst") for _ in range(3)])
    TK = [k.sb([128, NJ, 512], name="TK") for _ in range(4)]
    FM = [k.sb([128, BLK], name="FM") for _ in range(16)]
    sq = Ring([k.sb([128, 128], name="sq") for _ in range(16)])
    sq2 = Ring([k.sb([128, 256], name="sq2") for _ in range(6)])
    stt = Ring([k.sb([128, 32], name="st") for _ in range(8)])
    oT = [k.sb([128, 4, BLK], F32R if FAST else F32, name="oT") for _ in range(4)] + [k.sb([128, 2, BLK], F32R if FAST else F32, name="oTm")]
    obr = Ring([k.sb([128, 512], name="obr") for _ in range(2)])
    posi = k.sb([1, BLK], I32, name="posi")
    posf = k.sb([1, BLK], name="posf")
    cosT = k.sb([64, BLK], name="cosT")
    sinT = k.sb([64, BLK], name="sinT")
    rope_qi = k.sb([64, BLK], I32, name="rope_qi")
    rope_qf = k.sb([64, BLK], name="rope_qf")
    pi_c = k.sb([128, 1], name="pi_c")
    k.V(lambda: nc.vector.memset(pi_c[:, :], -math.pi), w=[pi_c])
    gcol = k.sb([128, 8], name="gcol")
    gla_w2 = k.sb([16, 256], name="gla_w2")
    gla_b = k.sb([1, 256], name="gla_b")
    gla_ng = k.sb([128, 128], name="gla_ng")
    ssd_cw = k.sb([128, 8, 4], name="ssd_cw")
    ssd_cb = k.sb([128, 8], name="ssd_cb")
    ssd_sm = k.sb([128, 32], name="ssd_sm")
    ssd_ng = k.sb([128, 512], name="ssd_ng")
    ssd_carry = k.sb([128, 8, 4], name="ssd_carry")
    rw_cols = k.sb([128, 64], name="rw_cols")
    rw_w2a2 = k.sb([128, 512], name="rw_w2a2")
    rw_lng = k.sb([128, 512], name="rw_lng")
    rw_lnb = k.sb([128, 512], name="rw_lnb")
    rw_carry = k.sb([128, 16], name="rw_carry")
    kmT = k.sb([64, 4, 256], name="kmT")
    vm = k.sb([128, 2, 256], name="vm")
    ret_S = [k.sb([64, 128], name="ret_S") for _ in range(4)]
    gla_S = [k.sb([64, 128], name="gla_S") for _ in range(4)]
    ssd_S = k.sb([128, 512], name="ssd_S")
    rw_S = [k.sb([128, 64], name="rw_S") for _ in range(4)]

    def load_w(wd, c0, ncol, rows=8, r0=0):
        wt = wst.next()
        k.dma("pool" if FAST else "sp", wt[:, 0:rows, 0:ncol],
              wd[r0 * 128:(r0 + rows) * 128, :].rearrange("(c p) n -> p c n", p=128)[:, :, c0:c0 + ncol], w=[wt])
        return wt

    def proj_fm(l, c0, ncol, evac):
        nc_ = 128 if FAST else ncol
        wt = load_w(dr["w_in"][l], c0, nc_)
        pa = psr.next()
        for dc in range(8):
            k.mm(pa[0:nc_, 0:BLK], wt[:, dc, 0:nc_], hT[:, dc, :], start=(dc == 0), stop=(dc == 7), r=[wt, hT], w=[pa],
                 fast=(nc_ == 128))
        evac(pa)

    def proj_tok(l, c0, ncol, evac):
        for g0 in range(0, ncol, 512):
            w_ = min(512, ncol - g0)
            wt = load_w(dr["w_in"][l], c0 + g0, w_)
            for j in range(NJ):
                pa = psr.next()
                for dc in range(8):
                    k.mm(pa[:, 0:w_], hT[:, dc, j * T:(j + 1) * T], wt[:, dc, 0:w_], start=(dc == 0), stop=(dc == 7),
                         r=[wt, hT], w=[pa], fast=(w_ % 2 == 0))
                evac(j, g0, w_, pa)

    def tr(dst_ap, dst_tl, src_ap, src_tl, npart, nfree, eng="act"):
        pa = psr.next()
        k.op("pe", lambda: nc.tensor.transpose(pa[0:nfree, 0:npart], src_ap, ident[0:npart, 0:npart]),
             r=[src_tl, cst], w=[pa])
        if eng == "act":
            k.A(lambda: nc.scalar.copy(out=dst_ap, in_=pa[0:nfree, 0:npart]), r=[pa], w=[dst_tl])
        else:
            k.V(lambda: nc.vector.tensor_copy(out=dst_ap, in_=pa[0:nfree, 0:npart]), r=[pa], w=[dst_tl])

    def rstd_groups(y_tl, y_ap_of, n, w, eps, st, c0):
        for g in range(n):
            k.A(lambda: nc.scalar.activation(out=junk[:, 0:w], in_=y_ap_of(g), func=AF.Square,
                                             accum_out=st[:, c0 + g:c0 + g + 1]), r=[y_tl], w=[junk, st])
        k.V(lambda: nc.vector.tensor_scalar(out=st[:, c0 + n:c0 + 2 * n], in0=st[:, c0:c0 + n], scalar1=1.0 / w,
                                            scalar2=eps, op0=ALU.mult, op1=ALU.add), r=[st], w=[st])
        k.A(lambda: nc.scalar.sqrt(out=st[:, c0 + n:c0 + 2 * n], in_=st[:, c0 + n:c0 + 2 * n]), r=[st], w=[st])
        k.V(lambda: nc.vector.reciprocal(out=st[:, c0 + 2 * n:c0 + 3 * n], in_=st[:, c0 + n:c0 + 2 * n]), r=[st], w=[st])

    def softplus_ip(x_ap, x_tl, n):
        a = sq2.next()
        k.A(lambda: nc.scalar.activation(out=a[:, 0:n], in_=x_ap, func=AF.Abs), r=[x_tl], w=[a])
        k.A(lambda: nc.scalar.activation(out=a[:, 0:n], in_=a[:, 0:n], func=AF.Exp, scale=-1.0), r=[a], w=[a])
        k.A(lambda: nc.scalar.activation(out=a[:, 0:n], in_=a[:, 0:n], func=AF.Ln, bias=1.0), r=[a], w=[a])
        k.V(lambda: nc.vector.scalar_tensor_tensor(out=x_ap, in0=x_ap, scalar=0.0, in1=a[:, 0:n], op0=ALU.max,
                                                   op1=ALU.add), r=[x_tl, a], w=[x_tl])

    def emit_out(bi, l, blk, j, o, width):
        t0 = blk * BLK
        for c in range(width // 128):
            tr(oT[bi][:, c, j * T:(j + 1) * T], oT[bi], o[:, c * 128:(c + 1) * 128], o, 128, 128,
               eng=("act" if c % 2 == 0 else "dve"))
        if debug is not None and l == dbg_layer:
            k.dma("pool", dbg_d[t0 + j * T:t0 + (j + 1) * T, bi * 512:bi * 512 + width], o[:, 0:width], r=[o])

    def load_params(l):
        NCg = dict(allow_slow_non_contiguous=True)
        k.dma("sp", gcol[:, :], dr["norm_g"][l].rearrange("(c p) -> p c", p=128), w=[gcol], **NCg)
        k.dma("sp", gla_w2[:, :], dr["gla_gk_w2"][l], w=[gla_w2])
        k.dma("sp", gla_b[:, :], dr["gla_gk_b"][l:l + 1, :], w=[gla_b])
        k.dma("sp", gla_ng[:, :], bc(dr["gla_norm_g"][l:l + 1, :], 128), w=[gla_ng])
        for j_ in range(4):
            k.dma("sp", ssd_cw[:, :, j_], dr["ssd_conv_w"][l, j_].rearrange("(c p) -> p c", p=128), w=[ssd_cw], **NCg)
        k.dma("sp", ssd_cb[:, :], dr["ssd_conv_b"][l].rearrange("(c p) -> p c", p=128), w=[ssd_cb], **NCg)
        k.dma("sp", ssd_sm[:, 0:8], bc(dr["ssd_dt_bias"][l:l + 1, :], 8), w=[ssd_sm])
        k.dma("sp", ssd_sm[:, 8:16], bc(dr["ssd_a_log"][l:l + 1, :], 8), w=[ssd_sm])
        k.dma("sp", ssd_sm[:, 16:24], bc(dr["ssd_d"][l:l + 1, :], 8), w=[ssd_sm])
        k.A(lambda: nc.scalar.activation(out=ssd_sm[:, 8:16], in_=ssd_sm[:, 8:16], func=AF.Exp), r=[ssd_sm], w=[ssd_sm])
        k.V(lambda: nc.vector.tensor_scalar(out=ssd_sm[:, 8:16], in0=ssd_sm[:, 8:16], scalar1=-1.0, scalar2=None,
                                            op0=ALU.mult), r=[ssd_sm], w=[ssd_sm])
        k.dma("sp", ssd_ng[:, :], bc(dr["ssd_norm_g"][l:l + 1, :], 512), w=[ssd_ng])
        k.dma("sp", rw_cols[:, 0:13], dr["rwkv_mu"][l].rearrange("(c p) -> p c", p=128), w=[rw_cols], **NCg)
        for i_, nm in enumerate(["rwkv_w0", "rwkv_a0", "rwkv_k_k", "rwkv_k_a", "rwkv_r_k"]):
            k.dma("sp", rw_cols[:, 16 + 4 * i_:20 + 4 * i_], dr[nm][l].rearrange("(c p) -> p c", p=128), w=[rw_cols], **NCg)
        k.V(lambda: nc.vector.tensor_scalar(out=rw_cols[:, 40:53], in0=rw_cols[:, 0:13], scalar1=-1.0, scalar2=1.0,
                                            op0=ALU.mult, op1=ALU.add), r=[rw_cols], w=[rw_cols])
        k.dma("sp", rw_w2a2[0:64, :], dr["rwkv_w2"][l], w=[rw_w2a2])
        k.dma("sp", rw_w2a2[64:128, :], dr["rwkv_a2"][l], w=[rw_w2a2])
        k.dma("sp", rw_lng[:, :], bc(dr["rwkv_ln_g"][l:l + 1, :], 512), w=[rw_lng])
        k.dma("sp", rw_lnb[:, :], bc(dr["rwkv_ln_b"][l:l + 1, :], 512), w=[rw_lnb])
        mg = stt.next()
        k.dma("sp", mg[:, 0:8], dr["mem_norm_g"][l].rearrange("(c p) -> p c", p=128), w=[mg], **NCg)
        memT = FM[0:8]
        for mt in range(2):
            k.dma("sp", xn[:, :], dr["mem"][mt * 128:(mt + 1) * 128, :], w=[xn])
            st = stt.next()
            k.A(lambda: nc.scalar.activation(out=junk[:, :], in_=xn[:, :], func=AF.Square, accum_out=st[:, 0:1]),
                r=[xn], w=[junk, st])
            k.V(lambda: nc.vector.tensor_scalar(out=st[:, 1:2], in0=st[:, 0:1], scalar1=1.0 / D, scalar2=1e-6,
                                                op0=ALU.mult, op1=ALU.add), r=[st], w=[st])
            k.A(lambda: nc.scalar.sqrt(out=st[:, 1:2], in_=st[:, 1:2]), r=[st], w=[st])
            k.V(lambda: nc.vector.reciprocal(out=st[:, 2:3], in_=st[:, 1:2]), r=[st], w=[st])
            k.V(lambda: nc.vector.tensor_scalar(out=xn[:, :], in0=xn[:, :], scalar1=st[:, 2:3], scalar2=None,
                                                op0=ALU.mult), r=[xn, st], w=[xn])
            for dc in range(8):
                pa = psr.next()
                k.op("pe", lambda: nc.tensor.transpose(pa[:, 0:T], xn[:, dc * T:(dc + 1) * T], ident), r=[xn, cst], w=[pa])
                k.A(lambda: nc.scalar.activation(out=memT[dc][:, mt * T:(mt + 1) * T], in_=pa[:, 0:T], func=AF.Identity,
                                                 scale=mg[:, dc:dc + 1]), r=[pa, mg], w=[memT[dc]])
        wt = load_w(dr["w_mem_kv"][l], 0, 512)
        for h in range(4):
            pa = psr.next()
            for dc in range(8):
                k.mm(pa[0:64, 0:256], wt[:, dc, h * 64:(h + 1) * 64], memT[dc][:, 0:256], start=(dc == 0), stop=(dc == 7),
                     r=[wt, memT[dc]], w=[pa])
            k.A(lambda: nc.scalar.copy(out=kmT[:, h, :], in_=pa[0:64, 0:256]), r=[pa], w=[kmT])
        for mt in range(2):
            pa = psr.next()
            for dc in range(8):
                k.mm(pa[:, 0:256], memT[dc][:, mt * T:(mt + 1) * T], wt[:, dc, 256:512], start=(dc == 0), stop=(dc == 7),
                     r=[wt, memT[dc]], w=[pa])
            k.A(lambda: nc.scalar.copy(out=vm[:, mt, :], in_=pa[:, 0:256]), r=[pa], w=[vm])
        for s_ in ret_S + gla_S + rw_S + [ssd_S]:
            k.V(lambda: nc.vector.memset(s_[:, :], 0.0), w=[s_])
        k.V(lambda: nc.vector.memset(ssd_carry[:, :, :], 0.0), w=[ssd_carry])
        k.V(lambda: nc.vector.memset(rw_carry[:, :], 0.0), w=[rw_carry])

    def la_step(ks_ap, ks_tl, qs_ap, qs_tl, qi_ap, qi_tl, mask_ap, mask_tl, v_ap, v_tl, S, py_ap, py):
        psc = psr.next()
        k.mm(psc[:, 0:T], ks_ap, qs_ap, r=[ks_tl, qs_tl], w=[psc])
        sc = sq.next()
        k.V(lambda: nc.vector.tensor_tensor(out=sc[:, :], in0=psc[:, 0:T], in1=mask_ap, op=ALU.mult),
            r=[psc, mask_tl], w=[sc])
        k.mm(py_ap, sc[:, :], v_ap, start=True, stop=False, r=[sc, v_tl], w=[py])
        k.mm(py_ap, qi_ap, S[:, :], start=False, stop=True, r=[qi_tl, S], w=[py])

    def ret_block(l, blk):
        t0 = blk * BLK
        qT, kT = FM[0:4], FM[4:8]
        vt, gt = TK[0], TK[1]
        for c_base, dst, scale in ((C_RET_Q, qT, 1.0), (C_RET_K, kT, 0.125)):
            for h in range(4):
                def ev(pa, h=h, dst=dst, scale=scale):
                    ta, tb = FM[8], FM[9]
                    k.A(lambda: nc.scalar.activation(out=ta[0:64, :], in_=pa[0:64, 0:BLK], func=AF.Copy, scale=scale),
                        r=[pa], w=[ta])
                    pb = psr.next()
                    k.mm(pb[0:64, 0:BLK], C("rot", 0, 64), ta[0:64, :], r=[cst, ta], w=[pb])
                    k.V(lambda: nc.vector.tensor_tensor(out=tb[0:64, :], in0=pb[0:64, 0:BLK], in1=sinT[:, :], op=ALU.mult),
                        r=[pb, sinT], w=[tb])
                    k.V(lambda: nc.vector.tensor_tensor(out=ta[0:64, :], in0=ta[0:64, :], in1=cosT[:, :], op=ALU.mult),
                        r=[ta, cosT], w=[ta])
                    k.V(lambda: nc.vector.tensor_tensor(out=dst[h][0:64, :], in0=ta[0:64, :], in1=tb[0:64, :], op=ALU.add),
                        r=[ta, tb], w=[dst[h]])
                proj_fm(l, c_base + 64 * h, 64, ev)
        proj_tok(l, C_RET_V, 512, lambda j, g0, w_, pa: k.A(
            lambda: nc.scalar.copy(out=vt[:, j, g0:g0 + w_], in_=pa[:, 0:w_]), r=[pa], w=[vt]))
        proj_tok(l, C_RET_G, 512, lambda j, g0, w_, pa: k.A(
            lambda: nc.scalar.activation(out=gt[:, j, g0:g0 + w_], in_=pa[:, 0:w_], func=AF.Silu), r=[pa], w=[gt]))
        ko, _ = CCOL["retks"]
        for j in range(NJ):
            py = pacc[j % 2]
            for h in range(4):
                qs_ = qT[h][0:64, j * T:(j + 1) * T]
                ks_ = kT[h][0:64, j * T:(j + 1) * T]
                vh = vt[:, j, h * 128:(h + 1) * 128]
                qi = sq.next()
                k.V(lambda: nc.vector.tensor_tensor(out=qi[0:64, :], in0=qs_, in1=C("retqs", 0, 64)[:, h * T:(h + 1) * T],
                                                    op=ALU.mult), r=[qT[h], cst], w=[qi])
                la_step(ks_, kT[h], qs_, qT[h], qi[0:64, :], qi, C("retdec")[:, h * T:(h + 1) * T], cst, vh, vt,
                        ret_S[h], py[:, h * 128:(h + 1) * 128], py)
                kt = sq.next()
                pt = psr.next()
                k.op("pe", lambda: nc.tensor.transpose(pt[:, 0:64], ks_, ident[0:64, 0:64]), r=[kT[h], cst], w=[pt])
                k.A(lambda: nc.scalar.activation(out=kt[:, 0:64], in_=pt[:, 0:64], func=AF.Identity,
                                                 scale=cst[:, ko + h:ko + h + 1]), r=[pt, cst], w=[kt])
                pds = psr.next()
                k.mm(pds[0:64, 0:128], kt[:, 0:64], vh, r=[kt, vt], w=[pds])
                k.V(lambda: nc.vector.scalar_tensor_tensor(out=ret_S[h][:, :], in0=ret_S[h][:, :], scalar=RET_SDEC[h],
                                                           in1=pds[0:64, 0:128], op0=ALU.mult, op1=ALU.add),
                    r=[ret_S[h], pds], w=[ret_S[h]])
            st = stt.next()
            rstd_groups(py, lambda g: py[:, g * 128:(g + 1) * 128], 4, 128, 1e-6, st, 0)
            o = obr.next()
            for h in range(4):
                k.V(lambda: nc.vector.scalar_tensor_tensor(out=o[:, h * 128:(h + 1) * 128], in0=py[:, h * 128:(h + 1) * 128],
                                                           scalar=st[:, 8 + h:9 + h], in1=gt[:, j, h * 128:(h + 1) * 128],
                                                           op0=ALU.mult, op1=ALU.mult), r=[py, st, gt], w=[o])
            emit_out(0, l, blk, j, o, 512)

    def gla_block(l, blk):
        qT, kT = FM[0:4], FM[4:8]
        vt, gt = TK[0], TK[1]
        glow = FM[8]
        for h in range(4):
            proj_fm(l, C_GLA_Q + 64 * h, 64, lambda pa, h=h: k.A(
                lambda: nc.scalar.activation(out=qT[h][0:64, :], in_=pa[0:64, 0:BLK], func=AF.Copy, scale=0.125),
                r=[pa], w=[qT[h]]))
            proj_fm(l, C_GLA_K + 64 * h, 64, lambda pa, h=h: k.A(
                lambda: nc.scalar.copy(out=kT[h][0:64, :], in_=pa[0:64, 0:BLK]), r=[pa], w=[kT[h]]))
        proj_fm(l, C_GLA_GK, 16, lambda pa: k.A(
            lambda: nc.scalar.copy(out=glow[0:16, :], in_=pa[0:16, 0:BLK]), r=[pa], w=[glow]))
        proj_tok(l, C_GLA_V, 512, lambda j, g0, w_, pa: k.A(
            lambda: nc.scalar.copy(out=vt[:, j, g0:g0 + w_], in_=pa[:, 0:w_]), r=[pa], w=[vt]))
        proj_tok(l, C_GLA_G, 512, lambda j, g0, w_, pa: k.A(
            lambda: nc.scalar.activation(out=gt[:, j, g0:g0 + w_], in_=pa[:, 0:w_], func=AF.Silu), r=[pa], w=[gt]))
        for j in range(NJ):
            pl = psr.next()
            k.mm(pl[:, 0:256], glow[0:16, j * T:(j + 1) * T], gla_w2[:, :], start=True, stop=False, r=[glow, gla_w2], w=[pl])
            k.mm(pl[:, 0:256], C("ones", 0, 1), gla_b[:, :], start=False, stop=True, r=[cst, gla_b], w=[pl])
            lg = sq2.next()
            k.A(lambda: nc.scalar.activation(out=lg[:, :], in_=pl[:, 0:256], func=AF.Copy, scale=-1.0), r=[pl], w=[lg])
            softplus_ip(lg[:, :], lg, 256)
            k.V(lambda: nc.vector.tensor_scalar(out=lg[:, :], in0=lg[:, :], scalar1=-1.0 / 16.0, scalar2=None,
                                                op0=ALU.mult), r=[lg], w=[lg])
            py = pacc[j % 2]
            for h in range(4):
                pc = psr.next()
                k.mm(pc[0:64, 0:T], lg[:, h * 64:(h + 1) * 64], C("maskT"), r=[lg, cst], w=[pc])
                st = stt.next()
                k.V(lambda: nc.vector.tensor_copy(out=st[0:64, 0:1], in_=pc[0:64, 64:65]), r=[pc], w=[st])
                k.V(lambda: nc.vector.tensor_scalar(out=st[0:64, 1:2], in0=pc[0:64, 64:65], scalar1=-1.0, scalar2=None,
                                                    op0=ALU.mult), r=[pc], w=[st])
                ekin, eqin, eq = sq.next(), sq.next(), sq.next()
                k.A(lambda: nc.scalar.activation(out=ekin[0:64, :], in_=pc[0:64, 0:T], func=AF.Exp, scale=-1.0,
                                                 bias=st[0:64, 0:1]), r=[pc, st], w=[ekin])
                k.A(lambda: nc.scalar.activation(out=eqin[0:64, :], in_=pc[0:64, 0:T], func=AF.Exp, scale=1.0,
                                                 bias=st[0:64, 1:2]), r=[pc, st], w=[eqin])
                k.A(lambda: nc.scalar.activation(out=eq[0:64, :], in_=pc[0:64, 0:T], func=AF.Exp), r=[pc], w=[eq])
                k.A(lambda: nc.scalar.activation(out=st[0:64, 2:3], in_=pc[0:64, T - 1:T], func=AF.Exp), r=[pc], w=[st])
                k.A(lambda: nc.scalar.activation(out=st[0:64, 3:4], in_=pc[0:64, T - 1:T], func=AF.Exp, scale=1.0,
                                                 bias=st[0:64, 1:2]), r=[pc, st], w=[st])
                qs_ = qT[h][0:64, j * T:(j + 1) * T]
                ks_ = kT[h][0:64, j * T:(j + 1) * T]
                k.V(lambda: nc.vector.tensor_tensor(out=ekin[0:64, :], in0=ekin[0:64, :], in1=ks_, op=ALU.mult),
                    r=[ekin, kT[h]], w=[ekin])
                k.V(lambda: nc.vector.tensor_tensor(out=eqin[0:64, :], in0=eqin[0:64, :], in1=qs_, op=ALU.mult),
                    r=[eqin, qT[h]], w=[eqin])
                k.V(lambda: nc.vector.tensor_tensor(out=eq[0:64, :], in0=eq[0:64, :], in1=qs_, op=ALU.mult),
                    r=[eq, qT[h]], w=[eq])
                vh = vt[:, j, h * 128:(h + 1) * 128]
                la_step(ekin[0:64, :], ekin, eqin[0:64, :], eqin, eq[0:64, :], eq, C("maskT"), cst, vh, vt,
                        gla_S[h], py[:, h * 128:(h + 1) * 128], py)
                kt = sq.next()
                tr(kt[:, 0:64], kt, ekin[0:64, :], ekin, 64, 128)
                pds = psr.next()
                k.mm(pds[0:64, 0:128], kt[:, 0:64], vh, r=[kt, vt], w=[pds])
                k.V(lambda: nc.vector.tensor_scalar(out=gla_S[h][:, :], in0=gla_S[h][:, :], scalar1=st[0:64, 2:3],
                                                    scalar2=None, op0=ALU.mult), r=[gla_S[h], st], w=[gla_S[h]])
                k.V(lambda: nc.vector.scalar_tensor_tensor(out=gla_S[h][:, :], in0=pds[0:64, 0:128], scalar=st[0:64, 3:4],
                                                           in1=gla_S[h][:, :], op0=ALU.mult, op1=ALU.add),
                    r=[gla_S[h], pds, st], w=[gla_S[h]])
            st = stt.next()
            rstd_groups(py, lambda g: py[:, g * 128:(g + 1) * 128], 4, 128, 1e-6, st, 0)
            o = obr.next()
            for h in range(4):
                k.V(lambda: nc.vector.scalar_tensor_tensor(out=o[:, h * 128:(h + 1) * 128], in0=py[:, h * 128:(h + 1) * 128],
                                                           scalar=st[:, 8 + h:9 + h], in1=gt[:, j, h * 128:(h + 1) * 128],
                                                           op0=ALU.mult, op1=ALU.mult), r=[py, st, gt], w=[o])
                k.V(lambda: nc.vector.tensor_tensor(out=o[:, h * 128:(h + 1) * 128], in0=o[:, h * 128:(h + 1) * 128],
                                                    in1=gla_ng[:, :], op=ALU.mult), r=[o, gla_ng], w=[o])
            emit_out(1, l, blk, j, o, 512)

    def ssd_block(l, blk):
        cv = FM[0:8]
        zt, dtt = TK[0], TK[1]
        raw = FM[8]
        for c in range(8):
            def ev(pa, c=c):
                k.A(lambda: nc.scalar.copy(out=raw[:, :], in_=pa[:, 0:BLK]), r=[pa], w=[raw])
                acc = cv[c]
                k.V(lambda: nc.vector.tensor_scalar(out=acc[:, :], in0=raw[:, :], scalar1=ssd_cw[:, c, 3:4], scalar2=None,
                                                    op0=ALU.mult), r=[raw, ssd_cw], w=[acc])
                for d_ in (1, 2, 3):
                    k.V(lambda: nc.vector.scalar_tensor_tensor(out=acc[:, d_:BLK], in0=raw[:, 0:BLK - d_],
                                                               scalar=ssd_cw[:, c, 3 - d_:4 - d_], in1=acc[:, d_:BLK],
                                                               op0=ALU.mult, op1=ALU.add), r=[raw, ssd_cw, acc], w=[acc])
                    k.V(lambda: nc.vector.scalar_tensor_tensor(out=acc[:, 0:d_], in0=ssd_carry[:, c, 3 - d_:3],
                                                               scalar=ssd_cw[:, c, 3 - d_:4 - d_], in1=acc[:, 0:d_],
                                                               op0=ALU.mult, op1=ALU.add),
                        r=[ssd_carry, ssd_cw, acc], w=[acc])
                k.V(lambda: nc.vector.tensor_copy(out=ssd_carry[:, c, 0:3], in_=raw[:, BLK - 3:BLK]), r=[raw], w=[ssd_carry])
                k.A(lambda: nc.scalar.activation(out=acc[:, :], in_=acc[:, :], func=AF.Silu, bias=ssd_cb[:, c:c + 1],
                                                 scale=1.0), r=[acc, ssd_cb], w=[acc])
            proj_fm(l, C_SSD_XBC + 128 * c, 128, ev)
        proj_tok(l, C_SSD_Z, 512, lambda j, g0, w_, pa: k.A(
            lambda: nc.scalar.activation(out=zt[:, j, g0:g0 + w_], in_=pa[:, 0:w_], func=AF.Silu), r=[pa], w=[zt]))

        def ev_dt(j, g0, w_, pa):
            k.V(lambda: nc.vector.tensor_tensor(out=dtt[:, j, 0:8], in0=pa[:, 0:8], in1=ssd_sm[:, 0:8], op=ALU.add),
                r=[pa, ssd_sm], w=[dtt])
            softplus_ip(dtt[:, j, 0:8], dtt, 8)
        proj_tok(l, C_SSD_DT, 8, ev_dt)
        for j in range(NJ):
            xs = obr.next()
            for c in range(4):
                tr(xs[:, c * 128:(c + 1) * 128], xs, cv[c][:, j * T:(j + 1) * T], cv[c], 128, 128,
                   eng=("act" if c % 2 == 0 else "dve"))
            btok = [sq.next(), sq.next()]
            for g in range(2):
                tr(btok[g][:, :], btok[g], cv[4 + g][:, j * T:(j + 1) * T], cv[4 + g], 128, 128)
            st = stt.next()
            k.V(lambda: nc.vector.tensor_tensor(out=st[:, 0:8], in0=dtt[:, j, 0:8], in1=ssd_sm[:, 8:16], op=ALU.mult),
                r=[dtt, ssd_sm], w=[st])
            pq = psr.next()
            k.mm(pq[:, 0:8], C("maskT"), st[:, 0:8], r=[cst, st], w=[pq])
            k.mm(pq[:, 8:16], C("maskL"), st[:, 0:8], r=[cst, st], w=[pq])
            k.mm(pq[:, 16:24], C("ones"), st[:, 0:8], r=[cst, st], w=[pq])
            k.A(lambda: nc.scalar.activation(out=st[:, 8:32], in_=pq[:, 0:24], func=AF.Exp), r=[pq], w=[st])
            rhsM = [sq2.next() for _ in range(4)]
            for h in range(8):
                k.V(lambda: nc.vector.tensor_scalar(out=rhsM[h // 2][:, (h % 2) * T:(h % 2 + 1) * T], in0=C("maskT"),
                                                    scalar1=st[:, h:h + 1], scalar2=None, op0=ALU.mult),
                    r=[cst, st], w=[rhsM[h // 2]])
            v1 = sq2.next(), sq2.next()
            st2 = stt.next()
            k.V(lambda: nc.vector.tensor_tensor(out=st2[:, 0:8], in0=dtt[:, j, 0:8], in1=st[:, 16:24], op=ALU.mult),
                r=[dtt, st], w=[st2])
            for g in range(2):
                k.V(lambda: nc.vector.tensor_tensor(
                    out=v1[g][:, :].rearrange("p (h d) -> p h d", h=4),
                    in0=xs[:, g * 256:(g + 1) * 256].rearrange("p (h d) -> p h d", h=4),
                    in1=dtt[:, j, g * 4:(g + 1) * 4].unsqueeze(2).to_broadcast([128, 4, 64]), op=ALU.mult),
                    r=[xs, dtt], w=[v1[g]])
            k.V(lambda: nc.vector.tensor_tensor(
                out=junk[:, 0:512].rearrange("p (h d) -> p h d", h=8),
                in0=xs[:, :].rearrange("p (h d) -> p h d", h=8),
                in1=st2[:, 0:8].unsqueeze(2).to_broadcast([128, 8, 64]), op=ALU.mult), r=[xs, st2], w=[junk])
            pyi = pacc[0]
            pye = pacc[1]
            for g in range(2):
                bT = cv[4 + g][:, j * T:(j + 1) * T]
                cT = cv[6 + g][:, j * T:(j + 1) * T]
                psc = psr.next()
                k.mm(psc[:, 0:T], bT, cT, r=[cv[4 + g], cv[6 + g]], w=[psc])
                scg = sq.next()
                k.A(lambda: nc.scalar.copy(out=scg[:, :], in_=psc[:, 0:T]), r=[psc], w=[scg])
                for hh in range(4):
                    h = g * 4 + hh
                    pd = psr.next()
                    k.mm(pd[:, 0:T], C("maskL"), rhsM[h // 2][:, (h % 2) * T:(h % 2 + 1) * T], start=True, stop=False,
                         r=[cst, rhsM[h // 2]], w=[pd])
                    k.mm(pd[:, 0:T], ident, C("negm"), start=False, stop=True, r=[cst], w=[pd])
                    ex = sq.next()
                    k.A(lambda: nc.scalar.activation(out=ex[:, :], in_=pd[:, 0:T], func=AF.Exp), r=[pd], w=[ex])
                    k.V(lambda: nc.vector.tensor_tensor(out=ex[:, :], in0=ex[:, :], in1=scg[:, :], op=ALU.mult),
                        r=[ex, scg], w=[ex])
                    k.mm(pyi[:, h * 64:(h + 1) * 64], ex[:, :], v1[g][:, hh * 64:(hh + 1) * 64], r=[ex, v1[g]], w=[pyi])
                k.mm(pye[:, g * 256:(g + 1) * 256], cT, ssd_S[:, g * 256:(g + 1) * 256], r=[cv[6 + g], ssd_S], w=[pye])
                pds = psr.next()
                k.mm(pds[:, 0:256], btok[g][:, :], junk[:, g * 256:(g + 1) * 256], r=[btok[g], junk], w=[pds])
                sg = ssd_S[:, g * 256:(g + 1) * 256].rearrange("p (h d) -> p h d", h=4)
                k.V(lambda: nc.vector.tensor_tensor(out=sg, in0=sg,
                                                    in1=st[:, 24 + g * 4:28 + g * 4].unsqueeze(2).to_broadcast([128, 4, 64]),
                                                    op=ALU.mult), r=[ssd_S, st], w=[ssd_S])
                k.V(lambda: nc.vector.tensor_tensor(out=ssd_S[:, g * 256:(g + 1) * 256], in0=ssd_S[:, g * 256:(g + 1) * 256],
                                                    in1=pds[:, 0:256], op=ALU.add), r=[ssd_S, pds], w=[ssd_S])
            o = obr.next()
            k.A(lambda: nc.scalar.copy(out=o[:, :], in_=pyi[:, :]), r=[pyi], w=[o])
            y3 = o[:, :].rearrange("p (h d) -> p h d", h=8)
            tmp = junk[:, 512:1024]
            k.V(lambda: nc.vector.tensor_tensor(out=tmp.rearrange("p (h d) -> p h d", h=8),
                                                in0=pye[:, :].rearrange("p (h d) -> p h d", h=8),
                                                in1=st[:, 8:16].unsqueeze(2).to_broadcast([128, 8, 64]), op=ALU.mult),
                r=[pye, st], w=[junk])
            k.V(lambda: nc.vector.tensor_tensor(out=o[:, :], in0=o[:, :], in1=tmp, op=ALU.add), r=[o, junk], w=[o])
            k.V(lambda: nc.vector.tensor_tensor(out=tmp.rearrange("p (h d) -> p h d", h=8),
                                                in0=xs[:, :].rearrange("p (h d) -> p h d", h=8),
                                                in1=ssd_sm[:, 16:24].unsqueeze(2).to_broadcast([128, 8, 64]), op=ALU.mult),
                r=[xs, ssd_sm], w=[junk])
            k.V(lambda: nc.vector.tensor_tensor(out=o[:, :], in0=o[:, :], in1=tmp, op=ALU.add), r=[o, junk], w=[o])
            k.V(lambda: nc.vector.tensor_tensor(out=o[:, :], in0=o[:, :], in1=zt[:, j, :], op=ALU.mult), r=[o, zt], w=[o])
            st3 = stt.next()
            rstd_groups(o, lambda g: o[:, g * 256:(g + 1) * 256], 2, 256, 1e-6, st3, 0)
            for g in range(2):
                k.V(lambda: nc.vector.scalar_tensor_tensor(out=o[:, g * 256:(g + 1) * 256], in0=o[:, g * 256:(g + 1) * 256],
                                                           scalar=st3[:, 4 + g:5 + g], in1=ssd_ng[:, g * 256:(g + 1) * 256],
                                                           op0=ALU.mult, op1=ALU.mult), r=[o, st3, ssd_ng], w=[o])
            emit_out(2, l, blk, j, o, 512)

    def mem_block(l, blk):
        qT = FM[0:4]
        for h in range(4):
            proj_fm(l, C_MEM_Q + 64 * h, 64, lambda pa, h=h: k.A(
                lambda: nc.scalar.activation(out=qT[h][0:64, :], in_=pa[0:64, 0:BLK], func=AF.Copy, scale=0.125),
                r=[pa], w=[qT[h]]))
        for j in range(NJ):
            st = stt.next()
            po = pacc[j % 2]
            o = obr.next()
            for h in range(4):
                psc = psr.next()
                k.mm(psc[:, 0:256], qT[h][0:64, j * T:(j + 1) * T], kmT[:, h, :], r=[qT[h], kmT], w=[psc])
                k.V(lambda: nc.vector.tensor_reduce(out=st[:, h:h + 1], in_=psc[:, 0:256], axis=AX.X, op=ALU.max),
                    r=[psc], w=[st])
                k.V(lambda: nc.vector.tensor_scalar(out=st[:, 4 + h:5 + h], in0=st[:, h:h + 1], scalar1=-1.0, scalar2=None,
                                                    op0=ALU.mult), r=[st], w=[st])
                pe_ = sq2.next()
                k.A(lambda: nc.scalar.activation(out=pe_[:, :], in_=psc[:, 0:256], func=AF.Exp, bias=st[:, 4 + h:5 + h],
                                                 scale=1.0, accum_out=st[:, 8 + h:9 + h]), r=[psc, st], w=[pe_, st])
                pT = [sq.next(), sq.next()]
                for mt in range(2):
                    tr(pT[mt][:, :], pT[mt], pe_[:, mt * T:(mt + 1) * T], pe_, 128, 128, eng=("act" if mt == 0 else "dve"))
                for mt in range(2):
                    k.mm(po[:, h * 64:(h + 1) * 64], pT[mt][:, :], vm[:, mt, h * 64:(h + 1) * 64], start=(mt == 0),
                         stop=(mt == 1), r=[pT[mt], vm], w=[po])
            k.V(lambda: nc.vector.reciprocal(out=st[:, 12:16], in_=st[:, 8:12]), r=[st], w=[st])
            k.V(lambda: nc.vector.tensor_tensor(out=o[:, 0:256].rearrange("p (h d) -> p h d", h=4),
                                                in0=po[:, 0:256].rearrange("p (h d) -> p h d", h=4),
                                                in1=st[:, 12:16].unsqueeze(2).to_broadcast([128, 4, 64]), op=ALU.mult),
                r=[po, st], w=[o])
            emit_out(4, l, blk, j, o, 256)

    RWD = F32R if (FAST and FAST_RW) else F32
    rwt = [k.sb([128, 128], F32 if i_ < 4 else RWD, name="rwt") for i_ in range(26)]
    rwx = [k.sb([128, 128], RWD, name="rwx") for _ in range(2)]
    rw_tmpS = k.sb([128, 64], name="rw_tmpS")

    def f32(ap):
        return ap.bitcast(F32) if ap.dtype == F32R else ap
    rw_AR = k.sb([128, 256], RWD, name="rw_AR")
    rw_bon = k.sb([128, NJ, 8], name="rw_bon")

    def rwkv_block(l, blk):
        xr, xk, xv, ldt, at, kkt, tmpt, k2t, raw, bvt, prod = FM[0], FM[1], FM[2], FM[3], FM[4], FM[5], FM[6], FM[7], FM[8], FM[9], FM[10]
        waT = FM[12]
        gt, ytok, vtok = TK[0], TK[2], TK[3]

        def shift_ev(dst, cidx):
            def ev(pa):
                k.A(lambda: nc.scalar.copy(out=raw[:, :], in_=pa[:, 0:BLK]), r=[pa], w=[raw])
                k.V(lambda: nc.vector.tensor_scalar(out=dst[:, :], in0=raw[:, :], scalar1=rw_cols[:, 40 + cidx:41 + cidx],
                                                    scalar2=None, op0=ALU.mult), r=[raw, rw_cols], w=[dst])
                k.V(lambda: nc.vector.scalar_tensor_tensor(out=dst[:, 1:BLK], in0=raw[:, 0:BLK - 1],
                                                           scalar=rw_cols[:, cidx:cidx + 1], in1=dst[:, 1:BLK],
                                                           op0=ALU.mult, op1=ALU.add), r=[raw, rw_cols, dst], w=[dst])
                k.V(lambda: nc.vector.scalar_tensor_tensor(out=dst[:, 0:1], in0=rw_carry[:, cidx:cidx + 1],
                                                           scalar=rw_cols[:, cidx:cidx + 1], in1=dst[:, 0:1],
                                                           op0=ALU.mult, op1=ALU.add), r=[rw_carry, rw_cols, dst], w=[dst])
                k.V(lambda: nc.vector.tensor_copy(out=rw_carry[:, cidx:cidx + 1], in_=raw[:, BLK - 1:BLK]),
                    r=[raw], w=[rw_carry])
            return ev

        proj_tok(l, C_RWKV_G, 512, lambda j, g0, w_, pa: k.A(
            lambda: nc.scalar.activation(out=gt[:, j, g0:g0 + w_], in_=pa[:, 0:w_], func=AF.Silu), r=[pa], w=[gt]))
        proj_fm(l, C_RWKV_IN + 12 * 128, 128, shift_ev(waT, 12))
        k.A(lambda: nc.scalar.activation(out=waT[0:64, :], in_=waT[0:64, :], func=AF.Tanh), r=[waT], w=[waT])
        for p in range(4):
            proj_fm(l, C_RWKV_IN + p * 128, 128, shift_ev(xr, p))
            proj_fm(l, C_RWKV_IN + (4 + p) * 128, 128, shift_ev(xk, 4 + p))
            proj_fm(l, C_RWKV_IN + (8 + p) * 128, 128, shift_ev(xv, 8 + p))
            pz = psr.next()
            k.mm(pz[:, 0:BLK], rw_w2a2[0:64, p * 128:(p + 1) * 128], waT[0:64, :], r=[rw_w2a2, waT], w=[pz])
            k.A(lambda: nc.scalar.activation(out=ldt[:, :], in_=pz[:, 0:BLK], func=AF.Sigmoid, bias=rw_cols[:, 16 + p:17 + p],
                                             scale=1.0), r=[pz, rw_cols], w=[ldt])
            k.V(lambda: nc.vector.tensor_scalar(out=ldt[:, :], in0=ldt[:, :], scalar1=-math.exp(-0.5), scalar2=None,
                                                op0=ALU.mult), r=[ldt], w=[ldt])
            pa_ = psr.next()
            k.mm(pa_[:, 0:BLK], rw_w2a2[64:128, p * 128:(p + 1) * 128], waT[64:128, :], r=[rw_w2a2, waT], w=[pa_])
            k.A(lambda: nc.scalar.activation(out=at[:, :], in_=pa_[:, 0:BLK], func=AF.Sigmoid, bias=rw_cols[:, 20 + p:21 + p],
                                             scale=1.0), r=[pa_, rw_cols], w=[at])
            k.V(lambda: nc.vector.tensor_scalar(out=kkt[:, :], in0=xk[:, :], scalar1=rw_cols[:, 24 + p:25 + p], scalar2=None,
                                                op0=ALU.mult), r=[xk, rw_cols], w=[kkt])
            k.V(lambda: nc.vector.tensor_tensor(out=tmpt[:, :], in0=kkt[:, :], in1=kkt[:, :], op=ALU.mult), r=[kkt], w=[tmpt])
            pn = psr.next()
            k.mm(pn[:, 0:BLK], C("blk64"), tmpt[:, :], r=[cst, tmpt], w=[pn])
            k.A(lambda: nc.scalar.sqrt(out=tmpt[:, :], in_=pn[:, 0:BLK]), r=[pn], w=[tmpt])
            k.V(lambda: nc.vector.tensor_scalar(out=tmpt[:, :], in0=tmpt[:, :], scalar1=1e-12, scalar2=None, op0=ALU.max),
                r=[tmpt], w=[tmpt])
            k.V(lambda: nc.vector.reciprocal(out=tmpt[:, :], in_=tmpt[:, :]), r=[tmpt], w=[tmpt])
            k.V(lambda: nc.vector.tensor_tensor(out=kkt[:, :], in0=kkt[:, :], in1=tmpt[:, :], op=ALU.mult), r=[kkt, tmpt], w=[kkt])
            k.V(lambda: nc.vector.tensor_scalar(out=tmpt[:, :], in0=at[:, :], scalar1=rw_cols[:, 28 + p:29 + p], scalar2=-1.0,
                                                op0=ALU.mult, op1=ALU.mult), r=[at, rw_cols], w=[tmpt])
            k.V(lambda: nc.vector.tensor_scalar(out=tmpt[:, :], in0=tmpt[:, :], scalar1=rw_cols[:, 28 + p:29 + p], scalar2=-1.0,
                                                op0=ALU.add, op1=ALU.add), r=[tmpt, rw_cols], w=[tmpt])
            k.V(lambda: nc.vector.scalar_tensor_tensor(out=k2t[:, :], in0=tmpt[:, :], scalar=-1.0, in1=xk[:, :],
                                                       op0=ALU.mult, op1=ALU.mult), r=[tmpt, xk], w=[k2t])
            k.V(lambda: nc.vector.tensor_tensor(out=bvt[:, :], in0=kkt[:, :], in1=at[:, :], op=ALU.mult), r=[kkt, at], w=[bvt])
            k.V(lambda: nc.vector.scalar_tensor_tensor(out=prod[:, :], in0=xr[:, :], scalar=rw_cols[:, 32 + p:33 + p],
                                                       in1=k2t[:, :], op0=ALU.mult, op1=ALU.mult),
                r=[xr, rw_cols, k2t], w=[prod])
            for j in range(NJ):
                js = slice(j * T, (j + 1) * T)
                (cum, epos, eneg, eposx, Bt, Kt, Btok, Ktok, Vt, S0p) = rwt[0:10]
                pb = psr.next()
                k.mm(pb[:, 0:2], prod[:, js], C("headsel"), r=[prod, cst], w=[pb])
                k.A(lambda: nc.scalar.copy(out=rw_bon[:, j, 2 * p:2 * p + 2], in_=pb[:, 0:2]), r=[pb], w=[rw_bon])
                k.V(lambda: nc.vector.tensor_tensor_scan(out=cum[:, :], data0=C("ones"), data1=ldt[:, js], initial=0.0,
                                                         op0=ALU.mult, op1=ALU.add), r=[cst, ldt], w=[cum])
                st = stt.next()
                k.V(lambda: nc.vector.tensor_copy(out=st[:, 0:1], in_=cum[:, 63:64]), r=[cum], w=[st])
                k.V(lambda: nc.vector.tensor_scalar(out=st[:, 1:2], in0=cum[:, 63:64], scalar1=-1.0, scalar2=None,
                                                    op0=ALU.mult), r=[cum], w=[st])
                k.A(lambda: nc.scalar.activation(out=epos[:, :], in_=cum[:, :], func=AF.Exp, bias=st[:, 1:2], scale=1.0),
                    r=[cum, st], w=[epos])
                k.A(lambda: nc.scalar.activation(out=eneg[:, :], in_=cum[:, :], func=AF.Exp, bias=st[:, 0:1], scale=-1.0),
                    r=[cum, st], w=[eneg])
                k.V(lambda: nc.vector.tensor_tensor(out=eposx[:, :], in0=cum[:, :], in1=ldt[:, js], op=ALU.subtract),
                    r=[cum, ldt], w=[eposx])
                k.A(lambda: nc.scalar.activation(out=eposx[:, :], in_=eposx[:, :], func=AF.Exp, bias=st[:, 1:2], scale=1.0),
                    r=[eposx, st], w=[eposx])
                k.A(lambda: nc.scalar.activation(out=st[:, 2:3], in_=st[:, 0:1], func=AF.Exp), r=[st], w=[st])
                k.A(lambda: nc.scalar.activation(out=st[:, 3:4], in_=cum[:, T - 1:T], func=AF.Exp, bias=st[:, 1:2], scale=1.0),
                    r=[cum, st], w=[st])
                k.V(lambda: nc.vector.scalar_tensor_tensor(out=rw_AR[:, 0:T], in0=kkt[:, js], scalar=-1.0, in1=eposx[:, :],
                                                           op0=ALU.mult, op1=ALU.mult), r=[kkt, eposx], w=[rw_AR])
                k.V(lambda: nc.vector.tensor_tensor(out=rw_AR[:, T:2 * T], in0=xr[:, js], in1=epos[:, :], op=ALU.mult),
                    r=[xr, epos], w=[rw_AR])
                k.V(lambda: nc.vector.tensor_tensor(out=Bt[:, :], in0=bvt[:, js], in1=eneg[:, :], op=ALU.mult),
                    r=[bvt, eneg], w=[Bt])
                k.V(lambda: nc.vector.tensor_tensor(out=Kt[:, :], in0=k2t[:, js], in1=eneg[:, :], op=ALU.mult),
                    r=[k2t, eneg], w=[Kt])
                k.V(lambda: nc.vector.tensor_scalar(out=S0p[:, 0:64], in0=rw_S[p][:, :], scalar1=st[:, 2:3], scalar2=None,
                                                    op0=ALU.mult), r=[rw_S[p], st], w=[S0p])
                tr(Btok[:, :], Btok, f32(Bt[:, :]), Bt, 128, 128, eng="act")
                tr(Ktok[:, :], Ktok, f32(Kt[:, :]), Kt, 128, 128, eng="dve")
                tr(Vt[:, :], Vt, xv[:, js], xv, 128, 128, eng="act")
                k.A(lambda: nc.scalar.copy(out=vtok[:, j, p * 128:(p + 1) * 128], in_=f32(Vt[:, :])), r=[Vt], w=[vtok])
                HS = []
                for hh in range(2):
                    r0 = 64 * hh
                    d = dict(rs=slice(r0, r0 + 64), X=rwt[10 + 8 * hh], XT=rwt[11 + 8 * hh], Xn=rwt[12 + 8 * hh],
                             XTn=rwt[13 + 8 * hh], Z=rwt[14 + 8 * hh], Zn=rwt[15 + 8 * hh], Mbr=rwt[16 + 8 * hh],
                             Nak=rwt[17 + 8 * hh], Mkr=rwx[hh], hh=hh)
                    HS.append(d)
                for d in HS:
                    rs = d["rs"]
                    pN = psr.next()
                    k.mm(pN[:, 0:2 * T], Bt[rs, :], rw_AR[rs, :], r=[Bt, rw_AR], w=[pN], fast=FAST_RW)
                    pK = psr.next()
                    k.mm(pK[:, 0:2 * T], Kt[rs, :], rw_AR[rs, :], r=[Kt, rw_AR], w=[pK], fast=FAST_RW)
                    pX = psr.next()
                    k.mm(pX[:, 0:T], rw_AR[rs, 0:T], Bt[rs, :], r=[Bt, rw_AR], w=[pX], fast=FAST_RW)
                    k.V(lambda: nc.vector.tensor_tensor(out=d["X"][:, :], in0=pN[:, 0:T], in1=C("maskS"), op=ALU.mult),
                        r=[pN, cst], w=[d["X"]])
                    k.V(lambda: nc.vector.tensor_tensor(out=d["Mbr"][:, :], in0=pN[:, T:2 * T], in1=C("maskT"), op=ALU.mult),
                        r=[pN, cst], w=[d["Mbr"]])
                    k.V(lambda: nc.vector.tensor_tensor(out=d["Nak"][:, :], in0=pK[:, 0:T], in1=C("maskS"), op=ALU.mult),
                        r=[pK, cst], w=[d["Nak"]])
                    k.V(lambda: nc.vector.tensor_tensor(out=d["Mkr"][:, :], in0=pK[:, T:2 * T], in1=C("maskT"), op=ALU.mult),
                        r=[pK, cst], w=[d["Mkr"]])
                    k.V(lambda: nc.vector.tensor_tensor(out=d["XT"][:, :], in0=pX[:, 0:T], in1=C("maskL"), op=ALU.mult),
                        r=[pX, cst], w=[d["XT"]])
                for d in HS:
                    rs = d["rs"]
                    pW = psr.next()
                    k.mm(pW[:, 0:64], rw_AR[rs, 0:T], S0p[rs, 0:64], start=True, stop=False, r=[rw_AR, S0p], w=[pW], fast=FAST_RW)
                    k.mm(pW[:, 0:64], d["Nak"][:, :], Vt[:, rs], start=False, stop=True, r=[d["Nak"], Vt], w=[pW], fast=FAST_RW)
                    k.A(lambda: nc.scalar.copy(out=d["Z"][:, 0:64], in_=pW[:, 0:64]), r=[pW], w=[d["Z"]])
                for lev in range(7):
                    for d in HS:
                        X, XT, Xn, XTn, Z, Zn = d["X"], d["XT"], d["Xn"], d["XTn"], d["Z"], d["Zn"]
                        pZ = psr.next()
                        k.mm(pZ[:, 0:64], X[:, :], Z[:, 0:64], r=[X, Z], w=[pZ], fast=FAST_RW)
                        k.V(lambda: nc.vector.tensor_tensor(out=Zn[:, 0:64], in0=f32(Z[:, 0:64]), in1=pZ[:, 0:64], op=ALU.add),
                            r=[Z, pZ], w=[Zn])
                        d["Z"], d["Zn"] = Zn, Z
                        if lev < 6:
                            p1 = psr.next()
                            k.mm(p1[:, 0:T], XT[:, :], X[:, :], r=[XT, X], w=[p1], fast=FAST_RW)
                            p2 = psr.next()
                            k.mm(p2[:, 0:T], X[:, :], XT[:, :], r=[XT, X], w=[p2], fast=FAST_RW)
                            k.A(lambda: nc.scalar.copy(out=Xn[:, :], in_=p1[:, 0:T]), r=[p1], w=[Xn])
                            if d["hh"] == 0:
                                k.V(lambda: nc.vector.tensor_copy(out=XTn[:, :], in_=p2[:, 0:T]), r=[p2], w=[XTn])
                            else:
                                k.A(lambda: nc.scalar.copy(out=XTn[:, :], in_=p2[:, 0:T]), r=[p2], w=[XTn])
                            d["X"], d["Xn"] = Xn, X
                            d["XT"], d["XTn"] = XTn, XT
                for d in HS:
                    rs = d["rs"]
                    U = d["Z"]
                    pY = psr.next()
                    k.mm(pY[:, 0:64], rw_AR[rs, T:2 * T], S0p[rs, 0:64], start=True, stop=False, r=[rw_AR, S0p], w=[pY], fast=FAST_RW)
                    k.mm(pY[:, 0:64], d["Mbr"][:, :], U[:, 0:64], start=False, stop=False, r=[d["Mbr"], U], w=[pY], fast=FAST_RW)
                    k.mm(pY[:, 0:64], d["Mkr"][:, :], Vt[:, rs], start=False, stop=True, r=[d["Mkr"], Vt], w=[pY], fast=FAST_RW)
                    hcol = (2 * p + d["hh"]) * 64
                    k.A(lambda: nc.scalar.copy(out=ytok[:, j, hcol:hcol + 64], in_=pY[:, 0:64]), r=[pY], w=[ytok])
                    pS = psr.next()
                    k.mm(pS[:, 0:64], Btok[:, :], U[:, 0:64], start=True, stop=False, r=[Btok, U], w=[pS], fast=FAST_RW)
                    k.mm(pS[:, 0:64], Ktok[:, :], Vt[:, rs], start=False, stop=True, r=[Ktok, Vt], w=[pS], fast=FAST_RW)
                    k.V(lambda: nc.vector.tensor_tensor(out=rw_tmpS[rs, 0:64], in0=f32(S0p[rs, 0:64]), in1=pS[rs, 0:64], op=ALU.add),
                        r=[S0p, pS], w=[rw_tmpS])
                    k.V(lambda: nc.vector.tensor_scalar(out=rw_S[p][rs, :], in0=rw_tmpS[rs, 0:64], scalar1=st[rs, 3:4],
                                                        scalar2=None, op0=ALU.mult), r=[rw_tmpS, st], w=[rw_S[p]])
        for j in range(NJ):
            st = stt.next()
            y3 = ytok[:, j, :].rearrange("p (h d) -> p h d", h=8)
            k.V(lambda: nc.vector.tensor_reduce(out=st[:, 0:8], in_=y3, axis=AX.X, op=ALU.add), r=[ytok], w=[st])
            k.A(lambda: nc.scalar.activation(out=junk[:, 0:512], in_=ytok[:, j, :], func=AF.Square), r=[ytok], w=[junk])
            k.V(lambda: nc.vector.tensor_reduce(out=st[:, 8:16], in_=junk[:, 0:512].rearrange("p (h d) -> p h d", h=8),
                                                axis=AX.X, op=ALU.add), r=[junk], w=[st])
            k.V(lambda: nc.vector.tensor_scalar(out=st[:, 0:8], in0=st[:, 0:8], scalar1=1.0 / 64, scalar2=None, op0=ALU.mult),
                r=[st], w=[st])
            k.V(lambda: nc.vector.tensor_tensor(out=st[:, 16:24], in0=st[:, 0:8], in1=st[:, 0:8], op=ALU.mult), r=[st], w=[st])
            k.V(lambda: nc.vector.scalar_tensor_tensor(out=st[:, 8:16], in0=st[:, 8:16], scalar=1.0 / 64, in1=st[:, 16:24],
                                                       op0=ALU.mult, op1=ALU.subtract), r=[st], w=[st])
            k.V(lambda: nc.vector.tensor_scalar(out=st[:, 8:16], in0=st[:, 8:16], scalar1=64e-5, scalar2=None, op0=ALU.add),
                r=[st], w=[st])
            k.A(lambda: nc.scalar.sqrt(out=st[:, 8:16], in_=st[:, 8:16]), r=[st], w=[st])
            k.V(lambda: nc.vector.reciprocal(out=st[:, 24:32], in_=st[:, 8:16]), r=[st], w=[st])
            o = obr.next()
            o3 = o[:, :].rearrange("p (h d) -> p h d", h=8)
            k.V(lambda: nc.vector.tensor_tensor(out=o3, in0=y3, in1=st[:, 0:8].unsqueeze(2).to_broadcast([128, 8, 64]),
                                                op=ALU.subtract), r=[ytok, st], w=[o])
            k.V(lambda: nc.vector.tensor_tensor(out=o3, in0=o3, in1=st[:, 24:32].unsqueeze(2).to_broadcast([128, 8, 64]),
                                                op=ALU.mult), r=[o, st], w=[o])
            k.V(lambda: nc.vector.tensor_tensor(out=o[:, :], in0=o[:, :], in1=rw_lng[:, :], op=ALU.mult), r=[o, rw_lng], w=[o])
            k.V(lambda: nc.vector.tensor_tensor(out=o[:, :], in0=o[:, :], in1=rw_lnb[:, :], op=ALU.add), r=[o, rw_lnb], w=[o])
            k.V(lambda: nc.vector.tensor_tensor(out=junk[:, 0:512].rearrange("p (h d) -> p h d", h=8),
                                                in0=vtok[:, j, :].rearrange("p (h d) -> p h d", h=8),
                                                in1=rw_bon[:, j, :].unsqueeze(2).to_broadcast([128, 8, 64]), op=ALU.mult),
                r=[vtok, rw_bon], w=[junk])
            k.V(lambda: nc.vector.tensor_tensor(out=o[:, :], in0=o[:, :], in1=junk[:, 0:512], op=ALU.add), r=[o, junk], w=[o])
            k.V(lambda: nc.vector.tensor_tensor(out=o[:, :], in0=o[:, :], in1=gt[:, j, :], op=ALU.mult), r=[o, gt], w=[o])
            emit_out(3, l, blk, j, o, 512)

    gsb = Ring([k.sb([128, 512], name="gsb") for _ in range(2)])
    fng = k.sb([128, D], name="fng")
    UP = ["w_up_ret", "w_up_gla", "w_up_ssd", "w_up_rwkv", "w_up_mem"]

    def merge_block(l, blk, last):
        t0 = blk * BLK
        mh = [TK[0], TK[1]]
        first = True
        for bi in range(5):
            if BR[bi] not in branches:
                continue
            nr = 2 if bi == 4 else 4
            for hf in range(2):
                wg = load_w(dr["w_in"][l], C_GATES + bi * 1024 + hf * 512, 512)
                wu = load_w(dr[UP[bi]][l], hf * 512, 512, rows=nr)
                for j in range(NJ):
                    pg = psr.next()
                    for dc in range(8):
                        k.mm(pg[:, :], hT[:, dc, j * T:(j + 1) * T], wg[:, dc, :], start=(dc == 0), stop=(dc == 7),
                             r=[hT, wg], w=[pg], fast=True)
                    gs = gsb.next()
                    k.A(lambda: nc.scalar.activation(out=gs[:, :], in_=pg[:, :], func=AF.Sigmoid), r=[pg], w=[gs])
                    pu = psr.next()
                    for c in range(nr):
                        k.mm(pu[:, :], oT[bi][:, c, j * T:(j + 1) * T], wu[:, c, :], start=(c == 0), stop=(c == nr - 1),
                             r=[oT[bi], wu], w=[pu], fast=True)
                    if first:
                        k.V(lambda: nc.vector.tensor_tensor(out=mh[hf][:, j, :], in0=gs[:, :], in1=pu[:, :], op=ALU.mult),
                            r=[gs, pu], w=[mh[hf]])
                    else:
                        k.V(lambda: nc.vector.tensor_tensor(out=gs[:, :], in0=gs[:, :], in1=pu[:, :], op=ALU.mult),
                            r=[gs, pu], w=[gs])
                        k.V(lambda: nc.vector.tensor_tensor(out=mh[hf][:, j, :], in0=mh[hf][:, j, :], in1=gs[:, :], op=ALU.add),
                            r=[gs, mh[hf]], w=[mh[hf]])
            first = False
        if debug is not None and l == dbg_layer:
            for j in range(NJ):
                for hf in range(2):
                    k.dma("pool", dbg_d[t0 + j * T:t0 + (j + 1) * T, 2304 + hf * 512:2304 + (hf + 1) * 512], mh[hf][:, j, :],
                          r=[mh[hf]])
        for j in range(NJ):
            for hf in range(2):
                for c in range(4):
                    tr(hT[:, hf * 4 + c, j * T:(j + 1) * T], hT, mh[hf][:, j, c * 128:(c + 1) * 128], mh[hf], 128, 128,
                       eng=("act" if c % 2 == 0 else "dve"))
        for hf in range(2):
            wo = load_w(dr["w_out"][l], hf * 512, 512)
            for j in range(NJ):
                po = psr.next()
                for dc in range(8):
                    k.mm(po[:, :], hT[:, dc, j * T:(j + 1) * T], wo[:, dc, :], start=(dc == 0), stop=(dc == 7),
                         r=[hT, wo], w=[po], fast=True)
                k.V(lambda: nc.vector.tensor_tensor(out=xb[:, j, hf * 512:(hf + 1) * 512], in0=xb[:, j, hf * 512:(hf + 1) * 512],
                                                    in1=po[:, :], op=ALU.add), r=[xb, po], w=[xb])
        if debug is not None and l == dbg_layer:
            for j in range(NJ):
                k.dma("pool", dbg_d[t0 + j * T:t0 + (j + 1) * T, 3328:4352], xb[:, j, :], r=[xb])
        if pipe:
            return
        if not last:
            k.dma("pool", x1_d[t0:t0 + BLK, :].rearrange("(j p) d -> p j d", p=128), xb[:, :, :], r=[xb], w=[x1_b[blk]])
        else:
            for j in range(NJ):
                st = stt.next()
                k.A(lambda: nc.scalar.activation(out=junk[:, :], in_=xb[:, j, :], func=AF.Square, accum_out=st[:, 0:1]),
                    r=[xb], w=[junk, st])
                k.V(lambda: nc.vector.tensor_scalar(out=st[:, 1:2], in0=st[:, 0:1], scalar1=1.0 / D, scalar2=1e-6,
                                                    op0=ALU.mult, op1=ALU.add), r=[st], w=[st])
                k.A(lambda: nc.scalar.sqrt(out=st[:, 1:2], in_=st[:, 1:2]), r=[st], w=[st])
                k.V(lambda: nc.vector.reciprocal(out=st[:, 2:3], in_=st[:, 1:2]), r=[st], w=[st])
                k.V(lambda: nc.vector.scalar_tensor_tensor(out=xb[:, j, :], in0=xb[:, j, :], scalar=st[:, 2:3], in1=fng[:, :],
                                                           op0=ALU.mult, op1=ALU.mult), r=[xb, st, fng], w=[xb])
            k.dma("pool", out_d[t0:t0 + BLK, :].rearrange("(j p) d -> p j d", p=128), xb[:, :, :], r=[xb])

    BR = ["ret", "gla", "ssd", "rwkv", "mem"]
    k.dma("sp", fng[:, :], bc(dr["final_norm_g"][0:1, :], D), w=[fng])

    def front(l, pos0):
        st = ss
        for j in range(NJ):
            k.A(lambda: nc.scalar.activation(out=junk[:, :], in_=xb[:, j, :], func=AF.Square,
                                             accum_out=st[:, j:j + 1]), r=[xb], w=[junk, st])
        k.V(lambda: nc.vector.tensor_scalar(out=st[:, 4:4 + NJ], in0=st[:, 0:NJ], scalar1=1.0 / D, scalar2=1e-6,
                                            op0=ALU.mult, op1=ALU.add), r=[st], w=[st])
        k.A(lambda: nc.scalar.sqrt(out=st[:, 4:4 + NJ], in_=st[:, 4:4 + NJ]), r=[st], w=[st])
        k.V(lambda: nc.vector.reciprocal(out=st[:, 8:8 + NJ], in_=st[:, 4:4 + NJ]), r=[st], w=[st])
        for j in range(NJ):
            k.V(lambda: nc.vector.tensor_scalar(out=xn[:, :], in0=xb[:, j, :], scalar1=st[:, 8 + j:9 + j],
                                                scalar2=None, op0=ALU.mult), r=[xb, st], w=[xn])
            for half in range(2):
                pa = psr.next()
                for q in range(4):
                    dc = half * 4 + q
                    k.op("pe", lambda: nc.tensor.transpose(pa[:, q * T:(q + 1) * T], xn[:, dc * T:(dc + 1) * T], ident),
                         r=[xn, cst], w=[pa])
                for q in range(4):
                    dc = half * 4 + q
                    k.A(lambda: nc.scalar.activation(out=hT[:, dc, j * T:(j + 1) * T], in_=pa[:, q * T:(q + 1) * T],
                                                     func=AF.Identity, scale=gcol[:, dc:dc + 1]), r=[pa, gcol], w=[hT])
        k.dma("sp", posi[:, :], dr["positions"][:, pos0:pos0 + BLK], w=[posi])
        k.V(lambda: nc.vector.tensor_copy(out=posf[:, :], in_=posi[:, :]), r=[posi], w=[posf])
        pa = psr.next()
        k.mm(pa[0:64, 0:BLK], C("invrow", 0, 1), posf[0:1, :], r=[cst, posf], w=[pa])
        for tab, shift in ((sinT, math.pi), (cosT, 1.5 * math.pi)):
            k.V(lambda: nc.vector.tensor_scalar(out=tab[:, :], in0=pa[0:64, 0:BLK], scalar1=shift, scalar2=None,
                                                op0=ALU.add), r=[pa], w=[tab])
            k.V(lambda: nc.vector.tensor_scalar(out=rope_qi[:, :], in0=tab[:, :], scalar1=1.0 / TWO_PI, scalar2=None,
                                                op0=ALU.mult), r=[tab], w=[rope_qi])
            k.V(lambda: nc.vector.tensor_copy(out=rope_qf[:, :], in_=rope_qi[:, :]), r=[rope_qi], w=[rope_qf])
            k.V(lambda: nc.vector.scalar_tensor_tensor(out=tab[:, :], in0=rope_qf[:, :], scalar=-TWO_PI, in1=tab[:, :],
                                                       op0=ALU.mult, op1=ALU.add), r=[rope_qf, tab], w=[tab])
            k.V(lambda: nc.vector.tensor_scalar(out=rope_qf[:, :], in0=tab[:, :], scalar1=0.0, scalar2=TWO_PI,
                                                op0=ALU.is_lt, op1=ALU.mult), r=[tab], w=[rope_qf])
            k.V(lambda: nc.vector.tensor_tensor(out=tab[:, :], in0=tab[:, :], in1=rope_qf[:, :], op=ALU.add),
                r=[tab, rope_qf], w=[tab])
            k.V(lambda: nc.vector.tensor_scalar(out=tab[:, :], in0=tab[:, :], scalar1=0.0, scalar2=TWO_PI,
                                                op0=ALU.max, op1=ALU.min), r=[tab], w=[tab])
            k.A(lambda: nc.scalar.activation(out=tab[:, :], in_=tab[:, :], func=AF.Sin, bias=pi_c[0:64, :], scale=1.0),
                r=[tab, pi_c], w=[tab])

    def branches_and_merge(l, blk, last):
        if "ret" in branches:
            ret_block(l, blk)
        if "gla" in branches:
            gla_block(l, blk)
        if "ssd" in branches:
            ssd_block(l, blk)
        if "rwkv" in branches:
            rwkv_block(l, blk)
        if "mem" in branches:
            mem_block(l, blk)
        if do_merge:
            merge_block(l, blk, last=last)

    if not pipe:
        for l in range(nlayers):
            load_params(l)
            for blk in range(nblk):
                t0 = blk * BLK
                src = dr["x"] if l == 0 else x1_d
                rb = [] if l == 0 else [x1_b[blk]]
                k.dma("sp", xb[:, :, :], src[t0:t0 + BLK, :].rearrange("(j p) d -> p j d", p=128), r=rb, w=[xb])
                front(l, t0)
                branches_and_merge(l, blk, last=(l == nlayers - 1))
    else:
        role = k.sb([128, 16], name="role")
        k.dma("sp", role[:, :], role_d[:, :], w=[role])
        load_params(0)
        k.V(lambda: nc.vector.memset(xb[:, :, :], 0.0), w=[xb])
        tmpX = [(TK[0], TK[1]), (TK[2], TK[3])]
        for it in range(nblk + 1):
            for s_ in range(4):
                ta = tmpX[s_ % 2]
                for hf in range(2):
                    eng = k.V if hf == 0 else k.G
                    e_ = nc.vector if hf == 0 else nc.gpsimd
                    eng(lambda: e_.tensor_scalar(out=ta[hf][:, :, :], in0=xb[:, :, hf * 512:(hf + 1) * 512],
                                                 scalar1=role[:, 1 + s_:2 + s_], scalar2=None, op0=ALU.mult),
                        r=[xb, role], w=[ta[hf]])
                    k.dma("pool", cin_d[s_ * BLK:(s_ + 1) * BLK, hf * 512:(hf + 1) * 512].rearrange("(j p) d -> p j d", p=128),
                          ta[hf][:, :, :], r=[ta[hf]], w=[cin_b])
            k.collective(cin_d.opt(), cout_d.opt(), r=[cin_b], w=[cout_b])
            ba = min(it, nblk - 1)
            k.dma("sp", xb[:, :, :], dr["x"][ba * BLK:(ba + 1) * BLK, :].rearrange("(j p) d -> p j d", p=128), w=[xb])
            k.V(lambda: nc.vector.tensor_scalar(out=xb[:, :, :], in0=xb[:, :, :], scalar1=role[:, 0:1], scalar2=None,
                                                op0=ALU.mult), r=[xb, role], w=[xb])
            for s_ in range(4):
                ta = tmpX[s_ % 2]
                for hf in range(2):
                    k.dma("sp", ta[hf][:, :, :],
                          cout_d[s_ * BLK:(s_ + 1) * BLK, hf * 512:(hf + 1) * 512].rearrange("(j p) d -> p j d", p=128),
                          r=[cout_b], w=[ta[hf]])
                    k.V(lambda: nc.vector.scalar_tensor_tensor(out=xb[:, :, hf * 512:(hf + 1) * 512], in0=ta[hf][:, :, :],
                                                               scalar=role[:, 5 + s_:6 + s_],
                                                               in1=xb[:, :, hf * 512:(hf + 1) * 512],
                                                               op0=ALU.mult, op1=ALU.add), r=[ta[hf], role, xb], w=[xb])
            front(0, it * BLK)
            branches_and_merge(0, it, last=False)
            if it == 0:
                for s_ in ret_S + gla_S + rw_S + [ssd_S]:
                    np_ = s_.t.shape[0]
                    k.V(lambda: nc.vector.tensor_scalar(out=s_[:, :], in0=s_[:, :], scalar1=role[0:np_, 9:10], scalar2=None,
                                                        op0=ALU.mult), r=[s_, role], w=[s_])
            ob = max(it - 1, 0)
            on = tmpX[1]
            for j in range(NJ):
                st = stt.next()
                k.A(lambda: nc.scalar.activation(out=junk[:, :], in_=xb[:, j, :], func=AF.Square, accum_out=st[:, 0:1]),
                    r=[xb], w=[junk, st])
                k.V(lambda: nc.vector.tensor_scalar(out=st[:, 1:2], in0=st[:, 0:1], scalar1=1.0 / D, scalar2=1e-6,
                                                    op0=ALU.mult, op1=ALU.add), r=[st], w=[st])
                k.A(lambda: nc.scalar.sqrt(out=st[:, 1:2], in_=st[:, 1:2]), r=[st], w=[st])
                k.V(lambda: nc.vector.reciprocal(out=st[:, 2:3], in_=st[:, 1:2]), r=[st], w=[st])
                for hf in range(2):
                    k.V(lambda: nc.vector.scalar_tensor_tensor(out=on[hf][:, j, :], in0=xb[:, j, hf * 512:(hf + 1) * 512],
                                                               scalar=st[:, 2:3], in1=fng[:, hf * 512:(hf + 1) * 512],
                                                               op0=ALU.mult, op1=ALU.mult), r=[xb, st, fng], w=[on[hf]])
            for hf in range(2):
                k.dma("pool", out_d[ob * BLK:(ob + 1) * BLK, hf * 512:(hf + 1) * 512].rearrange("(j p) d -> p j d", p=128),
                      on[hf][:, :, :], r=[on[hf]], w=[out_b[ob]])
    k.finish()
    return nc, k


NCORES = 4
PIPE = False


def make_in_maps(inputs, nblk=SEQ // BLK):
    ntok = nblk * BLK
    maps = []
    if not PIPE:
        params = {n: np.ascontiguousarray(np.asarray(inputs[n], np.float32).reshape(SHAPES[n])) for n in PARAM_NAMES}
        for c in range(4):
            m = {"x": np.ascontiguousarray(inputs["x"][c]), "mem": np.ascontiguousarray(inputs["mem"][c]),
                 "positions": np.ascontiguousarray(inputs["positions"][c:c + 1]).astype(np.int32), "cst": CST}
            m.update(params)
            maps.append(m)
        return maps
    per_stage = []
    for stg in range(2):
        p = {}
        for n in PARAM_NAMES:
            a = np.asarray(inputs[n], np.float32).reshape(SHAPES[n])
            if n != "final_norm_g":
                a = a[stg:stg + 1]
            p[n] = np.ascontiguousarray(a)
        per_stage.append(p)
    for c in range(8):
        b, stg = c % 4, c // 4
        pos = np.asarray(inputs["positions"][b], np.int32)[:ntok]
        if stg == 0:
            pos2 = np.concatenate([pos, pos[ntok - BLK:ntok]])
        else:
            pos2 = np.concatenate([np.zeros(BLK, np.int32), pos])
        role = np.zeros((128, 16), np.float32)
        if stg == 0:
            role[:, 0] = 1.0
            role[:, 1 + b] = 1.0
            role[:, 9] = 1.0
        else:
            role[:, 5 + b] = 1.0
        m = {"x": np.ascontiguousarray(inputs["x"][b][:ntok]), "mem": np.ascontiguousarray(inputs["mem"][b]),
             "positions": np.ascontiguousarray(pos2[None, :]), "cst": CST, "role": role}
        m.update(per_stage[stg])
        maps.append(m)
    return maps


def kernel(**inputs):
    if PIPE:
        nc, k = build(nlayers=1, pipe=True)
        maps = make_in_maps(inputs)
        res = run_bass_kernel_spmd(nc, maps, core_ids=list(range(8)))
        out = np.stack([np.asarray(res.results[4 + b]["out"]) for b in range(4)], axis=0)
    else:
        nc, k = build()
        maps = make_in_maps(inputs)
        res = run_bass_kernel_spmd(nc, maps, core_ids=list(range(4)))
        out = np.stack([np.asarray(res.results[b]["out"]) for b in range(4)], axis=0)
    return out.astype(np.float32)
```

```python
import contextlib
import math
import numpy as np
import concourse.bass as bass
import concourse.mybir as mybir
from concourse.bass_utils import run_bass_kernel_spmd

F32 = mybir.dt.float32
F32R = mybir.dt.float32r
FAST = True
FAST_RW = True
I32 = mybir.dt.int32
AF = mybir.ActivationFunctionType
ALU = mybir.AluOpType
AX = mybir.AxisListType

SEQ = 4096
D = 1024
T = 128
BLK = 256
NJ = BLK // T
IN_TOTAL = 12184
C_RET_Q, C_RET_K, C_RET_V, C_RET_G = 0, 256, 512, 1024
C_GLA_Q, C_GLA_K, C_GLA_V, C_GLA_GK, C_GLA_G = 1536, 1792, 2048, 2560, 2576
C_SSD_XBC, C_SSD_DT, C_SSD_Z = 3088, 4112, 4120
C_RWKV_IN, C_RWKV_G, C_MEM_Q, C_GATES = 4632, 6296, 6808, 7064
SEM_LIMIT = 30000
TWO_PI = 2.0 * math.pi


class Buf:
    __slots__ = ("w", "r")

    def __init__(self):
        self.w = None
        self.r = {}


class Tl:
    def __init__(self, t):
        self.t = t
        self.b = Buf()

    def __getitem__(self, k):
        return self.t[k]


def _bufs(lst):
    out = []
    for x in lst:
        if x is None:
            continue
        out.append(x.b if isinstance(x, Tl) else x)
    return out


class KB:
    def __init__(self, nc):
        self.nc = nc
        self.es = contextlib.ExitStack()
        self.eng = {"pe": nc.tensor, "dve": nc.vector, "act": nc.scalar, "pool": nc.gpsimd, "sp": nc.sync}
        self.nsem = 0
        self.sem = {}
        self.cnt = {}
        for e in ("pe", "dve", "act", "pool"):
            self.sem[e] = self.newsem(e)
            self.cnt[e] = 0
        self.seen = {e: {} for e in self.eng}
        self.NS = 8
        self.dsem = {q: [self.newsem("d" + q) for _ in range(self.NS)] for q in ("sp", "pool")}
        self.dval = {q: [0] * self.NS for q in ("sp", "pool")}
        self.drr = {q: 0 for q in ("sp", "pool")}
        self.ntile = 0
        self.ninst = 0

    def newsem(self, name):
        self.nsem += 1
        return self.es.enter_context(self.nc.semaphore(f"{name}_{self.nsem}"))

    def sb(self, shape, dtype=F32, name=None):
        self.ntile += 1
        return Tl(self.es.enter_context(self.nc.sbuf_tensor(f"{name or 'sb'}_{self.ntile}", list(shape), dtype)))

    def ps(self, shape=(128, 512), dtype=F32, name=None):
        self.ntile += 1
        return Tl(self.es.enter_context(self.nc.psum_tensor(f"{name or 'ps'}_{self.ntile}", list(shape), dtype)))

    def _collect(self, e, reads, writes):
        need = {}

        def add(tok):
            if tok is None:
                return
            sem, val, te = tok
            if te == "pe" and e == "pe":
                return
            if self.seen[e].get(sem, 0) >= val:
                return
            if need.get(sem, 0) < val:
                need[sem] = val

        for b in reads:
            add(b.w)
        for b in writes:
            add(b.w)
            for tok in b.r.values():
                add(tok)
        return need

    def _mark(self, tok, reads, writes, e):
        for b in reads:
            b.r[e] = tok
        for b in writes:
            b.w = tok
            b.r = {}

    def op(self, e, emit, r=(), w=()):
        reads, writes = _bufs(r), _bufs(w)
        need = self._collect(e, reads, writes)
        items = list(need.items())
        eng = self.eng[e]
        for sem, val in items[:-1]:
            eng.wait_ge(sem, val)
            self.seen[e][sem] = val
        ins = emit()
        if items:
            sem, val = items[-1]
            ins._wait_ge(sem, val)
            self.seen[e][sem] = val
        if self.cnt[e] >= SEM_LIMIT:
            self.sem[e] = self.newsem(e)
            self.cnt[e] = 0
        self.cnt[e] += 1
        ins.then_inc(self.sem[e], 1)
        self.ninst += 1
        self._mark((self.sem[e], self.cnt[e], e), reads, writes, e)
        return ins

    def dma(self, q, out, in_, r=(), w=(), **kw):
        reads, writes = _bufs(r), _bufs(w)
        need = self._collect(q, reads, writes)
        slot = self.drr[q] % self.NS
        self.drr[q] += 1
        sem = self.dsem[q][slot]
        prev = self.dval[q][slot]
        if prev > 0 and self.seen[q].get(sem, 0) < prev:
            need[sem] = max(need.get(sem, 0), prev)
        eng = self.eng[q]
        for s, v in need.items():
            eng.wait_ge(s, v)
            self.seen[q][s] = v
        eng.dma_start(out=out, in_=in_, **kw).then_inc(sem, 16)
        self.dval[q][slot] = prev + 16
        self.ninst += 1
        self._mark((sem, prev + 16, "dma"), reads, writes, "dma_" + q + str(slot))

    def collective(self, in_ap, out_ap, r=(), w=()):
        e = "pool"
        reads, writes = _bufs(r), _bufs(w)
        need = self._collect(e, reads, writes)
        eng = self.eng[e]
        for sem, val in need.items():
            eng.wait_ge(sem, val)
            self.seen[e][sem] = val
        if not hasattr(self, "cc_sem"):
            self.cc_sem = self.newsem("cc")
            self.cc_cnt = 0
        ins = self.nc.gpsimd.collective_compute("AllReduce", ALU.add, replica_groups=[list(range(8))],
                                                ins=[in_ap], outs=[out_ap])
        self.cc_cnt += 1
        ins.then_inc(self.cc_sem)
        self.ninst += 1
        self._mark((self.cc_sem, self.cc_cnt, "cc"), reads, writes, "cc")

    def finish(self):
        sp = self.nc.sync
        for q in ("sp", "pool"):
            for s, v in zip(self.dsem[q], self.dval[q]):
                if v > 0:
                    sp.wait_ge(s, v)
        for e in ("pe", "dve", "act", "pool"):
            if self.cnt[e] > 0:
                sp.wait_ge(self.sem[e], self.cnt[e])
        if hasattr(self, "cc_sem"):
            sp.wait_ge(self.cc_sem, self.cc_cnt)

    def mm(self, out, lhsT, rhs, start=True, stop=True, r=(), w=(), fast=False):
        nc = self.nc
        if fast and FAST:
            assert lhsT.dtype == F32R and rhs.dtype == F32R
        else:
            if lhsT.dtype == F32R:
                lhsT = lhsT.bitcast(F32)
            if rhs.dtype == F32R:
                rhs = rhs.bitcast(F32)
        return self.op("pe", lambda: nc.tensor.matmul(out, lhsT, rhs, start=start, stop=stop), r, w)

    def V(self, fn, r=(), w=()):
        return self.op("dve", fn, r, w)

    def A(self, fn, r=(), w=()):
        return self.op("act", fn, r, w)

    def G(self, fn, r=(), w=()):
        return self.op("pool", fn, r, w)


class Ring:
    def __init__(self, tiles):
        self.tiles = tiles
        self.i = 0

    def next(self):
        t = self.tiles[self.i % len(self.tiles)]
        self.i += 1
        return t


def make_consts():
    cols = {}
    parts = []
    off = [0]

    def add(name, arr):
        arr = np.asarray(arr, np.float32)
        assert arr.shape[0] == 128
        arr = arr.reshape(128, -1)
        cols[name] = (off[0], arr.shape[1])
        parts.append(arr)
        off[0] += arr.shape[1]

    i = np.arange(128)
    add("ident", np.eye(128))
    add("maskT", (i[:, None] <= i[None, :]))
    add("maskS", (i[:, None] < i[None, :]))
    add("maskL", (i[:, None] > i[None, :]))
    add("ones", np.ones((128, 128)))
    add("negm", np.where(i[:, None] > i[None, :], -30000.0, 0.0))
    inv = 1.0 / (10000.0 ** np.linspace(0.0, 1.0, 32, dtype=np.float32))
    invrow = np.zeros((128, 64), np.float32)
    invrow[0, :] = np.repeat(inv.astype(np.float32), 2)
    add("invrow", invrow)
    rot = np.zeros((128, 64), np.float32)
    for p in range(32):
        rot[2 * p + 1, 2 * p] = -1.0
        rot[2 * p, 2 * p + 1] = 1.0
    add("rot", rot)
    lg = np.log(1.0 - 2.0 ** (-5.0 - np.arange(4, dtype=np.float64)))
    dec = np.zeros((128, 4, 128))
    for h in range(4):
        dec[:, h, :] = np.where(i[:, None] <= i[None, :], np.exp(lg[h] * (i[None, :] - i[:, None])), 0.0)
    add("retdec", dec)
    qs = np.zeros((128, 4, 128))
    for h in range(4):
        qs[:, h, :] = np.exp(lg[h] * (i[None, :] + 1))
    add("retqs", qs)
    ks = np.zeros((128, 4))
    for h in range(4):
        ks[:, h] = np.exp(lg[h] * (127 - i))
    add("retks", ks)
    bo = np.zeros((128, 128))
    bo[:64, :64] = 1
    bo[64:, 64:] = 1
    add("blk64", bo)
    hs = np.zeros((128, 2))
    hs[:64, 0] = 1
    hs[64:, 1] = 1
    add("headsel", hs)
    return np.concatenate(parts, axis=1), cols, [float(np.exp(lg[h] * 128)) for h in range(4)]


CST, CCOL, RET_SDEC = make_consts()

PARAM_NAMES = ["norm_g", "w_in", "gla_gk_w2", "gla_gk_b", "gla_norm_g", "ssd_conv_w", "ssd_conv_b",
               "ssd_dt_bias", "ssd_a_log", "ssd_d", "ssd_norm_g", "rwkv_mu", "rwkv_w0", "rwkv_w2",
               "rwkv_a0", "rwkv_a2", "rwkv_k_k", "rwkv_k_a", "rwkv_r_k", "rwkv_ln_g", "rwkv_ln_b",
               "mem_norm_g", "w_mem_kv", "w_up_ret", "w_up_gla", "w_up_ssd", "w_up_rwkv", "w_up_mem",
               "w_out", "final_norm_g"]
SHAPES = {
    "x": [SEQ, D], "mem": [256, D], "positions": [1, SEQ],
    "norm_g": [2, D], "w_in": [2, D, IN_TOTAL], "gla_gk_w2": [2, 16, 256], "gla_gk_b": [2, 256],
    "gla_norm_g": [2, 128], "ssd_conv_w": [2, 4, 1024], "ssd_conv_b": [2, 1024], "ssd_dt_bias": [2, 8],
    "ssd_a_log": [2, 8], "ssd_d": [2, 8], "ssd_norm_g": [2, 512], "rwkv_mu": [2, 1664],
    "rwkv_w0": [2, 512], "rwkv_w2": [2, 64, 512], "rwkv_a0": [2, 512], "rwkv_a2": [2, 64, 512],
    "rwkv_k_k": [2, 512], "rwkv_k_a": [2, 512], "rwkv_r_k": [2, 512], "rwkv_ln_g": [2, 512],
    "rwkv_ln_b": [2, 512], "mem_norm_g": [2, D], "w_mem_kv": [2, D, 512], "w_up_ret": [2, 512, D],
    "w_up_gla": [2, 512, D], "w_up_ssd": [2, 512, D], "w_up_rwkv": [2, 512, D], "w_up_mem": [2, 256, D],
    "w_out": [2, D, D], "final_norm_g": [1, D],
}


def build(nblk=SEQ // BLK, nlayers=2, debug=None, branches=("ret", "gla", "ssd", "rwkv", "mem"), dbg_what=0, dbg_layer=0, do_merge=True, pipe=False):
    nc = bass.Bass("TRN2", target_bir_lowering=False)
    k = KB(nc)
    dr = {}
    ntok_all = nblk * BLK
    for n, shp in SHAPES.items():
        shp = list(shp)
        if n in ("x",):
            shp[0] = ntok_all
        elif n == "positions":
            shp[1] = ntok_all + (BLK if pipe else 0)
        elif n not in ("mem", "final_norm_g"):
            shp[0] = nlayers
        dr[n] = nc.dram_tensor(n, shp, I32 if n == "positions" else F32, kind="ExternalInput").ap()
    cst_d = nc.dram_tensor("cst", list(CST.shape), F32, kind="ExternalInput").ap()
    out_d = nc.dram_tensor("out", [ntok_all, D], F32, kind="ExternalOutput").ap()
    x1_d = nc.dram_tensor("x1s", [ntok_all, D], F32, kind="Internal").ap()
    x1_b = [Buf() for _ in range(SEQ // BLK)]
    if pipe:
        role_d = nc.dram_tensor("role", [128, 16], F32, kind="ExternalInput").ap()
        cin_d = nc.dram_tensor("cin", [4 * BLK, D], F32, kind="Internal").ap()
        cout_d = nc.dram_tensor("cout", [4 * BLK, D], F32, kind="Internal").ap()
        cin_b, cout_b = Buf(), Buf()
        out_b = [Buf() for _ in range(SEQ // BLK)]
    dbg_d = None
    if debug is not None:
        dbg_d = nc.dram_tensor("dbg", [ntok_all, debug], F32, kind="ExternalOutput").ap()

    cst = k.sb([128, CST.shape[1]], name="cst")
    k.dma("sp", cst[:, :], cst_d[:, :], w=[cst])

    def C(name, p0=0, p1=128):
        o, n = CCOL[name]
        return cst[p0:p1, o:o + n]

    ident = C("ident")


    psr = Ring([k.ps() for _ in range(6)])
    pacc = [k.ps(name='pacc') for _ in range(2)]
    ntok = nblk * BLK
    NCH = SEQ // BLK

    def bc(ap_row, n):
        return ap_row.to_broadcast([128, n])

    xb = k.sb([128, NJ, D], name="xb")
    hT = k.sb([128, 8, BLK], F32R if FAST else F32, name="hT")
    xn = k.sb([128, D], name="xn")
    junk = k.sb([128, D], name="junk")
    ss = k.sb([128, 16], name="ss")
    wst = Ring([k.sb([128, 8, 512], F32R if FAST else F32, name="wst") for _ in range(3)])
    TK = [k.sb([128, NJ, 512], name="TK") for _ in range(4)]
    FM = [k.sb([128, BLK], name="FM") for _ in range(16)]
    sq = Ring([k.sb([128, 128], name="sq") for _ in range(16)])
    sq2 = Ring([k.sb([128, 256], name="sq2") for _ in range(6)])
    stt = Ring([k.sb([128, 32], name="st") for _ in range(8)])
    oT = [k.sb([128, 4, BLK], F32R if FAST else F32, name="oT") for _ in range(4)] + [k.sb([128, 2, BLK], F32R if FAST else F32, name="oTm")]
    obr = Ring([k.sb([128, 512], name="obr") for _ in range(2)])
    posi = k.sb([1, BLK], I32, name="posi")
    posf = k.sb([1, BLK], name="posf")
    cosT = k.sb([64, BLK], name="cosT")
    sinT = k.sb([64, BLK], name="sinT")
    rope_qi = k.sb([64, BLK], I32, name="rope_qi")
    rope_qf = k.sb([64, BLK], name="rope_qf")
    pi_c = k.sb([128, 1], name="pi_c")
    k.V(lambda: nc.vector.memset(pi_c[:, :], -math.pi), w=[pi_c])
    gcol = k.sb([128, 8], name="gcol")
    gla_w2 = k.sb([16, 256], name="gla_w2")
    gla_b = k.sb([1, 256], name="gla_b")
    gla_ng = k.sb([128, 128], name="gla_ng")
    ssd_cw = k.sb([128, 8, 4], name="ssd_cw")
    ssd_cb = k.sb([128, 8], name="ssd_cb")
    ssd_sm = k.sb([128, 32], name="ssd_sm")
    ssd_ng = k.sb([128, 512], name="ssd_ng")
    ssd_carry = k.sb([128, 8, 4], name="ssd_carry")
    rw_cols = k.sb([128, 64], name="rw_cols")
    rw_w2a2 = k.sb([128, 512], name="rw_w2a2")
    rw_lng = k.sb([128, 512], name="rw_lng")
    rw_lnb = k.sb([128, 512], name="rw_lnb")
    rw_carry = k.sb([128, 16], name="rw_carry")
    kmT = k.sb([64, 4, 256], name="kmT")
    vm = k.sb([128, 2, 256], name="vm")
    ret_S = [k.sb([64, 128], name="ret_S") for _ in range(4)]
    gla_S = [k.sb([64, 128], name="gla_S") for _ in range(4)]
    ssd_S = k.sb([128, 512], name="ssd_S")
    rw_S = [k.sb([128, 64], name="rw_S") for _ in range(4)]

    def load_w(wd, c0, ncol, rows=8, r0=0):
        wt = wst.next()
        k.dma("pool" if FAST else "sp", wt[:, 0:rows, 0:ncol],
              wd[r0 * 128:(r0 + rows) * 128, :].rearrange("(c p) n -> p c n", p=128)[:, :, c0:c0 + ncol], w=[wt])
        return wt

    def proj_fm(l, c0, ncol, evac):
        nc_ = 128 if FAST else ncol
        wt = load_w(dr["w_in"][l], c0, nc_)
        pa = psr.next()
        for dc in range(8):
            k.mm(pa[0:nc_, 0:BLK], wt[:, dc, 0:nc_], hT[:, dc, :], start=(dc == 0), stop=(dc == 7), r=[wt, hT], w=[pa],
                 fast=(nc_ == 128))
        evac(pa)

    def proj_tok(l, c0, ncol, evac):
        for g0 in range(0, ncol, 512):
            w_ = min(512, ncol - g0)
            wt = load_w(dr["w_in"][l], c0 + g0, w_)
            for j in range(NJ):
                pa = psr.next()
                for dc in range(8):
                    k.mm(pa[:, 0:w_], hT[:, dc, j * T:(j + 1) * T], wt[:, dc, 0:w_], start=(dc == 0), stop=(dc == 7),
                         r=[wt, hT], w=[pa], fast=(w_ % 2 == 0))
                evac(j, g0, w_, pa)

    def tr(dst_ap, dst_tl, src_ap, src_tl, npart, nfree, eng="act"):
        pa = psr.next()
        k.op("pe", lambda: nc.tensor.transpose(pa[0:nfree, 0:npart], src_ap, ident[0:npart, 0:npart]),
             r=[src_tl, cst], w=[pa])
        if eng == "act":
            k.A(lambda: nc.scalar.copy(out=dst_ap, in_=pa[0:nfree, 0:npart]), r=[pa], w=[dst_tl])
        else:
            k.V(lambda: nc.vector.tensor_copy(out=dst_ap, in_=pa[0:nfree, 0:npart]), r=[pa], w=[dst_tl])

    def rstd_groups(y_tl, y_ap_of, n, w, eps, st, c0):
        for g in range(n):
            k.A(lambda: nc.scalar.activation(out=junk[:, 0:w], in_=y_ap_of(g), func=AF.Square,
                                             accum_out=st[:, c0 + g:c0 + g + 1]), r=[y_tl], w=[junk, st])
        k.V(lambda: nc.vector.tensor_scalar(out=st[:, c0 + n:c0 + 2 * n], in0=st[:, c0:c0 + n], scalar1=1.0 / w,
                                            scalar2=eps, op0=ALU.mult, op1=ALU.add), r=[st], w=[st])
        k.A(lambda: nc.scalar.sqrt(out=st[:, c0 + n:c0 + 2 * n], in_=st[:, c0 + n:c0 + 2 * n]), r=[st], w=[st])
        k.V(lambda: nc.vector.reciprocal(out=st[:, c0 + 2 * n:c0 + 3 * n], in_=st[:, c0 + n:c0 + 2 * n]), r=[st], w=[st])

    def softplus_ip(x_ap, x_tl, n):
        a = sq2.next()
        k.A(lambda: nc.scalar.activation(out=a[:, 0:n], in_=x_ap, func=AF.Abs), r=[x_tl], w=[a])
        k.A(lambda: nc.scalar.activation(out=a[:, 0:n], in_=a[:, 0:n], func=AF.Exp, scale=-1.0), r=[a], w=[a])
        k.A(lambda: nc.scalar.activation(out=a[:, 0:n], in_=a[:, 0:n], func=AF.Ln, bias=1.0), r=[a], w=[a])
        k.V(lambda: nc.vector.scalar_tensor_tensor(out=x_ap, in0=x_ap, scalar=0.0, in1=a[:, 0:n], op0=ALU.max,
                                                   op1=ALU.add), r=[x_tl, a], w=[x_tl])

    def emit_out(bi, l, blk, j, o, width):
        t0 = blk * BLK
        for c in range(width // 128):
            tr(oT[bi][:, c, j * T:(j + 1) * T], oT[bi], o[:, c * 128:(c + 1) * 128], o, 128, 128,
               eng=("act" if c % 2 == 0 else "dve"))
        if debug is not None and l == dbg_layer:
            k.dma("pool", dbg_d[t0 + j * T:t0 + (j + 1) * T, bi * 512:bi * 512 + width], o[:, 0:width], r=[o])

    def load_params(l):
        NCg = dict(allow_slow_non_contiguous=True)
        k.dma("sp", gcol[:, :], dr["norm_g"][l].rearrange("(c p) -> p c", p=128), w=[gcol], **NCg)
        k.dma("sp", gla_w2[:, :], dr["gla_gk_w2"][l], w=[gla_w2])
        k.dma("sp", gla_b[:, :], dr["gla_gk_b"][l:l + 1, :], w=[gla_b])
        k.dma("sp", gla_ng[:, :], bc(dr["gla_norm_g"][l:l + 1, :], 128), w=[gla_ng])
        for j_ in range(4):
            k.dma("sp", ssd_cw[:, :, j_], dr["ssd_conv_w"][l, j_].rearrange("(c p) -> p c", p=128), w=[ssd_cw], **NCg)
        k.dma("sp", ssd_cb[:, :], dr["ssd_conv_b"][l].rearrange("(c p) -> p c", p=128), w=[ssd_cb], **NCg)
        k.dma("sp", ssd_sm[:, 0:8], bc(dr["ssd_dt_bias"][l:l + 1, :], 8), w=[ssd_sm])
        k.dma("sp", ssd_sm[:, 8:16], bc(dr["ssd_a_log"][l:l + 1, :], 8), w=[ssd_sm])
        k.dma("sp", ssd_sm[:, 16:24], bc(dr["ssd_d"][l:l + 1, :], 8), w=[ssd_sm])
        k.A(lambda: nc.scalar.activation(out=ssd_sm[:, 8:16], in_=ssd_sm[:, 8:16], func=AF.Exp), r=[ssd_sm], w=[ssd_sm])
        k.V(lambda: nc.vector.tensor_scalar(out=ssd_sm[:, 8:16], in0=ssd_sm[:, 8:16], scalar1=-1.0, scalar2=None,
                                            op0=ALU.mult), r=[ssd_sm], w=[ssd_sm])
        k.dma("sp", ssd_ng[:, :], bc(dr["ssd_norm_g"][l:l + 1, :], 512), w=[ssd_ng])
        k.dma("sp", rw_cols[:, 0:13], dr["rwkv_mu"][l].rearrange("(c p) -> p c", p=128), w=[rw_cols], **NCg)
        for i_, nm in enumerate(["rwkv_w0", "rwkv_a0", "rwkv_k_k", "rwkv_k_a", "rwkv_r_k"]):
            k.dma("sp", rw_cols[:, 16 + 4 * i_:20 + 4 * i_], dr[nm][l].rearrange("(c p) -> p c", p=128), w=[rw_cols], **NCg)
        k.V(lambda: nc.vector.tensor_scalar(out=rw_cols[:, 40:53], in0=rw_cols[:, 0:13], scalar1=-1.0, scalar2=1.0,
                                            op0=ALU.mult, op1=ALU.add), r=[rw_cols], w=[rw_cols])
        k.dma("sp", rw_w2a2[0:64, :], dr["rwkv_w2"][l], w=[rw_w2a2])
        k.dma("sp", rw_w2a2[64:128, :], dr["rwkv_a2"][l], w=[rw_w2a2])
        k.dma("sp", rw_lng[:, :], bc(dr["rwkv_ln_g"][l:l + 1, :], 512), w=[rw_lng])
        k.dma("sp", rw_lnb[:, :], bc(dr["rwkv_ln_b"][l:l + 1, :], 512), w=[rw_lnb])
        mg = stt.next()
        k.dma("sp", mg[:, 0:8], dr["mem_norm_g"][l].rearrange("(c p) -> p c", p=128), w=[mg], **NCg)
        memT = FM[0:8]
        for mt in range(2):
            k.dma("sp", xn[:, :], dr["mem"][mt * 128:(mt + 1) * 128, :], w=[xn])
            st = stt.next()
            k.A(lambda: nc.scalar.activation(out=junk[:, :], in_=xn[:, :], func=AF.Square, accum_out=st[:, 0:1]),
                r=[xn], w=[junk, st])
            k.V(lambda: nc.vector.tensor_scalar(out=st[:, 1:2], in0=st[:, 0:1], scalar1=1.0 / D, scalar2=1e-6,
                                                op0=ALU.mult, op1=ALU.add), r=[st], w=[st])
            k.A(lambda: nc.scalar.sqrt(out=st[:, 1:2], in_=st[:, 1:2]), r=[st], w=[st])
            k.V(lambda: nc.vector.reciprocal(out=st[:, 2:3], in_=st[:, 1:2]), r=[st], w=[st])
            k.V(lambda: nc.vector.tensor_scalar(out=xn[:, :], in0=xn[:, :], scalar1=st[:, 2:3], scalar2=None,
                                                op0=ALU.mult), r=[xn, st], w=[xn])
            for dc in range(8):
                pa = psr.next()
                k.op("pe", lambda: nc.tensor.transpose(pa[:, 0:T], xn[:, dc * T:(dc + 1) * T], ident), r=[xn, cst], w=[pa])
                k.A(lambda: nc.scalar.activation(out=memT[dc][:, mt * T:(mt + 1) * T], in_=pa[:, 0:T], func=AF.Identity,
                                                 scale=mg[:, dc:dc + 1]), r=[pa, mg], w=[memT[dc]])
        wt = load_w(dr["w_mem_kv"][l], 0, 512)
        for h in range(4):
            pa = psr.next()
            for dc in range(8):
                k.mm(pa[0:64, 0:256], wt[:, dc, h * 64:(h + 1) * 64], memT[dc][:, 0:256], start=(dc == 0), stop=(dc == 7),
                     r=[wt, memT[dc]], w=[pa])
            k.A(lambda: nc.scalar.copy(out=kmT[:, h, :], in_=pa[0:64, 0:256]), r=[pa], w=[kmT])
        for mt in range(2):
            pa = psr.next()
            for dc in range(8):
                k.mm(pa[:, 0:256], memT[dc][:, mt * T:(mt + 1) * T], wt[:, dc, 256:512], start=(dc == 0), stop=(dc == 7),
                     r=[wt, memT[dc]], w=[pa])
            k.A(lambda: nc.scalar.copy(out=vm[:, mt, :], in_=pa[:, 0:256]), r=[pa], w=[vm])
        for s_ in ret_S + gla_S + rw_S + [ssd_S]:
            k.V(lambda: nc.vector.memset(s_[:, :], 0.0), w=[s_])
        k.V(lambda: nc.vector.memset(ssd_carry[:, :, :], 0.0), w=[ssd_carry])
        k.V(lambda: nc.vector.memset(rw_carry[:, :], 0.0), w=[rw_carry])

    def la_step(ks_ap, ks_tl, qs_ap, qs_tl, qi_ap, qi_tl, mask_ap, mask_tl, v_ap, v_tl, S, py_ap, py):
        psc = psr.next()
        k.mm(psc[:, 0:T], ks_ap, qs_ap, r=[ks_tl, qs_tl], w=[psc])
        sc = sq.next()
        k.V(lambda: nc.vector.tensor_tensor(out=sc[:, :], in0=psc[:, 0:T], in1=mask_ap, op=ALU.mult),
            r=[psc, mask_tl], w=[sc])
        k.mm(py_ap, sc[:, :], v_ap, start=True, stop=False, r=[sc, v_tl], w=[py])
        k.mm(py_ap, qi_ap, S[:, :], start=False, stop=True, r=[qi_tl, S], w=[py])

    def ret_block(l, blk):
        t0 = blk * BLK
        qT, kT = FM[0:4], FM[4:8]
        vt, gt = TK[0], TK[1]
        for c_base, dst, scale in ((C_RET_Q, qT, 1.0), (C_RET_K, kT, 0.125)):
            for h in range(4):
                def ev(pa, h=h, dst=dst, scale=scale):
                    ta, tb = FM[8], FM[9]
                    k.A(lambda: nc.scalar.activation(out=ta[0:64, :], in_=pa[0:64, 0:BLK], func=AF.Copy, scale=scale),
                        r=[pa], w=[ta])
                    pb = psr.next()
                    k.mm(pb[0:64, 0:BLK], C("rot", 0, 64), ta[0:64, :], r=[cst, ta], w=[pb])
                    k.V(lambda: nc.vector.tensor_tensor(out=tb[0:64, :], in0=pb[0:64, 0:BLK], in1=sinT[:, :], op=ALU.mult),
                        r=[pb, sinT], w=[tb])
                    k.V(lambda: nc.vector.tensor_tensor(out=ta[0:64, :], in0=ta[0:64, :], in1=cosT[:, :], op=ALU.mult),
                        r=[ta, cosT], w=[ta])
                    k.V(lambda: nc.vector.tensor_tensor(out=dst[h][0:64, :], in0=ta[0:64, :], in1=tb[0:64, :], op=ALU.add),
                        r=[ta, tb], w=[dst[h]])
                proj_fm(l, c_base + 64 * h, 64, ev)
        proj_tok(l, C_RET_V, 512, lambda j, g0, w_, pa: k.A(
            lambda: nc.scalar.copy(out=vt[:, j, g0:g0 + w_], in_=pa[:, 0:w_]), r=[pa], w=[vt]))
        proj_tok(l, C_RET_G, 512, lambda j, g0, w_, pa: k.A(
            lambda: nc.scalar.activation(out=gt[:, j, g0:g0 + w_], in_=pa[:, 0:w_], func=AF.Silu), r=[pa], w=[gt]))
        ko, _ = CCOL["retks"]
        for j in range(NJ):
            py = pacc[j % 2]
            for hg in range(2):
                hs = (2 * hg, 2 * hg + 1)
                qs_ = {h: qT[h][0:64, j * T:(j + 1) * T] for h in hs}
                ks_ = {h: kT[h][0:64, j * T:(j + 1) * T] for h in hs}
                vh = {h: vt[:, j, h * 128:(h + 1) * 128] for h in hs}
                qi, psc, sc, kt, pt, pds = {}, {}, {}, {}, {}, {}
                for h in hs:
                    psc[h] = psr.next()
                    k.mm(psc[h][:, 0:T], ks_[h], qs_[h], r=[kT[h], qT[h]], w=[psc[h]])
                for h in hs:
                    pt[h] = psr.next()
                    k.op("pe", lambda: nc.tensor.transpose(pt[h][:, 0:64], ks_[h], ident[0:64, 0:64]), r=[kT[h], cst], w=[pt[h]])
                for h in hs:
                    qi[h] = sq.next()
                    k.V(lambda: nc.vector.tensor_tensor(out=qi[h][0:64, :], in0=qs_[h],
                                                        in1=C("retqs", 0, 64)[:, h * T:(h + 1) * T], op=ALU.mult),
                        r=[qT[h], cst], w=[qi[h]])
                    sc[h] = sq.next()
                    k.V(lambda: nc.vector.tensor_tensor(out=sc[h][:, :], in0=psc[h][:, 0:T],
                                                        in1=C("retdec")[:, h * T:(h + 1) * T], op=ALU.mult),
                        r=[psc[h], cst], w=[sc[h]])
                    kt[h] = sq.next()
                    k.A(lambda: nc.scalar.activation(out=kt[h][:, 0:64], in_=pt[h][:, 0:64], func=AF.Identity,
                                                     scale=cst[:, ko + h:ko + h + 1]), r=[pt[h], cst], w=[kt[h]])
                for h in hs:
                    k.mm(py[:, h * 128:(h + 1) * 128], sc[h][:, :], vh[h], start=True, stop=False, r=[sc[h], vt], w=[py])
                    k.mm(py[:, h * 128:(h + 1) * 128], qi[h][0:64, :], ret_S[h][:, :], start=False, stop=True,
                         r=[qi[h], ret_S[h]], w=[py])
                for h in hs:
                    pds[h] = psr.next()
                    k.mm(pds[h][0:64, 0:128], kt[h][:, 0:64], vh[h], r=[kt[h], vt], w=[pds[h]])
                for h in hs:
                    k.V(lambda: nc.vector.scalar_tensor_tensor(out=ret_S[h][:, :], in0=ret_S[h][:, :], scalar=RET_SDEC[h],
                                                               in1=pds[h][0:64, 0:128], op0=ALU.mult, op1=ALU.add),
                        r=[ret_S[h], pds[h]], w=[ret_S[h]])
            st = stt.next()
            rstd_groups(py, lambda g: py[:, g * 128:(g + 1) * 128], 4, 128, 1e-6, st, 0)
            o = obr.next()
            for h in range(4):
                k.V(lambda: nc.vector.scalar_tensor_tensor(out=o[:, h * 128:(h + 1) * 128], in0=py[:, h * 128:(h + 1) * 128],
                                                           scalar=st[:, 8 + h:9 + h], in1=gt[:, j, h * 128:(h + 1) * 128],
                                                           op0=ALU.mult, op1=ALU.mult), r=[py, st, gt], w=[o])
            emit_out(0, l, blk, j, o, 512)

    def gla_block(l, blk):
        qT, kT = FM[0:4], FM[4:8]
        vt, gt = TK[0], TK[1]
        glow = FM[8]
        for h in range(4):
            proj_fm(l, C_GLA_Q + 64 * h, 64, lambda pa, h=h: k.A(
                lambda: nc.scalar.activation(out=qT[h][0:64, :], in_=pa[0:64, 0:BLK], func=AF.Copy, scale=0.125),
                r=[pa], w=[qT[h]]))
            proj_fm(l, C_GLA_K + 64 * h, 64, lambda pa, h=h: k.A(
                lambda: nc.scalar.copy(out=kT[h][0:64, :], in_=pa[0:64, 0:BLK]), r=[pa], w=[kT[h]]))
        proj_fm(l, C_GLA_GK, 16, lambda pa: k.A(
            lambda: nc.scalar.copy(out=glow[0:16, :], in_=pa[0:16, 0:BLK]), r=[pa], w=[glow]))
        proj_tok(l, C_GLA_V, 512, lambda j, g0, w_, pa: k.A(
            lambda: nc.scalar.copy(out=vt[:, j, g0:g0 + w_], in_=pa[:, 0:w_]), r=[pa], w=[vt]))
        proj_tok(l, C_GLA_G, 512, lambda j, g0, w_, pa: k.A(
            lambda: nc.scalar.activation(out=gt[:, j, g0:g0 + w_], in_=pa[:, 0:w_], func=AF.Silu), r=[pa], w=[gt]))
        for j in range(NJ):
            pl = psr.next()
            k.mm(pl[:, 0:256], glow[0:16, j * T:(j + 1) * T], gla_w2[:, :], start=True, stop=False, r=[glow, gla_w2], w=[pl])
            k.mm(pl[:, 0:256], C("ones", 0, 1), gla_b[:, :], start=False, stop=True, r=[cst, gla_b], w=[pl])
            lg = sq2.next()
            k.A(lambda: nc.scalar.activation(out=lg[:, :], in_=pl[:, 0:256], func=AF.Copy, scale=-1.0), r=[pl], w=[lg])
            softplus_ip(lg[:, :], lg, 256)
            k.V(lambda: nc.vector.tensor_scalar(out=lg[:, :], in0=lg[:, :], scalar1=-1.0 / 16.0, scalar2=None,
                                                op0=ALU.mult), r=[lg], w=[lg])
            py = pacc[j % 2]
            for hg in range(2):
                hs = (2 * hg, 2 * hg + 1)
                qs_ = {h: qT[h][0:64, j * T:(j + 1) * T] for h in hs}
                ks_ = {h: kT[h][0:64, j * T:(j + 1) * T] for h in hs}
                vh = {h: vt[:, j, h * 128:(h + 1) * 128] for h in hs}
                pc, sth, ekin, eqin, eq, psc, sc, kt, pds = {}, {}, {}, {}, {}, {}, {}, {}, {}
                for h in hs:
                    pc[h] = psr.next()
                    k.mm(pc[h][0:64, 0:T], lg[:, h * 64:(h + 1) * 64], C("maskT"), r=[lg, cst], w=[pc[h]])
                for h in hs:
                    st = sth[h] = stt.next()
                    p_ = pc[h]
                    k.V(lambda: nc.vector.tensor_copy(out=st[0:64, 0:1], in_=p_[0:64, 64:65]), r=[p_], w=[st])
                    k.V(lambda: nc.vector.tensor_scalar(out=st[0:64, 1:2], in0=p_[0:64, 64:65], scalar1=-1.0, scalar2=None,
                                                        op0=ALU.mult), r=[p_], w=[st])
                    ekin[h], eqin[h], eq[h] = sq.next(), sq.next(), sq.next()
                    k.A(lambda: nc.scalar.activation(out=ekin[h][0:64, :], in_=p_[0:64, 0:T], func=AF.Exp, scale=-1.0,
                                                     bias=st[0:64, 0:1]), r=[p_, st], w=[ekin[h]])
                    k.A(lambda: nc.scalar.activation(out=eqin[h][0:64, :], in_=p_[0:64, 0:T], func=AF.Exp, scale=1.0,
                                                     bias=st[0:64, 1:2]), r=[p_, st], w=[eqin[h]])
                    k.A(lambda: nc.scalar.activation(out=eq[h][0:64, :], in_=p_[0:64, 0:T], func=AF.Exp), r=[p_], w=[eq[h]])
                    k.A(lambda: nc.scalar.activation(out=st[0:64, 2:3], in_=p_[0:64, T - 1:T], func=AF.Exp), r=[p_], w=[st])
                    k.A(lambda: nc.scalar.activation(out=st[0:64, 3:4], in_=p_[0:64, T - 1:T], func=AF.Exp, scale=1.0,
                                                     bias=st[0:64, 1:2]), r=[p_, st], w=[st])
                for h in hs:
                    k.V(lambda: nc.vector.tensor_tensor(out=ekin[h][0:64, :], in0=ekin[h][0:64, :], in1=ks_[h], op=ALU.mult),
                        r=[ekin[h], kT[h]], w=[ekin[h]])
                    k.V(lambda: nc.vector.tensor_tensor(out=eqin[h][0:64, :], in0=eqin[h][0:64, :], in1=qs_[h], op=ALU.mult),
                        r=[eqin[h], qT[h]], w=[eqin[h]])
                    k.V(lambda: nc.vector.tensor_tensor(out=eq[h][0:64, :], in0=eq[h][0:64, :], in1=qs_[h], op=ALU.mult),
                        r=[eq[h], qT[h]], w=[eq[h]])
                for h in hs:
                    psc[h] = psr.next()
                    k.mm(psc[h][:, 0:T], ekin[h][0:64, :], eqin[h][0:64, :], r=[ekin[h], eqin[h]], w=[psc[h]])
                for h in hs:
                    kt[h] = sq.next()
                    tr(kt[h][:, 0:64], kt[h], ekin[h][0:64, :], ekin[h], 64, 128)
                for h in hs:
                    sc[h] = sq.next()
                    k.V(lambda: nc.vector.tensor_tensor(out=sc[h][:, :], in0=psc[h][:, 0:T], in1=C("maskT"), op=ALU.mult),
                        r=[psc[h], cst], w=[sc[h]])
                for h in hs:
                    k.mm(py[:, h * 128:(h + 1) * 128], sc[h][:, :], vh[h], start=True, stop=False, r=[sc[h], vt], w=[py])
                    k.mm(py[:, h * 128:(h + 1) * 128], eq[h][0:64, :], gla_S[h][:, :], start=False, stop=True,
                         r=[eq[h], gla_S[h]], w=[py])
                for h in hs:
                    pds[h] = psr.next()
                    k.mm(pds[h][0:64, 0:128], kt[h][:, 0:64], vh[h], r=[kt[h], vt], w=[pds[h]])
                for h in hs:
                    st = sth[h]
                    k.V(lambda: nc.vector.tensor_scalar(out=gla_S[h][:, :], in0=gla_S[h][:, :], scalar1=st[0:64, 2:3],
                                                        scalar2=None, op0=ALU.mult), r=[gla_S[h], st], w=[gla_S[h]])
                    k.V(lambda: nc.vector.scalar_tensor_tensor(out=gla_S[h][:, :], in0=pds[h][0:64, 0:128], scalar=st[0:64, 3:4],
                                                               in1=gla_S[h][:, :], op0=ALU.mult, op1=ALU.add),
                        r=[gla_S[h], pds[h], st], w=[gla_S[h]])
            st = stt.next()
            rstd_groups(py, lambda g: py[:, g * 128:(g + 1) * 128], 4, 128, 1e-6, st, 0)
            o = obr.next()
            for h in range(4):
                k.V(lambda: nc.vector.scalar_tensor_tensor(out=o[:, h * 128:(h + 1) * 128], in0=py[:, h * 128:(h + 1) * 128],
                                                           scalar=st[:, 8 + h:9 + h], in1=gt[:, j, h * 128:(h + 1) * 128],
                                                           op0=ALU.mult, op1=ALU.mult), r=[py, st, gt], w=[o])
                k.V(lambda: nc.vector.tensor_tensor(out=o[:, h * 128:(h + 1) * 128], in0=o[:, h * 128:(h + 1) * 128],
                                                    in1=gla_ng[:, :], op=ALU.mult), r=[o, gla_ng], w=[o])
            emit_out(1, l, blk, j, o, 512)

    def ssd_block(l, blk):
        cv = FM[0:8]
        zt, dtt = TK[0], TK[1]
        raw = FM[8]
        for c in range(8):
            def ev(pa, c=c):
                k.A(lambda: nc.scalar.copy(out=raw[:, :], in_=pa[:, 0:BLK]), r=[pa], w=[raw])
                acc = cv[c]
                k.V(lambda: nc.vector.tensor_scalar(out=acc[:, :], in0=raw[:, :], scalar1=ssd_cw[:, c, 3:4], scalar2=None,
                                                    op0=ALU.mult), r=[raw, ssd_cw], w=[acc])
                for d_ in (1, 2, 3):
                    k.V(lambda: nc.vector.scalar_tensor_tensor(out=acc[:, d_:BLK], in0=raw[:, 0:BLK - d_],
                                                               scalar=ssd_cw[:, c, 3 - d_:4 - d_], in1=acc[:, d_:BLK],
                                                               op0=ALU.mult, op1=ALU.add), r=[raw, ssd_cw, acc], w=[acc])
                    k.V(lambda: nc.vector.scalar_tensor_tensor(out=acc[:, 0:d_], in0=ssd_carry[:, c, 3 - d_:3],
                                                               scalar=ssd_cw[:, c, 3 - d_:4 - d_], in1=acc[:, 0:d_],
                                                               op0=ALU.mult, op1=ALU.add),
                        r=[ssd_carry, ssd_cw, acc], w=[acc])
                k.V(lambda: nc.vector.tensor_copy(out=ssd_carry[:, c, 0:3], in_=raw[:, BLK - 3:BLK]), r=[raw], w=[ssd_carry])
                k.A(lambda: nc.scalar.activation(out=acc[:, :], in_=acc[:, :], func=AF.Silu, bias=ssd_cb[:, c:c + 1],
                                                 scale=1.0), r=[acc, ssd_cb], w=[acc])
            proj_fm(l, C_SSD_XBC + 128 * c, 128, ev)
        proj_tok(l, C_SSD_Z, 512, lambda j, g0, w_, pa: k.A(
            lambda: nc.scalar.activation(out=zt[:, j, g0:g0 + w_], in_=pa[:, 0:w_], func=AF.Silu), r=[pa], w=[zt]))

        def ev_dt(j, g0, w_, pa):
            k.V(lambda: nc.vector.tensor_tensor(out=dtt[:, j, 0:8], in0=pa[:, 0:8], in1=ssd_sm[:, 0:8], op=ALU.add),
                r=[pa, ssd_sm], w=[dtt])
            softplus_ip(dtt[:, j, 0:8], dtt, 8)
        proj_tok(l, C_SSD_DT, 8, ev_dt)
        for j in range(NJ):
            xs = obr.next()
            for c in range(4):
                tr(xs[:, c * 128:(c + 1) * 128], xs, cv[c][:, j * T:(j + 1) * T], cv[c], 128, 128,
                   eng=("act" if c % 2 == 0 else "dve"))
            btok = [sq.next(), sq.next()]
            for g in range(2):
                tr(btok[g][:, :], btok[g], cv[4 + g][:, j * T:(j + 1) * T], cv[4 + g], 128, 128)
            st = stt.next()
            k.V(lambda: nc.vector.tensor_tensor(out=st[:, 0:8], in0=dtt[:, j, 0:8], in1=ssd_sm[:, 8:16], op=ALU.mult),
                r=[dtt, ssd_sm], w=[st])
            pq = psr.next()
            k.mm(pq[:, 0:8], C("maskT"), st[:, 0:8], r=[cst, st], w=[pq])
            k.mm(pq[:, 8:16], C("maskL"), st[:, 0:8], r=[cst, st], w=[pq])
            k.mm(pq[:, 16:24], C("ones"), st[:, 0:8], r=[cst, st], w=[pq])
            k.A(lambda: nc.scalar.activation(out=st[:, 8:32], in_=pq[:, 0:24], func=AF.Exp), r=[pq], w=[st])
            rhsM = [sq2.next() for _ in range(4)]
            for h in range(8):
                k.V(lambda: nc.vector.tensor_scalar(out=rhsM[h // 2][:, (h % 2) * T:(h % 2 + 1) * T], in0=C("maskT"),
                                                    scalar1=st[:, h:h + 1], scalar2=None, op0=ALU.mult),
                    r=[cst, st], w=[rhsM[h // 2]])
            v1 = sq2.next(), sq2.next()
            st2 = stt.next()
            k.V(lambda: nc.vector.tensor_tensor(out=st2[:, 0:8], in0=dtt[:, j, 0:8], in1=st[:, 16:24], op=ALU.mult),
                r=[dtt, st], w=[st2])
            for g in range(2):
                k.V(lambda: nc.vector.tensor_tensor(
                    out=v1[g][:, :].rearrange("p (h d) -> p h d", h=4),
                    in0=xs[:, g * 256:(g + 1) * 256].rearrange("p (h d) -> p h d", h=4),
                    in1=dtt[:, j, g * 4:(g + 1) * 4].unsqueeze(2).to_broadcast([128, 4, 64]), op=ALU.mult),
                    r=[xs, dtt], w=[v1[g]])
            k.V(lambda: nc.vector.tensor_tensor(
                out=junk[:, 0:512].rearrange("p (h d) -> p h d", h=8),
                in0=xs[:, :].rearrange("p (h d) -> p h d", h=8),
                in1=st2[:, 0:8].unsqueeze(2).to_broadcast([128, 8, 64]), op=ALU.mult), r=[xs, st2], w=[junk])
            pyi = pacc[0]
            pye = pacc[1]
            for g in range(2):
                bT = cv[4 + g][:, j * T:(j + 1) * T]
                cT = cv[6 + g][:, j * T:(j + 1) * T]
                psc = psr.next()
                k.mm(psc[:, 0:T], bT, cT, r=[cv[4 + g], cv[6 + g]], w=[psc])
                scg = sq.next()
                k.A(lambda: nc.scalar.copy(out=scg[:, :], in_=psc[:, 0:T]), r=[psc], w=[scg])
                pdd, exx = {}, {}
                for hh in range(4):
                    h = g * 4 + hh
                    pdd[hh] = pd = psr.next()
                    k.mm(pd[:, 0:T], C("maskL"), rhsM[h // 2][:, (h % 2) * T:(h % 2 + 1) * T], start=True, stop=False,
                         r=[cst, rhsM[h // 2]], w=[pd])
                    k.mm(pd[:, 0:T], ident, C("negm"), start=False, stop=True, r=[cst], w=[pd])
                for hh in range(4):
                    exx[hh] = ex = sq.next()
                    pd = pdd[hh]
                    k.A(lambda: nc.scalar.activation(out=ex[:, :], in_=pd[:, 0:T], func=AF.Exp), r=[pd], w=[ex])
                    k.V(lambda: nc.vector.tensor_tensor(out=ex[:, :], in0=ex[:, :], in1=scg[:, :], op=ALU.mult),
                        r=[ex, scg], w=[ex])
                for hh in range(4):
                    h = g * 4 + hh
                    ex = exx[hh]
                    k.mm(pyi[:, h * 64:(h + 1) * 64], ex[:, :], v1[g][:, hh * 64:(hh + 1) * 64], r=[ex, v1[g]], w=[pyi])
                k.mm(pye[:, g * 256:(g + 1) * 256], cT, ssd_S[:, g * 256:(g + 1) * 256], r=[cv[6 + g], ssd_S], w=[pye])
                pds = psr.next()
                k.mm(pds[:, 0:256], btok[g][:, :], junk[:, g * 256:(g + 1) * 256], r=[btok[g], junk], w=[pds])
                sg = ssd_S[:, g * 256:(g + 1) * 256].rearrange("p (h d) -> p h d", h=4)
                k.V(lambda: nc.vector.tensor_tensor(out=sg, in0=sg,
                                                    in1=st[:, 24 + g * 4:28 + g * 4].unsqueeze(2).to_broadcast([128, 4, 64]),
                                                    op=ALU.mult), r=[ssd_S, st], w=[ssd_S])
                k.V(lambda: nc.vector.tensor_tensor(out=ssd_S[:, g * 256:(g + 1) * 256], in0=ssd_S[:, g * 256:(g + 1) * 256],
                                                    in1=pds[:, 0:256], op=ALU.add), r=[ssd_S, pds], w=[ssd_S])
            o = obr.next()
            k.A(lambda: nc.scalar.copy(out=o[:, :], in_=pyi[:, :]), r=[pyi], w=[o])
            y3 = o[:, :].rearrange("p (h d) -> p h d", h=8)
            tmp = junk[:, 512:1024]
            k.V(lambda: nc.vector.tensor_tensor(out=tmp.rearrange("p (h d) -> p h d", h=8),
                                                in0=pye[:, :].rearrange("p (h d) -> p h d", h=8),
                                                in1=st[:, 8:16].unsqueeze(2).to_broadcast([128, 8, 64]), op=ALU.mult),
                r=[pye, st], w=[junk])
            k.V(lambda: nc.vector.tensor_tensor(out=o[:, :], in0=o[:, :], in1=tmp, op=ALU.add), r=[o, junk], w=[o])
            k.V(lambda: nc.vector.tensor_tensor(out=tmp.rearrange("p (h d) -> p h d", h=8),
                                                in0=xs[:, :].rearrange("p (h d) -> p h d", h=8),
                                                in1=ssd_sm[:, 16:24].unsqueeze(2).to_broadcast([128, 8, 64]), op=ALU.mult),
                r=[xs, ssd_sm], w=[junk])
            k.V(lambda: nc.vector.tensor_tensor(out=o[:, :], in0=o[:, :], in1=tmp, op=ALU.add), r=[o, junk], w=[o])
            k.V(lambda: nc.vector.tensor_tensor(out=o[:, :], in0=o[:, :], in1=zt[:, j, :], op=ALU.mult), r=[o, zt], w=[o])
            st3 = stt.next()
            rstd_groups(o, lambda g: o[:, g * 256:(g + 1) * 256], 2, 256, 1e-6, st3, 0)
            for g in range(2):
                k.V(lambda: nc.vector.scalar_tensor_tensor(out=o[:, g * 256:(g + 1) * 256], in0=o[:, g * 256:(g + 1) * 256],
                                                           scalar=st3[:, 4 + g:5 + g], in1=ssd_ng[:, g * 256:(g + 1) * 256],
                                                           op0=ALU.mult, op1=ALU.mult), r=[o, st3, ssd_ng], w=[o])
            emit_out(2, l, blk, j, o, 512)

    def mem_proj(l, blk):
        qT = FM[0:4]
        for h in range(4):
            proj_fm(l, C_MEM_Q + 64 * h, 64, lambda pa, h=h: k.A(
                lambda: nc.scalar.activation(out=qT[h][0:64, :], in_=pa[0:64, 0:BLK], func=AF.Copy, scale=0.125),
                r=[pa], w=[qT[h]]))

    def mem_block(l, blk, do_proj=True):
        qT = FM[0:4]
        if do_proj:
            mem_proj(l, blk)
        for j in range(NJ):
            st = stt.next()
            po = pacc[j % 2]
            o = obr.next()
            for hg in range(2):
                hs = (2 * hg, 2 * hg + 1)
                psc, pe_, pT = {}, {}, {}
                for h in hs:
                    psc[h] = psr.next()
                    k.mm(psc[h][:, 0:256], qT[h][0:64, j * T:(j + 1) * T], kmT[:, h, :], r=[qT[h], kmT], w=[psc[h]])
                for h in hs:
                    k.V(lambda: nc.vector.tensor_reduce(out=st[:, h:h + 1], in_=psc[h][:, 0:256], axis=AX.X, op=ALU.max),
                        r=[psc[h]], w=[st])
                    k.V(lambda: nc.vector.tensor_scalar(out=st[:, 4 + h:5 + h], in0=st[:, h:h + 1], scalar1=-1.0, scalar2=None,
                                                        op0=ALU.mult), r=[st], w=[st])
                    pe_[h] = sq2.next()
                    k.A(lambda: nc.scalar.activation(out=pe_[h][:, :], in_=psc[h][:, 0:256], func=AF.Exp,
                                                     bias=st[:, 4 + h:5 + h], scale=1.0, accum_out=st[:, 8 + h:9 + h]),
                        r=[psc[h], st], w=[pe_[h], st])
                for h in hs:
                    pT[h] = [sq.next(), sq.next()]
                    for mt in range(2):
                        tr(pT[h][mt][:, :], pT[h][mt], pe_[h][:, mt * T:(mt + 1) * T], pe_[h], 128, 128,
                           eng=("act" if mt == 0 else "dve"))
                for h in hs:
                    for mt in range(2):
                        k.mm(po[:, h * 64:(h + 1) * 64], pT[h][mt][:, :], vm[:, mt, h * 64:(h + 1) * 64], start=(mt == 0),
                             stop=(mt == 1), r=[pT[h][mt], vm], w=[po])
            k.V(lambda: nc.vector.reciprocal(out=st[:, 12:16], in_=st[:, 8:12]), r=[st], w=[st])
            k.V(lambda: nc.vector.tensor_tensor(out=o[:, 0:256].rearrange("p (h d) -> p h d", h=4),
                                                in0=po[:, 0:256].rearrange("p (h d) -> p h d", h=4),
                                                in1=st[:, 12:16].unsqueeze(2).to_broadcast([128, 4, 64]), op=ALU.mult),
                r=[po, st], w=[o])
            emit_out(4, l, blk, j, o, 256)

    RWD = F32R if (FAST and FAST_RW) else F32
    rwt = [k.sb([128, 128], F32 if i_ < 4 else RWD, name="rwt") for i_ in range(26)]
    rwx = [k.sb([128, 128], RWD, name="rwx") for _ in range(2)]
    rw_tmpS = k.sb([128, 64], name="rw_tmpS")

    def f32(ap):
        return ap.bitcast(F32) if ap.dtype == F32R else ap
    rw_AR = k.sb([128, 256], RWD, name="rw_AR")
    rw_bon = k.sb([128, NJ, 8], name="rw_bon")

    def rwkv_block(l, blk, before_epilogue=None):
        xr, xk, xv, ldt, at, kkt, tmpt, k2t, raw, bvt, prod = FM[0], FM[1], FM[2], FM[3], FM[4], FM[5], FM[6], FM[7], FM[8], FM[9], FM[10]
        waT = FM[12]
        gt, ytok, vtok = TK[0], TK[2], TK[3]

        def shift_ev(dst, cidx):
            def ev(pa):
                k.A(lambda: nc.scalar.copy(out=raw[:, :], in_=pa[:, 0:BLK]), r=[pa], w=[raw])
                k.V(lambda: nc.vector.tensor_scalar(out=dst[:, :], in0=raw[:, :], scalar1=rw_cols[:, 40 + cidx:41 + cidx],
                                                    scalar2=None, op0=ALU.mult), r=[raw, rw_cols], w=[dst])
                k.V(lambda: nc.vector.scalar_tensor_tensor(out=dst[:, 1:BLK], in0=raw[:, 0:BLK - 1],
                                                           scalar=rw_cols[:, cidx:cidx + 1], in1=dst[:, 1:BLK],
                                                           op0=ALU.mult, op1=ALU.add), r=[raw, rw_cols, dst], w=[dst])
                k.V(lambda: nc.vector.scalar_tensor_tensor(out=dst[:, 0:1], in0=rw_carry[:, cidx:cidx + 1],
                                                           scalar=rw_cols[:, cidx:cidx + 1], in1=dst[:, 0:1],
                                                           op0=ALU.mult, op1=ALU.add), r=[rw_carry, rw_cols, dst], w=[dst])
                k.V(lambda: nc.vector.tensor_copy(out=rw_carry[:, cidx:cidx + 1], in_=raw[:, BLK - 1:BLK]),
                    r=[raw], w=[rw_carry])
            return ev

        proj_tok(l, C_RWKV_G, 512, lambda j, g0, w_, pa: k.A(
            lambda: nc.scalar.activation(out=gt[:, j, g0:g0 + w_], in_=pa[:, 0:w_], func=AF.Silu), r=[pa], w=[gt]))
        proj_fm(l, C_RWKV_IN + 12 * 128, 128, shift_ev(waT, 12))
        k.A(lambda: nc.scalar.activation(out=waT[0:64, :], in_=waT[0:64, :], func=AF.Tanh), r=[waT], w=[waT])
        for p in range(4):
            proj_fm(l, C_RWKV_IN + p * 128, 128, shift_ev(xr, p))
            proj_fm(l, C_RWKV_IN + (4 + p) * 128, 128, shift_ev(xk, 4 + p))
            proj_fm(l, C_RWKV_IN + (8 + p) * 128, 128, shift_ev(xv, 8 + p))
            pz = psr.next()
            k.mm(pz[:, 0:BLK], rw_w2a2[0:64, p * 128:(p + 1) * 128], waT[0:64, :], r=[rw_w2a2, waT], w=[pz])
            k.A(lambda: nc.scalar.activation(out=ldt[:, :], in_=pz[:, 0:BLK], func=AF.Sigmoid, bias=rw_cols[:, 16 + p:17 + p],
                                             scale=1.0), r=[pz, rw_cols], w=[ldt])
            k.V(lambda: nc.vector.tensor_scalar(out=ldt[:, :], in0=ldt[:, :], scalar1=-math.exp(-0.5), scalar2=None,
                                                op0=ALU.mult), r=[ldt], w=[ldt])
            pa_ = psr.next()
            k.mm(pa_[:, 0:BLK], rw_w2a2[64:128, p * 128:(p + 1) * 128], waT[64:128, :], r=[rw_w2a2, waT], w=[pa_])
            k.A(lambda: nc.scalar.activation(out=at[:, :], in_=pa_[:, 0:BLK], func=AF.Sigmoid, bias=rw_cols[:, 20 + p:21 + p],
                                             scale=1.0), r=[pa_, rw_cols], w=[at])
            k.V(lambda: nc.vector.tensor_scalar(out=kkt[:, :], in0=xk[:, :], scalar1=rw_cols[:, 24 + p:25 + p], scalar2=None,
                                                op0=ALU.mult), r=[xk, rw_cols], w=[kkt])
            k.V(lambda: nc.vector.tensor_tensor(out=tmpt[:, :], in0=kkt[:, :], in1=kkt[:, :], op=ALU.mult), r=[kkt], w=[tmpt])
            pn = psr.next()
            k.mm(pn[:, 0:BLK], C("blk64"), tmpt[:, :], r=[cst, tmpt], w=[pn])
            k.A(lambda: nc.scalar.sqrt(out=tmpt[:, :], in_=pn[:, 0:BLK]), r=[pn], w=[tmpt])
            k.V(lambda: nc.vector.tensor_scalar(out=tmpt[:, :], in0=tmpt[:, :], scalar1=1e-12, scalar2=None, op0=ALU.max),
                r=[tmpt], w=[tmpt])
            k.V(lambda: nc.vector.reciprocal(out=tmpt[:, :], in_=tmpt[:, :]), r=[tmpt], w=[tmpt])
            k.V(lambda: nc.vector.tensor_tensor(out=kkt[:, :], in0=kkt[:, :], in1=tmpt[:, :], op=ALU.mult), r=[kkt, tmpt], w=[kkt])
            k.V(lambda: nc.vector.tensor_scalar(out=tmpt[:, :], in0=at[:, :], scalar1=rw_cols[:, 28 + p:29 + p], scalar2=-1.0,
                                                op0=ALU.mult, op1=ALU.mult), r=[at, rw_cols], w=[tmpt])
            k.V(lambda: nc.vector.tensor_scalar(out=tmpt[:, :], in0=tmpt[:, :], scalar1=rw_cols[:, 28 + p:29 + p], scalar2=-1.0,
                                                op0=ALU.add, op1=ALU.add), r=[tmpt, rw_cols], w=[tmpt])
            k.V(lambda: nc.vector.scalar_tensor_tensor(out=k2t[:, :], in0=tmpt[:, :], scalar=-1.0, in1=xk[:, :],
                                                       op0=ALU.mult, op1=ALU.mult), r=[tmpt, xk], w=[k2t])
            k.V(lambda: nc.vector.tensor_tensor(out=bvt[:, :], in0=kkt[:, :], in1=at[:, :], op=ALU.mult), r=[kkt, at], w=[bvt])
            k.V(lambda: nc.vector.scalar_tensor_tensor(out=prod[:, :], in0=xr[:, :], scalar=rw_cols[:, 32 + p:33 + p],
                                                       in1=k2t[:, :], op0=ALU.mult, op1=ALU.mult),
                r=[xr, rw_cols, k2t], w=[prod])
            for j in range(NJ):
                js = slice(j * T, (j + 1) * T)
                (cum, epos, eneg, eposx, Bt, Kt, Btok, Ktok, Vt, S0p) = rwt[0:10]
                pb = psr.next()
                k.mm(pb[:, 0:2], prod[:, js], C("headsel"), r=[prod, cst], w=[pb])
                k.A(lambda: nc.scalar.copy(out=rw_bon[:, j, 2 * p:2 * p + 2], in_=pb[:, 0:2]), r=[pb], w=[rw_bon])
                k.V(lambda: nc.vector.tensor_tensor_scan(out=cum[:, :], data0=C("ones"), data1=ldt[:, js], initial=0.0,
                                                         op0=ALU.mult, op1=ALU.add), r=[cst, ldt], w=[cum])
                st = stt.next()
                k.V(lambda: nc.vector.tensor_copy(out=st[:, 0:1], in_=cum[:, 63:64]), r=[cum], w=[st])
                k.V(lambda: nc.vector.tensor_scalar(out=st[:, 1:2], in0=cum[:, 63:64], scalar1=-1.0, scalar2=None,
                                                    op0=ALU.mult), r=[cum], w=[st])
                k.A(lambda: nc.scalar.activation(out=epos[:, :], in_=cum[:, :], func=AF.Exp, bias=st[:, 1:2], scale=1.0),
                    r=[cum, st], w=[epos])
                k.A(lambda: nc.scalar.activation(out=eneg[:, :], in_=cum[:, :], func=AF.Exp, bias=st[:, 0:1], scale=-1.0),
                    r=[cum, st], w=[eneg])
                k.V(lambda: nc.vector.tensor_tensor(out=eposx[:, :], in0=cum[:, :], in1=ldt[:, js], op=ALU.subtract),
                    r=[cum, ldt], w=[eposx])
                k.A(lambda: nc.scalar.activation(out=eposx[:, :], in_=eposx[:, :], func=AF.Exp, bias=st[:, 1:2], scale=1.0),
                    r=[eposx, st], w=[eposx])
                k.A(lambda: nc.scalar.activation(out=st[:, 2:3], in_=st[:, 0:1], func=AF.Exp), r=[st], w=[st])
                k.A(lambda: nc.scalar.activation(out=st[:, 3:4], in_=cum[:, T - 1:T], func=AF.Exp, bias=st[:, 1:2], scale=1.0),
                    r=[cum, st], w=[st])
                k.V(lambda: nc.vector.scalar_tensor_tensor(out=rw_AR[:, 0:T], in0=kkt[:, js], scalar=-1.0, in1=eposx[:, :],
                                                           op0=ALU.mult, op1=ALU.mult), r=[kkt, eposx], w=[rw_AR])
                k.V(lambda: nc.vector.tensor_tensor(out=rw_AR[:, T:2 * T], in0=xr[:, js], in1=epos[:, :], op=ALU.mult),
                    r=[xr, epos], w=[rw_AR])
                k.V(lambda: nc.vector.tensor_tensor(out=Bt[:, :], in0=bvt[:, js], in1=eneg[:, :], op=ALU.mult),
                    r=[bvt, eneg], w=[Bt])
                k.V(lambda: nc.vector.tensor_tensor(out=Kt[:, :], in0=k2t[:, js], in1=eneg[:, :], op=ALU.mult),
                    r=[k2t, eneg], w=[Kt])
                k.V(lambda: nc.vector.tensor_scalar(out=S0p[:, 0:64], in0=rw_S[p][:, :], scalar1=st[:, 2:3], scalar2=None,
                                                    op0=ALU.mult), r=[rw_S[p], st], w=[S0p])
                tr(Btok[:, :], Btok, f32(Bt[:, :]), Bt, 128, 128, eng="act")
                tr(Ktok[:, :], Ktok, f32(Kt[:, :]), Kt, 128, 128, eng="dve")
                tr(Vt[:, :], Vt, xv[:, js], xv, 128, 128, eng="act")
                k.A(lambda: nc.scalar.copy(out=vtok[:, j, p * 128:(p + 1) * 128], in_=f32(Vt[:, :])), r=[Vt], w=[vtok])
                HS = []
                for hh in range(2):
                    r0 = 64 * hh
                    d = dict(rs=slice(r0, r0 + 64), X=rwt[10 + 8 * hh], XT=rwt[11 + 8 * hh], Xn=rwt[12 + 8 * hh],
                             XTn=rwt[13 + 8 * hh], Z=rwt[14 + 8 * hh], Zn=rwt[15 + 8 * hh], Mbr=rwt[16 + 8 * hh],
                             Nak=rwt[17 + 8 * hh], Mkr=rwx[hh], hh=hh)
                    HS.append(d)
                for d in HS:
                    rs = d["rs"]
                    pN = psr.next()
                    k.mm(pN[:, 0:2 * T], Bt[rs, :], rw_AR[rs, :], r=[Bt, rw_AR], w=[pN], fast=FAST_RW)
                    pK = psr.next()
                    k.mm(pK[:, 0:2 * T], Kt[rs, :], rw_AR[rs, :], r=[Kt, rw_AR], w=[pK], fast=FAST_RW)
                    pX = psr.next()
                    k.mm(pX[:, 0:T], rw_AR[rs, 0:T], Bt[rs, :], r=[Bt, rw_AR], w=[pX], fast=FAST_RW)
                    k.V(lambda: nc.vector.tensor_tensor(out=d["X"][:, :], in0=pN[:, 0:T], in1=C("maskS"), op=ALU.mult),
                        r=[pN, cst], w=[d["X"]])
                    k.V(lambda: nc.vector.tensor_tensor(out=d["Mbr"][:, :], in0=pN[:, T:2 * T], in1=C("maskT"), op=ALU.mult),
                        r=[pN, cst], w=[d["Mbr"]])
                    k.V(lambda: nc.vector.tensor_tensor(out=d["Nak"][:, :], in0=pK[:, 0:T], in1=C("maskS"), op=ALU.mult),
                        r=[pK, cst], w=[d["Nak"]])
                    k.V(lambda: nc.vector.tensor_tensor(out=d["Mkr"][:, :], in0=pK[:, T:2 * T], in1=C("maskT"), op=ALU.mult),
                        r=[pK, cst], w=[d["Mkr"]])
                    k.V(lambda: nc.vector.tensor_tensor(out=d["XT"][:, :], in0=pX[:, 0:T], in1=C("maskL"), op=ALU.mult),
                        r=[pX, cst], w=[d["XT"]])
                for d in HS:
                    rs = d["rs"]
                    pW = psr.next()
                    k.mm(pW[:, 0:64], rw_AR[rs, 0:T], S0p[rs, 0:64], start=True, stop=False, r=[rw_AR, S0p], w=[pW], fast=FAST_RW)
                    k.mm(pW[:, 0:64], d["Nak"][:, :], Vt[:, rs], start=False, stop=True, r=[d["Nak"], Vt], w=[pW], fast=FAST_RW)
                    k.A(lambda: nc.scalar.copy(out=d["Z"][:, 0:64], in_=pW[:, 0:64]), r=[pW], w=[d["Z"]])
                for lev in range(7):
                    for d in HS:
                        X, XT, Xn, XTn, Z, Zn = d["X"], d["XT"], d["Xn"], d["XTn"], d["Z"], d["Zn"]
                        pZ = psr.next()
                        k.mm(pZ[:, 0:64], X[:, :], Z[:, 0:64], r=[X, Z], w=[pZ], fast=FAST_RW)
                        k.V(lambda: nc.vector.tensor_tensor(out=Zn[:, 0:64], in0=f32(Z[:, 0:64]), in1=pZ[:, 0:64], op=ALU.add),
                            r=[Z, pZ], w=[Zn])
                        d["Z"], d["Zn"] = Zn, Z
                        if lev < 6:
                            p1 = psr.next()
                            k.mm(p1[:, 0:T], XT[:, :], X[:, :], r=[XT, X], w=[p1], fast=FAST_RW)
                            p2 = psr.next()
                            k.mm(p2[:, 0:T], X[:, :], XT[:, :], r=[XT, X], w=[p2], fast=FAST_RW)
                            k.A(lambda: nc.scalar.copy(out=Xn[:, :], in_=p1[:, 0:T]), r=[p1], w=[Xn])
                            if d["hh"] == 0:
                                k.V(lambda: nc.vector.tensor_copy(out=XTn[:, :], in_=p2[:, 0:T]), r=[p2], w=[XTn])
                            else:
                                k.A(lambda: nc.scalar.copy(out=XTn[:, :], in_=p2[:, 0:T]), r=[p2], w=[XTn])
                            d["X"], d["Xn"] = Xn, X
                            d["XT"], d["XTn"] = XTn, XT
                for d in HS:
                    rs = d["rs"]
                    U = d["Z"]
                    pY = psr.next()
                    k.mm(pY[:, 0:64], rw_AR[rs, T:2 * T], S0p[rs, 0:64], start=True, stop=False, r=[rw_AR, S0p], w=[pY], fast=FAST_RW)
                    k.mm(pY[:, 0:64], d["Mbr"][:, :], U[:, 0:64], start=False, stop=False, r=[d["Mbr"], U], w=[pY], fast=FAST_RW)
                    k.mm(pY[:, 0:64], d["Mkr"][:, :], Vt[:, rs], start=False, stop=True, r=[d["Mkr"], Vt], w=[pY], fast=FAST_RW)
                    hcol = (2 * p + d["hh"]) * 64
                    k.A(lambda: nc.scalar.copy(out=ytok[:, j, hcol:hcol + 64], in_=pY[:, 0:64]), r=[pY], w=[ytok])
                    pS = psr.next()
                    k.mm(pS[:, 0:64], Btok[:, :], U[:, 0:64], start=True, stop=False, r=[Btok, U], w=[pS], fast=FAST_RW)
                    k.mm(pS[:, 0:64], Ktok[:, :], Vt[:, rs], start=False, stop=True, r=[Ktok, Vt], w=[pS], fast=FAST_RW)
                    k.V(lambda: nc.vector.tensor_tensor(out=rw_tmpS[rs, 0:64], in0=f32(S0p[rs, 0:64]), in1=pS[rs, 0:64], op=ALU.add),
                        r=[S0p, pS], w=[rw_tmpS])
                    k.V(lambda: nc.vector.tensor_scalar(out=rw_S[p][rs, :], in0=rw_tmpS[rs, 0:64], scalar1=st[rs, 3:4],
                                                        scalar2=None, op0=ALU.mult), r=[rw_tmpS, st], w=[rw_S[p]])
        if before_epilogue is not None:
            before_epilogue()
        for j in range(NJ):
            st = stt.next()
            y3 = ytok[:, j, :].rearrange("p (h d) -> p h d", h=8)
            k.V(lambda: nc.vector.tensor_reduce(out=st[:, 0:8], in_=y3, axis=AX.X, op=ALU.add), r=[ytok], w=[st])
            k.A(lambda: nc.scalar.activation(out=junk[:, 0:512], in_=ytok[:, j, :], func=AF.Square), r=[ytok], w=[junk])
            k.V(lambda: nc.vector.tensor_reduce(out=st[:, 8:16], in_=junk[:, 0:512].rearrange("p (h d) -> p h d", h=8),
                                                axis=AX.X, op=ALU.add), r=[junk], w=[st])
            k.V(lambda: nc.vector.tensor_scalar(out=st[:, 0:8], in0=st[:, 0:8], scalar1=1.0 / 64, scalar2=None, op0=ALU.mult),
                r=[st], w=[st])
            k.V(lambda: nc.vector.tensor_tensor(out=st[:, 16:24], in0=st[:, 0:8], in1=st[:, 0:8], op=ALU.mult), r=[st], w=[st])
            k.V(lambda: nc.vector.scalar_tensor_tensor(out=st[:, 8:16], in0=st[:, 8:16], scalar=1.0 / 64, in1=st[:, 16:24],
                                                       op0=ALU.mult, op1=ALU.subtract), r=[st], w=[st])
            k.V(lambda: nc.vector.tensor_scalar(out=st[:, 8:16], in0=st[:, 8:16], scalar1=64e-5, scalar2=None, op0=ALU.add),
                r=[st], w=[st])
            k.A(lambda: nc.scalar.sqrt(out=st[:, 8:16], in_=st[:, 8:16]), r=[st], w=[st])
            k.V(lambda: nc.vector.reciprocal(out=st[:, 24:32], in_=st[:, 8:16]), r=[st], w=[st])
            o = obr.next()
            o3 = o[:, :].rearrange("p (h d) -> p h d", h=8)
            k.V(lambda: nc.vector.tensor_tensor(out=o3, in0=y3, in1=st[:, 0:8].unsqueeze(2).to_broadcast([128, 8, 64]),
                                                op=ALU.subtract), r=[ytok, st], w=[o])
            k.V(lambda: nc.vector.tensor_tensor(out=o3, in0=o3, in1=st[:, 24:32].unsqueeze(2).to_broadcast([128, 8, 64]),
                                                op=ALU.mult), r=[o, st], w=[o])
            k.V(lambda: nc.vector.tensor_tensor(out=o[:, :], in0=o[:, :], in1=rw_lng[:, :], op=ALU.mult), r=[o, rw_lng], w=[o])
            k.V(lambda: nc.vector.tensor_tensor(out=o[:, :], in0=o[:, :], in1=rw_lnb[:, :], op=ALU.add), r=[o, rw_lnb], w=[o])
            k.V(lambda: nc.vector.tensor_tensor(out=junk[:, 0:512].rearrange("p (h d) -> p h d", h=8),
                                                in0=vtok[:, j, :].rearrange("p (h d) -> p h d", h=8),
                                                in1=rw_bon[:, j, :].unsqueeze(2).to_broadcast([128, 8, 64]), op=ALU.mult),
                r=[vtok, rw_bon], w=[junk])
            k.V(lambda: nc.vector.tensor_tensor(out=o[:, :], in0=o[:, :], in1=junk[:, 0:512], op=ALU.add), r=[o, junk], w=[o])
            k.V(lambda: nc.vector.tensor_tensor(out=o[:, :], in0=o[:, :], in1=gt[:, j, :], op=ALU.mult), r=[o, gt], w=[o])
            emit_out(3, l, blk, j, o, 512)

    gsb = Ring([k.sb([128, 512], name="gsb") for _ in range(2)])
    fng = k.sb([128, D], name="fng")
    UP = ["w_up_ret", "w_up_gla", "w_up_ssd", "w_up_rwkv", "w_up_mem"]

    def merge_block(l, blk, last):
        t0 = blk * BLK
        mh = [TK[0], TK[1]]
        first = True
        for bi in range(5):
            if BR[bi] not in branches:
                continue
            nr = 2 if bi == 4 else 4
            for hf in range(2):
                wg = load_w(dr["w_in"][l], C_GATES + bi * 1024 + hf * 512, 512)
                wu = load_w(dr[UP[bi]][l], hf * 512, 512, rows=nr)
                for j in range(NJ):
                    pg = psr.next()
                    for dc in range(8):
                        k.mm(pg[:, :], hT[:, dc, j * T:(j + 1) * T], wg[:, dc, :], start=(dc == 0), stop=(dc == 7),
                             r=[hT, wg], w=[pg], fast=True)
                    gs = gsb.next()
                    k.A(lambda: nc.scalar.activation(out=gs[:, :], in_=pg[:, :], func=AF.Sigmoid), r=[pg], w=[gs])
                    pu = psr.next()
                    for c in range(nr):
                        k.mm(pu[:, :], oT[bi][:, c, j * T:(j + 1) * T], wu[:, c, :], start=(c == 0), stop=(c == nr - 1),
                             r=[oT[bi], wu], w=[pu], fast=True)
                    if first:
                        k.V(lambda: nc.vector.tensor_tensor(out=mh[hf][:, j, :], in0=gs[:, :], in1=pu[:, :], op=ALU.mult),
                            r=[gs, pu], w=[mh[hf]])
                    else:
                        k.V(lambda: nc.vector.tensor_tensor(out=gs[:, :], in0=gs[:, :], in1=pu[:, :], op=ALU.mult),
                            r=[gs, pu], w=[gs])
                        k.V(lambda: nc.vector.tensor_tensor(out=mh[hf][:, j, :], in0=mh[hf][:, j, :], in1=gs[:, :], op=ALU.add),
                            r=[gs, mh[hf]], w=[mh[hf]])
            first = False
        if debug is not None and l == dbg_layer:
            for j in range(NJ):
                for hf in range(2):
                    k.dma("pool", dbg_d[t0 + j * T:t0 + (j + 1) * T, 2304 + hf * 512:2304 + (hf + 1) * 512], mh[hf][:, j, :],
                          r=[mh[hf]])
        for j in range(NJ):
            for hf in range(2):
                for c in range(4):
                    tr(hT[:, hf * 4 + c, j * T:(j + 1) * T], hT, mh[hf][:, j, c * 128:(c + 1) * 128], mh[hf], 128, 128,
                       eng=("act" if c % 2 == 0 else "dve"))
        for hf in range(2):
            wo = load_w(dr["w_out"][l], hf * 512, 512)
            for j in range(NJ):
                po = psr.next()
                for dc in range(8):
                    k.mm(po[:, :], hT[:, dc, j * T:(j + 1) * T], wo[:, dc, :], start=(dc == 0), stop=(dc == 7),
                         r=[hT, wo], w=[po], fast=True)
                k.V(lambda: nc.vector.tensor_tensor(out=xb[:, j, hf * 512:(hf + 1) * 512], in0=xb[:, j, hf * 512:(hf + 1) * 512],
                                                    in1=po[:, :], op=ALU.add), r=[xb, po], w=[xb])
        if debug is not None and l == dbg_layer:
            for j in range(NJ):
                k.dma("pool", dbg_d[t0 + j * T:t0 + (j + 1) * T, 3328:4352], xb[:, j, :], r=[xb])
        if pipe:
            return
        if not last:
            k.dma("pool", x1_d[t0:t0 + BLK, :].rearrange("(j p) d -> p j d", p=128), xb[:, :, :], r=[xb], w=[x1_b[blk]])
        else:
            for j in range(NJ):
                st = stt.next()
                k.A(lambda: nc.scalar.activation(out=junk[:, :], in_=xb[:, j, :], func=AF.Square, accum_out=st[:, 0:1]),
                    r=[xb], w=[junk, st])
                k.V(lambda: nc.vector.tensor_scalar(out=st[:, 1:2], in0=st[:, 0:1], scalar1=1.0 / D, scalar2=1e-6,
                                                    op0=ALU.mult, op1=ALU.add), r=[st], w=[st])
                k.A(lambda: nc.scalar.sqrt(out=st[:, 1:2], in_=st[:, 1:2]), r=[st], w=[st])
                k.V(lambda: nc.vector.reciprocal(out=st[:, 2:3], in_=st[:, 1:2]), r=[st], w=[st])
                k.V(lambda: nc.vector.scalar_tensor_tensor(out=xb[:, j, :], in0=xb[:, j, :], scalar=st[:, 2:3], in1=fng[:, :],
                                                           op0=ALU.mult, op1=ALU.mult), r=[xb, st, fng], w=[xb])
            k.dma("pool", out_d[t0:t0 + BLK, :].rearrange("(j p) d -> p j d", p=128), xb[:, :, :], r=[xb])

    BR = ["ret", "gla", "ssd", "rwkv", "mem"]
    k.dma("sp", fng[:, :], bc(dr["final_norm_g"][0:1, :], D), w=[fng])

    def front(l, pos0):
        st = ss
        for j in range(NJ):
            k.A(lambda: nc.scalar.activation(out=junk[:, :], in_=xb[:, j, :], func=AF.Square,
                                             accum_out=st[:, j:j + 1]), r=[xb], w=[junk, st])
        k.V(lambda: nc.vector.tensor_scalar(out=st[:, 4:4 + NJ], in0=st[:, 0:NJ], scalar1=1.0 / D, scalar2=1e-6,
                                            op0=ALU.mult, op1=ALU.add), r=[st], w=[st])
        k.A(lambda: nc.scalar.sqrt(out=st[:, 4:4 + NJ], in_=st[:, 4:4 + NJ]), r=[st], w=[st])
        k.V(lambda: nc.vector.reciprocal(out=st[:, 8:8 + NJ], in_=st[:, 4:4 + NJ]), r=[st], w=[st])
        for j in range(NJ):
            k.V(lambda: nc.vector.tensor_scalar(out=xn[:, :], in0=xb[:, j, :], scalar1=st[:, 8 + j:9 + j],
                                                scalar2=None, op0=ALU.mult), r=[xb, st], w=[xn])
            for half in range(2):
                pa = psr.next()
                for q in range(4):
                    dc = half * 4 + q
                    k.op("pe", lambda: nc.tensor.transpose(pa[:, q * T:(q + 1) * T], xn[:, dc * T:(dc + 1) * T], ident),
                         r=[xn, cst], w=[pa])
                for q in range(4):
                    dc = half * 4 + q
                    k.A(lambda: nc.scalar.activation(out=hT[:, dc, j * T:(j + 1) * T], in_=pa[:, q * T:(q + 1) * T],
                                                     func=AF.Identity, scale=gcol[:, dc:dc + 1]), r=[pa, gcol], w=[hT])
        k.dma("sp", posi[:, :], dr["positions"][:, pos0:pos0 + BLK], w=[posi])
        k.V(lambda: nc.vector.tensor_copy(out=posf[:, :], in_=posi[:, :]), r=[posi], w=[posf])
        pa = psr.next()
        k.mm(pa[0:64, 0:BLK], C("invrow", 0, 1), posf[0:1, :], r=[cst, posf], w=[pa])
        for tab, shift in ((sinT, math.pi), (cosT, 1.5 * math.pi)):
            k.V(lambda: nc.vector.tensor_scalar(out=tab[:, :], in0=pa[0:64, 0:BLK], scalar1=shift, scalar2=None,
                                                op0=ALU.add), r=[pa], w=[tab])
            k.V(lambda: nc.vector.tensor_scalar(out=rope_qi[:, :], in0=tab[:, :], scalar1=1.0 / TWO_PI, scalar2=None,
                                                op0=ALU.mult), r=[tab], w=[rope_qi])
            k.V(lambda: nc.vector.tensor_copy(out=rope_qf[:, :], in_=rope_qi[:, :]), r=[rope_qi], w=[rope_qf])
            k.V(lambda: nc.vector.scalar_tensor_tensor(out=tab[:, :], in0=rope_qf[:, :], scalar=-TWO_PI, in1=tab[:, :],
                                                       op0=ALU.mult, op1=ALU.add), r=[rope_qf, tab], w=[tab])
            k.V(lambda: nc.vector.tensor_scalar(out=rope_qf[:, :], in0=tab[:, :], scalar1=0.0, scalar2=TWO_PI,
                                                op0=ALU.is_lt, op1=ALU.mult), r=[tab], w=[rope_qf])
            k.V(lambda: nc.vector.tensor_tensor(out=tab[:, :], in0=tab[:, :], in1=rope_qf[:, :], op=ALU.add),
                r=[tab, rope_qf], w=[tab])
            k.V(lambda: nc.vector.tensor_scalar(out=tab[:, :], in0=tab[:, :], scalar1=0.0, scalar2=TWO_PI,
                                                op0=ALU.max, op1=ALU.min), r=[tab], w=[tab])
            k.A(lambda: nc.scalar.activation(out=tab[:, :], in_=tab[:, :], func=AF.Sin, bias=pi_c[0:64, :], scale=1.0),
                r=[tab, pi_c], w=[tab])

    def branches_and_merge(l, blk, last):
        if "ret" in branches:
            ret_block(l, blk)
        if "gla" in branches:
            gla_block(l, blk)
        if "ssd" in branches:
            ssd_block(l, blk)
        hoist = ("rwkv" in branches) and ("mem" in branches)
        if "rwkv" in branches:
            rwkv_block(l, blk, before_epilogue=(lambda: mem_proj(l, blk)) if hoist else None)
        if "mem" in branches:
            mem_block(l, blk, do_proj=not hoist)
        if do_merge:
            merge_block(l, blk, last=last)

    if not pipe:
        for l in range(nlayers):
            load_params(l)
            for blk in range(nblk):
                t0 = blk * BLK
                src = dr["x"] if l == 0 else x1_d
                rb = [] if l == 0 else [x1_b[blk]]
                k.dma("sp", xb[:, :, :], src[t0:t0 + BLK, :].rearrange("(j p) d -> p j d", p=128), r=rb, w=[xb])
                front(l, t0)
                branches_and_merge(l, blk, last=(l == nlayers - 1))
    else:
        role = k.sb([128, 16], name="role")
        k.dma("sp", role[:, :], role_d[:, :], w=[role])
        load_params(0)
        k.V(lambda: nc.vector.memset(xb[:, :, :], 0.0), w=[xb])
        tmpX = [(TK[0], TK[1]), (TK[2], TK[3])]
        for it in range(nblk + 1):
            for s_ in range(4):
                ta = tmpX[s_ % 2]
                for hf in range(2):
                    eng = k.V if hf == 0 else k.G
                    e_ = nc.vector if hf == 0 else nc.gpsimd
                    eng(lambda: e_.tensor_scalar(out=ta[hf][:, :, :], in0=xb[:, :, hf * 512:(hf + 1) * 512],
                                                 scalar1=role[:, 1 + s_:2 + s_], scalar2=None, op0=ALU.mult),
                        r=[xb, role], w=[ta[hf]])
                    k.dma("pool", cin_d[s_ * BLK:(s_ + 1) * BLK, hf * 512:(hf + 1) * 512].rearrange("(j p) d -> p j d", p=128),
                          ta[hf][:, :, :], r=[ta[hf]], w=[cin_b])
            k.collective(cin_d.opt(), cout_d.opt(), r=[cin_b], w=[cout_b])
            ba = min(it, nblk - 1)
            k.dma("sp", xb[:, :, :], dr["x"][ba * BLK:(ba + 1) * BLK, :].rearrange("(j p) d -> p j d", p=128), w=[xb])
            k.V(lambda: nc.vector.tensor_scalar(out=xb[:, :, :], in0=xb[:, :, :], scalar1=role[:, 0:1], scalar2=None,
                                                op0=ALU.mult), r=[xb, role], w=[xb])
            for s_ in range(4):
                ta = tmpX[s_ % 2]
                for hf in range(2):
                    k.dma("sp", ta[hf][:, :, :],
                          cout_d[s_ * BLK:(s_ + 1) * BLK, hf * 512:(hf + 1) * 512].rearrange("(j p) d -> p j d", p=128),
                          r=[cout_b], w=[ta[hf]])
                    k.V(lambda: nc.vector.scalar_tensor_tensor(out=xb[:, :, hf * 512:(hf + 1) * 512], in0=ta[hf][:, :, :],
                                                               scalar=role[:, 5 + s_:6 + s_],
                                                               in1=xb[:, :, hf * 512:(hf + 1) * 512],
                                                               op0=ALU.mult, op1=ALU.add), r=[ta[hf], role, xb], w=[xb])
            front(0, it * BLK)
            branches_and_merge(0, it, last=False)
            if it == 0:
                for s_ in ret_S + gla_S + rw_S + [ssd_S]:
                    np_ = s_.t.shape[0]
                    k.V(lambda: nc.vector.tensor_scalar(out=s_[:, :], in0=s_[:, :], scalar1=role[0:np_, 9:10], scalar2=None,
                                                        op0=ALU.mult), r=[s_, role], w=[s_])
            ob = max(it - 1, 0)
            on = tmpX[1]
            for j in range(NJ):
                st = stt.next()
                k.A(lambda: nc.scalar.activation(out=junk[:, :], in_=xb[:, j, :], func=AF.Square, accum_out=st[:, 0:1]),
                    r=[xb], w=[junk, st])
                k.V(lambda: nc.vector.tensor_scalar(out=st[:, 1:2], in0=st[:, 0:1], scalar1=1.0 / D, scalar2=1e-6,
                                                    op0=ALU.mult, op1=ALU.add), r=[st], w=[st])
                k.A(lambda: nc.scalar.sqrt(out=st[:, 1:2], in_=st[:, 1:2]), r=[st], w=[st])
                k.V(lambda: nc.vector.reciprocal(out=st[:, 2:3], in_=st[:, 1:2]), r=[st], w=[st])
                for hf in range(2):
                    k.V(lambda: nc.vector.scalar_tensor_tensor(out=on[hf][:, j, :], in0=xb[:, j, hf * 512:(hf + 1) * 512],
                                                               scalar=st[:, 2:3], in1=fng[:, hf * 512:(hf + 1) * 512],
                                                               op0=ALU.mult, op1=ALU.mult), r=[xb, st, fng], w=[on[hf]])
            for hf in range(2):
                k.dma("pool", out_d[ob * BLK:(ob + 1) * BLK, hf * 512:(hf + 1) * 512].rearrange("(j p) d -> p j d", p=128),
                      on[hf][:, :, :], r=[on[hf]], w=[out_b[ob]])
    k.finish()
    return nc, k


NCORES = 4
PIPE = False


def make_in_maps(inputs, nblk=SEQ // BLK):
    ntok = nblk * BLK
    maps = []
    if not PIPE:
        params = {n: np.ascontiguousarray(np.asarray(inputs[n], np.float32).reshape(SHAPES[n])) for n in PARAM_NAMES}
        for c in range(4):
            m = {"x": np.ascontiguousarray(inputs["x"][c]), "mem": np.ascontiguousarray(inputs["mem"][c]),
                 "positions": np.ascontiguousarray(inputs["positions"][c:c + 1]).astype(np.int32), "cst": CST}
            m.update(params)
            maps.append(m)
        return maps
    per_stage = []
    for stg in range(2):
        p = {}
        for n in PARAM_NAMES:
            a = np.asarray(inputs[n], np.float32).reshape(SHAPES[n])
            if n != "final_norm_g":
                a = a[stg:stg + 1]
            p[n] = np.ascontiguousarray(a)
        per_stage.append(p)
    for c in range(8):
        b, stg = c % 4, c // 4
        pos = np.asarray(inputs["positions"][b], np.int32)[:ntok]
        if stg == 0:
            pos2 = np.concatenate([pos, pos[ntok - BLK:ntok]])
        else:
            pos2 = np.concatenate([np.zeros(BLK, np.int32), pos])
        role = np.zeros((128, 16), np.float32)
        if stg == 0:
            role[:, 0] = 1.0
            role[:, 1 + b] = 1.0
            role[:, 9] = 1.0
        else:
            role[:, 5 + b] = 1.0
        m = {"x": np.ascontiguousarray(inputs["x"][b][:ntok]), "mem": np.ascontiguousarray(inputs["mem"][b]),
             "positions": np.ascontiguousarray(pos2[None, :]), "cst": CST, "role": role}
        m.update(per_stage[stg])
        maps.append(m)
    return maps


def kernel(**inputs):
    if PIPE:
        nc, k = build(nlayers=1, pipe=True)
        maps = make_in_maps(inputs)
        res = run_bass_kernel_spmd(nc, maps, core_ids=list(range(8)))
        out = np.stack([np.asarray(res.results[4 + b]["out"]) for b in range(4)], axis=0)
    else:
        nc, k = build()
        maps = make_in_maps(inputs)
        res = run_bass_kernel_spmd(nc, maps, core_ids=list(range(4)))
        out = np.stack([np.asarray(res.results[b]["out"]) for b in range(4)], axis=0)
    return out.astype(np.float32)
```

```python
import contextlib
import math
import numpy as np
import concourse.bass as bass
import concourse.mybir as mybir
from concourse.bass_utils import run_bass_kernel_spmd

F32 = mybir.dt.float32
F32R = mybir.dt.float32r
FAST = True
FAST_RW = True
I32 = mybir.dt.int32
AF = mybir.ActivationFunctionType
ALU = mybir.AluOpType
AX = mybir.AxisListType

SEQ = 4096
D = 1024
T = 128
BLK = 256
NJ = BLK // T
IN_TOTAL = 12184
C_RET_Q, C_RET_K, C_RET_V, C_RET_G = 0, 256, 512, 1024
C_GLA_Q, C_GLA_K, C_GLA_V, C_GLA_GK, C_GLA_G = 1536, 1792, 2048, 2560, 2576
C_SSD_XBC, C_SSD_DT, C_SSD_Z = 3088, 4112, 4120
C_RWKV_IN, C_RWKV_G, C_MEM_Q, C_GATES = 4632, 6296, 6808, 7064
SEM_LIMIT = 30000
TWO_PI = 2.0 * math.pi


class Buf:
    __slots__ = ("w", "r")

    def __init__(self):
        self.w = None
        self.r = {}


class Tl:
    def __init__(self, t):
        self.t = t
        self.b = Buf()

    def __getitem__(self, k):
        return self.t[k]


def _bufs(lst):
    out = []
    for x in lst:
        if x is None:
            continue
        out.append(x.b if isinstance(x, Tl) else x)
    return out


class KB:
    def __init__(self, nc):
        self.nc = nc
        self.es = contextlib.ExitStack()
        self.eng = {"pe": nc.tensor, "dve": nc.vector, "act": nc.scalar, "pool": nc.gpsimd, "sp": nc.sync}
        self.nsem = 0
        self.sem = {}
        self.cnt = {}
        for e in ("pe", "dve", "act", "pool"):
            self.sem[e] = self.newsem(e)
            self.cnt[e] = 0
        self.seen = {e: {} for e in self.eng}
        self.NS = 8
        self.dsem = {q: [self.newsem("d" + q) for _ in range(self.NS)] for q in ("sp", "pool")}
        self.dval = {q: [0] * self.NS for q in ("sp", "pool")}
        self.drr = {q: 0 for q in ("sp", "pool")}
        self.ntile = 0
        self.ninst = 0

    def newsem(self, name):
        self.nsem += 1
        return self.es.enter_context(self.nc.semaphore(f"{name}_{self.nsem}"))

    def sb(self, shape, dtype=F32, name=None):
        self.ntile += 1
        return Tl(self.es.enter_context(self.nc.sbuf_tensor(f"{name or 'sb'}_{self.ntile}", list(shape), dtype)))

    def ps(self, shape=(128, 512), dtype=F32, name=None):
        self.ntile += 1
        return Tl(self.es.enter_context(self.nc.psum_tensor(f"{name or 'ps'}_{self.ntile}", list(shape), dtype)))

    def _collect(self, e, reads, writes):
        need = {}

        def add(tok):
            if tok is None:
                return
            sem, val, te = tok
            if te == "pe" and e == "pe":
                return
            if self.seen[e].get(sem, 0) >= val:
                return
            if need.get(sem, 0) < val:
                need[sem] = val

        for b in reads:
            add(b.w)
        for b in writes:
            add(b.w)
            for tok in b.r.values():
                add(tok)
        return need

    def _mark(self, tok, reads, writes, e):
        for b in reads:
            b.r[e] = tok
        for b in writes:
            b.w = tok
            b.r = {}

    def op(self, e, emit, r=(), w=()):
        reads, writes = _bufs(r), _bufs(w)
        need = self._collect(e, reads, writes)
        items = list(need.items())
        eng = self.eng[e]
        for sem, val in items[:-1]:
            eng.wait_ge(sem, val)
            self.seen[e][sem] = val
        ins = emit()
        if items:
            sem, val = items[-1]
            ins._wait_ge(sem, val)
            self.seen[e][sem] = val
        if self.cnt[e] >= SEM_LIMIT:
            self.sem[e] = self.newsem(e)
            self.cnt[e] = 0
        self.cnt[e] += 1
        ins.then_inc(self.sem[e], 1)
        self.ninst += 1
        self._mark((self.sem[e], self.cnt[e], e), reads, writes, e)
        return ins

    def dma(self, q, out, in_, r=(), w=(), **kw):
        reads, writes = _bufs(r), _bufs(w)
        need = self._collect(q, reads, writes)
        slot = self.drr[q] % self.NS
        self.drr[q] += 1
        sem = self.dsem[q][slot]
        prev = self.dval[q][slot]
        if prev > 0 and self.seen[q].get(sem, 0) < prev:
            need[sem] = max(need.get(sem, 0), prev)
        eng = self.eng[q]
        for s, v in need.items():
            eng.wait_ge(s, v)
            self.seen[q][s] = v
        eng.dma_start(out=out, in_=in_, **kw).then_inc(sem, 16)
        self.dval[q][slot] = prev + 16
        self.ninst += 1
        self._mark((sem, prev + 16, "dma"), reads, writes, "dma_" + q + str(slot))

    def collective(self, in_ap, out_ap, r=(), w=()):
        e = "pool"
        reads, writes = _bufs(r), _bufs(w)
        need = self._collect(e, reads, writes)
        eng = self.eng[e]
        for sem, val in need.items():
            eng.wait_ge(sem, val)
            self.seen[e][sem] = val
        if not hasattr(self, "cc_sem"):
            self.cc_sem = self.newsem("cc")
            self.cc_cnt = 0
        ins = self.nc.gpsimd.collective_compute("AllReduce", ALU.add, replica_groups=[list(range(8))],
                                                ins=[in_ap], outs=[out_ap])
        self.cc_cnt += 1
        ins.then_inc(self.cc_sem)
        self.ninst += 1
        self._mark((self.cc_sem, self.cc_cnt, "cc"), reads, writes, "cc")

    def finish(self):
        sp = self.nc.sync
        for q in ("sp", "pool"):
            for s, v in zip(self.dsem[q], self.dval[q]):
                if v > 0:
                    sp.wait_ge(s, v)
        for e in ("pe", "dve", "act", "pool"):
            if self.cnt[e] > 0:
                sp.wait_ge(self.sem[e], self.cnt[e])
        if hasattr(self, "cc_sem"):
            sp.wait_ge(self.cc_sem, self.cc_cnt)

    def mm(self, out, lhsT, rhs, start=True, stop=True, r=(), w=(), fast=False):
        nc = self.nc
        if fast and FAST:
            assert lhsT.dtype == F32R and rhs.dtype == F32R
        else:
            if lhsT.dtype == F32R:
                lhsT = lhsT.bitcast(F32)
            if rhs.dtype == F32R:
                rhs = rhs.bitcast(F32)
        return self.op("pe", lambda: nc.tensor.matmul(out, lhsT, rhs, start=start, stop=stop), r, w)

    def V(self, fn, r=(), w=()):
        return self.op("dve", fn, r, w)

    def A(self, fn, r=(), w=()):
        return self.op("act", fn, r, w)

    def G(self, fn, r=(), w=()):
        return self.op("pool", fn, r, w)


class Ring:
    def __init__(self, tiles):
        self.tiles = tiles
        self.i = 0

    def next(self):
        t = self.tiles[self.i % len(self.tiles)]
        self.i += 1
        return t


def make_consts():
    cols = {}
    parts = []
    off = [0]

    def add(name, arr):
        arr = np.asarray(arr, np.float32)
        assert arr.shape[0] == 128
        arr = arr.reshape(128, -1)
        cols[name] = (off[0], arr.shape[1])
        parts.append(arr)
        off[0] += arr.shape[1]

    i = np.arange(128)
    add("ident", np.eye(128))
    add("maskT", (i[:, None] <= i[None, :]))
    add("maskS", (i[:, None] < i[None, :]))
    add("maskL", (i[:, None] > i[None, :]))
    add("ones", np.ones((128, 128)))
    add("negm", np.where(i[:, None] > i[None, :], -30000.0, 0.0))
    inv = 1.0 / (10000.0 ** np.linspace(0.0, 1.0, 32, dtype=np.float32))
    invrow = np.zeros((128, 64), np.float32)
    invrow[0, :] = np.repeat(inv.astype(np.float32), 2)
    add("invrow", invrow)
    rot = np.zeros((128, 64), np.float32)
    for p in range(32):
        rot[2 * p + 1, 2 * p] = -1.0
        rot[2 * p, 2 * p + 1] = 1.0
    add("rot", rot)
    lg = np.log(1.0 - 2.0 ** (-5.0 - np.arange(4, dtype=np.float64)))
    dec = np.zeros((128, 4, 128))
    for h in range(4):
        dec[:, h, :] = np.where(i[:, None] <= i[None, :], np.exp(lg[h] * (i[None, :] - i[:, None])), 0.0)
    add("retdec", dec)
    qs = np.zeros((128, 4, 128))
    for h in range(4):
        qs[:, h, :] = np.exp(lg[h] * (i[None, :] + 1))
    add("retqs", qs)
    ks = np.zeros((128, 4))
    for h in range(4):
        ks[:, h] = np.exp(lg[h] * (127 - i))
    add("retks", ks)
    bo = np.zeros((128, 128))
    bo[:64, :64] = 1
    bo[64:, 64:] = 1
    add("blk64", bo)
    hs = np.zeros((128, 2))
    hs[:64, 0] = 1
    hs[64:, 1] = 1
    add("headsel", hs)
    return np.concatenate(parts, axis=1), cols, [float(np.exp(lg[h] * 128)) for h in range(4)]


CST, CCOL, RET_SDEC = make_consts()

PARAM_NAMES = ["norm_g", "w_in", "gla_gk_w2", "gla_gk_b", "gla_norm_g", "ssd_conv_w", "ssd_conv_b",
               "ssd_dt_bias", "ssd_a_log", "ssd_d", "ssd_norm_g", "rwkv_mu", "rwkv_w0", "rwkv_w2",
               "rwkv_a0", "rwkv_a2", "rwkv_k_k", "rwkv_k_a", "rwkv_r_k", "rwkv_ln_g", "rwkv_ln_b",
               "mem_norm_g", "w_mem_kv", "w_up_ret", "w_up_gla", "w_up_ssd", "w_up_rwkv", "w_up_mem",
               "w_out", "final_norm_g"]
SHAPES = {
    "x": [SEQ, D], "mem": [256, D], "positions": [1, SEQ],
    "norm_g": [2, D], "w_in": [2, D, IN_TOTAL], "gla_gk_w2": [2, 16, 256], "gla_gk_b": [2, 256],
    "gla_norm_g": [2, 128], "ssd_conv_w": [2, 4, 1024], "ssd_conv_b": [2, 1024], "ssd_dt_bias": [2, 8],
    "ssd_a_log": [2, 8], "ssd_d": [2, 8], "ssd_norm_g": [2, 512], "rwkv_mu": [2, 1664],
    "rwkv_w0": [2, 512], "rwkv_w2": [2, 64, 512], "rwkv_a0": [2, 512], "rwkv_a2": [2, 64, 512],
    "rwkv_k_k": [2, 512], "rwkv_k_a": [2, 512], "rwkv_r_k": [2, 512], "rwkv_ln_g": [2, 512],
    "rwkv_ln_b": [2, 512], "mem_norm_g": [2, D], "w_mem_kv": [2, D, 512], "w_up_ret": [2, 512, D],
    "w_up_gla": [2, 512, D], "w_up_ssd": [2, 512, D], "w_up_rwkv": [2, 512, D], "w_up_mem": [2, 256, D],
    "w_out": [2, D, D], "final_norm_g": [1, D],
}


def build(nblk=SEQ // BLK, nlayers=2, debug=None, branches=("ret", "gla", "ssd", "rwkv", "mem"), dbg_what=0, dbg_layer=0, do_merge=True, pipe=False):
    nc = bass.Bass("TRN2", target_bir_lowering=False)
    k = KB(nc)
    dr = {}
    ntok_all = nblk * BLK
    for n, shp in SHAPES.items():
        shp = list(shp)
        if n in ("x",):
            shp[0] = ntok_all
        elif n == "positions":
            shp[1] = ntok_all + (BLK if pipe else 0)
        elif n not in ("mem", "final_norm_g"):
            shp[0] = nlayers
        dr[n] = nc.dram_tensor(n, shp, I32 if n == "positions" else F32, kind="ExternalInput").ap()
    cst_d = nc.dram_tensor("cst", list(CST.shape), F32, kind="ExternalInput").ap()
    out_d = nc.dram_tensor("out", [ntok_all, D], F32, kind="ExternalOutput").ap()
    x1_d = nc.dram_tensor("x1s", [ntok_all, D], F32, kind="Internal").ap()
    x1_b = [Buf() for _ in range(SEQ // BLK)]
    if pipe:
        role_d = nc.dram_tensor("role", [128, 16], F32, kind="ExternalInput").ap()
        cin_d = nc.dram_tensor("cin", [4 * BLK, D], F32, kind="Internal").ap()
        cout_d = nc.dram_tensor("cout", [4 * BLK, D], F32, kind="Internal").ap()
        cin_b, cout_b = Buf(), Buf()
        out_b = [Buf() for _ in range(SEQ // BLK)]
    dbg_d = None
    if debug is not None:
        dbg_d = nc.dram_tensor("dbg", [ntok_all, debug], F32, kind="ExternalOutput").ap()

    cst = k.sb([128, CST.shape[1]], name="cst")
    k.dma("sp", cst[:, :], cst_d[:, :], w=[cst])

    def C(name, p0=0, p1=128):
        o, n = CCOL[name]
        return cst[p0:p1, o:o + n]

    ident = C("ident")


    psr = Ring([k.ps() for _ in range(6)])
    pacc = [k.ps(name='pacc') for _ in range(2)]
    ntok = nblk * BLK
    NCH = SEQ // BLK

    def bc(ap_row, n):
        return ap_row.to_broadcast([128, n])

    xb = k.sb([128, NJ, D], name="xb")
    hT = k.sb([128, 8, BLK], F32R if FAST else F32, name="hT")
    xn = k.sb([128, D], name="xn")
    junk = k.sb([128, D], name="junk")
    ss = k.sb([128, 16], name="ss")
    wst = Ring([k.sb([128, 8, 512], F32R if FAST else F32, name="wst") for _ in range(3)])
    TK = [k.sb([128, NJ, 512], name="TK") for _ in range(4)]
    FM = [k.sb([128, BLK], name="FM") for _ in range(16)]
    sq = Ring([k.sb([128, 128], name="sq") for _ in range(16)])
    sq2 = Ring([k.sb([128, 256], name="sq2") for _ in range(6)])
    stt = Ring([k.sb([128, 32], name="st") for _ in range(8)])
    oT = [k.sb([128, 4, BLK], F32R if FAST else F32, name="oT") for _ in range(4)] + [k.sb([128, 2, BLK], F32R if FAST else F32, name="oTm")]
    obr = Ring([k.sb([128, 512], name="obr") for _ in range(4)])
    posi = k.sb([1, BLK], I32, name="posi")
    posf = k.sb([1, BLK], name="posf")
    cosT = k.sb([64, BLK], name="cosT")
    sinT = k.sb([64, BLK], name="sinT")
    rope_qi = k.sb([64, BLK], I32, name="rope_qi")
    rope_qf = k.sb([64, BLK], name="rope_qf")
    pi_c = k.sb([128, 1], name="pi_c")
    k.V(lambda: nc.vector.memset(pi_c[:, :], -math.pi), w=[pi_c])
    gcol = k.sb([128, 8], name="gcol")
    gla_w2 = k.sb([16, 256], name="gla_w2")
    gla_b = k.sb([1, 256], name="gla_b")
    gla_ng = k.sb([128, 128], name="gla_ng")
    ssd_cw = k.sb([128, 8, 4], name="ssd_cw")
    ssd_cb = k.sb([128, 8], name="ssd_cb")
    ssd_sm = k.sb([128, 32], name="ssd_sm")
    ssd_ng = k.sb([128, 512], name="ssd_ng")
    ssd_carry = k.sb([128, 8, 4], name="ssd_carry")
    rw_cols = k.sb([128, 64], name="rw_cols")
    rw_w2a2 = k.sb([128, 512], name="rw_w2a2")
    rw_lng = k.sb([128, 512], name="rw_lng")
    rw_lnb = k.sb([128, 512], name="rw_lnb")
    rw_carry = k.sb([128, 16], name="rw_carry")
    kmT = k.sb([64, 4, 256], name="kmT")
    vm = k.sb([128, 2, 256], name="vm")
    ret_S = [k.sb([64, 128], name="ret_S") for _ in range(4)]
    gla_S = [k.sb([64, 128], name="gla_S") for _ in range(4)]
    ssd_S = k.sb([128, 512], name="ssd_S")
    rw_S = [k.sb([128, 64], name="rw_S") for _ in range(4)]

    def load_w(wd, c0, ncol, rows=8, r0=0):
        wt = wst.next()
        k.dma("pool" if FAST else "sp", wt[:, 0:rows, 0:ncol],
              wd[r0 * 128:(r0 + rows) * 128, :].rearrange("(c p) n -> p c n", p=128)[:, :, c0:c0 + ncol], w=[wt])
        return wt

    def proj_fm(l, c0, ncol, evac):
        nc_ = 128 if FAST else ncol
        wt = load_w(dr["w_in"][l], c0, nc_)
        pa = psr.next()
        for dc in range(8):
            k.mm(pa[0:nc_, 0:BLK], wt[:, dc, 0:nc_], hT[:, dc, :], start=(dc == 0), stop=(dc == 7), r=[wt, hT], w=[pa],
                 fast=(nc_ == 128))
        evac(pa)

    def proj_tok(l, c0, ncol, evac):
        for g0 in range(0, ncol, 512):
            w_ = min(512, ncol - g0)
            wt = load_w(dr["w_in"][l], c0 + g0, w_)
            for j in range(NJ):
                pa = psr.next()
                for dc in range(8):
                    k.mm(pa[:, 0:w_], hT[:, dc, j * T:(j + 1) * T], wt[:, dc, 0:w_], start=(dc == 0), stop=(dc == 7),
                         r=[wt, hT], w=[pa], fast=(w_ % 2 == 0))
                evac(j, g0, w_, pa)

    def tr(dst_ap, dst_tl, src_ap, src_tl, npart, nfree, eng="act"):
        pa = psr.next()
        k.op("pe", lambda: nc.tensor.transpose(pa[0:nfree, 0:npart], src_ap, ident[0:npart, 0:npart]),
             r=[src_tl, cst], w=[pa])
        if eng == "act":
            k.A(lambda: nc.scalar.copy(out=dst_ap, in_=pa[0:nfree, 0:npart]), r=[pa], w=[dst_tl])
        else:
            k.V(lambda: nc.vector.tensor_copy(out=dst_ap, in_=pa[0:nfree, 0:npart]), r=[pa], w=[dst_tl])

    def rstd_groups(y_tl, y_ap_of, n, w, eps, st, c0):
        for g in range(n):
            k.A(lambda: nc.scalar.activation(out=junk[:, 0:w], in_=y_ap_of(g), func=AF.Square,
                                             accum_out=st[:, c0 + g:c0 + g + 1]), r=[y_tl], w=[junk, st])
        k.V(lambda: nc.vector.tensor_scalar(out=st[:, c0 + n:c0 + 2 * n], in0=st[:, c0:c0 + n], scalar1=1.0 / w,
                                            scalar2=eps, op0=ALU.mult, op1=ALU.add), r=[st], w=[st])
        k.A(lambda: nc.scalar.sqrt(out=st[:, c0 + n:c0 + 2 * n], in_=st[:, c0 + n:c0 + 2 * n]), r=[st], w=[st])
        k.V(lambda: nc.vector.reciprocal(out=st[:, c0 + 2 * n:c0 + 3 * n], in_=st[:, c0 + n:c0 + 2 * n]), r=[st], w=[st])

    def softplus_ip(x_ap, x_tl, n):
        a = sq2.next()
        k.A(lambda: nc.scalar.activation(out=a[:, 0:n], in_=x_ap, func=AF.Abs), r=[x_tl], w=[a])
        k.A(lambda: nc.scalar.activation(out=a[:, 0:n], in_=a[:, 0:n], func=AF.Exp, scale=-1.0), r=[a], w=[a])
        k.A(lambda: nc.scalar.activation(out=a[:, 0:n], in_=a[:, 0:n], func=AF.Ln, bias=1.0), r=[a], w=[a])
        k.V(lambda: nc.vector.scalar_tensor_tensor(out=x_ap, in0=x_ap, scalar=0.0, in1=a[:, 0:n], op0=ALU.max,
                                                   op1=ALU.add), r=[x_tl, a], w=[x_tl])

    def emit_out(bi, l, blk, j, o, width):
        t0 = blk * BLK
        for c in range(width // 128):
            tr(oT[bi][:, c, j * T:(j + 1) * T], oT[bi], o[:, c * 128:(c + 1) * 128], o, 128, 128,
               eng=("act" if c % 2 == 0 else "dve"))
        if debug is not None and l == dbg_layer:
            k.dma("pool", dbg_d[t0 + j * T:t0 + (j + 1) * T, bi * 512:bi * 512 + width], o[:, 0:width], r=[o])

    def load_params(l):
        NCg = dict(allow_slow_non_contiguous=True)
        k.dma("sp", gcol[:, :], dr["norm_g"][l].rearrange("(c p) -> p c", p=128), w=[gcol], **NCg)
        k.dma("sp", gla_w2[:, :], dr["gla_gk_w2"][l], w=[gla_w2])
        k.dma("sp", gla_b[:, :], dr["gla_gk_b"][l:l + 1, :], w=[gla_b])
        k.dma("sp", gla_ng[:, :], bc(dr["gla_norm_g"][l:l + 1, :], 128), w=[gla_ng])
        for j_ in range(4):
            k.dma("sp", ssd_cw[:, :, j_], dr["ssd_conv_w"][l, j_].rearrange("(c p) -> p c", p=128), w=[ssd_cw], **NCg)
        k.dma("sp", ssd_cb[:, :], dr["ssd_conv_b"][l].rearrange("(c p) -> p c", p=128), w=[ssd_cb], **NCg)
        k.dma("sp", ssd_sm[:, 0:8], bc(dr["ssd_dt_bias"][l:l + 1, :], 8), w=[ssd_sm])
        k.dma("sp", ssd_sm[:, 8:16], bc(dr["ssd_a_log"][l:l + 1, :], 8), w=[ssd_sm])
        k.dma("sp", ssd_sm[:, 16:24], bc(dr["ssd_d"][l:l + 1, :], 8), w=[ssd_sm])
        k.A(lambda: nc.scalar.activation(out=ssd_sm[:, 8:16], in_=ssd_sm[:, 8:16], func=AF.Exp), r=[ssd_sm], w=[ssd_sm])
        k.V(lambda: nc.vector.tensor_scalar(out=ssd_sm[:, 8:16], in0=ssd_sm[:, 8:16], scalar1=-1.0, scalar2=None,
                                            op0=ALU.mult), r=[ssd_sm], w=[ssd_sm])
        k.dma("sp", ssd_ng[:, :], bc(dr["ssd_norm_g"][l:l + 1, :], 512), w=[ssd_ng])
        k.dma("sp", rw_cols[:, 0:13], dr["rwkv_mu"][l].rearrange("(c p) -> p c", p=128), w=[rw_cols], **NCg)
        for i_, nm in enumerate(["rwkv_w0", "rwkv_a0", "rwkv_k_k", "rwkv_k_a", "rwkv_r_k"]):
            k.dma("sp", rw_cols[:, 16 + 4 * i_:20 + 4 * i_], dr[nm][l].rearrange("(c p) -> p c", p=128), w=[rw_cols], **NCg)
        k.V(lambda: nc.vector.tensor_scalar(out=rw_cols[:, 40:53], in0=rw_cols[:, 0:13], scalar1=-1.0, scalar2=1.0,
                                            op0=ALU.mult, op1=ALU.add), r=[rw_cols], w=[rw_cols])
        k.dma("sp", rw_w2a2[0:64, :], dr["rwkv_w2"][l], w=[rw_w2a2])
        k.dma("sp", rw_w2a2[64:128, :], dr["rwkv_a2"][l], w=[rw_w2a2])
        k.dma("sp", rw_lng[:, :], bc(dr["rwkv_ln_g"][l:l + 1, :], 512), w=[rw_lng])
        k.dma("sp", rw_lnb[:, :], bc(dr["rwkv_ln_b"][l:l + 1, :], 512), w=[rw_lnb])
        mg = stt.next()
        k.dma("sp", mg[:, 0:8], dr["mem_norm_g"][l].rearrange("(c p) -> p c", p=128), w=[mg], **NCg)
        memT = FM[0:8]
        for mt in range(2):
            k.dma("sp", xn[:, :], dr["mem"][mt * 128:(mt + 1) * 128, :], w=[xn])
            st = stt.next()
            k.A(lambda: nc.scalar.activation(out=junk[:, :], in_=xn[:, :], func=AF.Square, accum_out=st[:, 0:1]),
                r=[xn], w=[junk, st])
            k.V(lambda: nc.vector.tensor_scalar(out=st[:, 1:2], in0=st[:, 0:1], scalar1=1.0 / D, scalar2=1e-6,
                                                op0=ALU.mult, op1=ALU.add), r=[st], w=[st])
            k.A(lambda: nc.scalar.sqrt(out=st[:, 1:2], in_=st[:, 1:2]), r=[st], w=[st])
            k.V(lambda: nc.vector.reciprocal(out=st[:, 2:3], in_=st[:, 1:2]), r=[st], w=[st])
            k.V(lambda: nc.vector.tensor_scalar(out=xn[:, :], in0=xn[:, :], scalar1=st[:, 2:3], scalar2=None,
                                                op0=ALU.mult), r=[xn, st], w=[xn])
            for dc in range(8):
                pa = psr.next()
                k.op("pe", lambda: nc.tensor.transpose(pa[:, 0:T], xn[:, dc * T:(dc + 1) * T], ident), r=[xn, cst], w=[pa])
                k.A(lambda: nc.scalar.activation(out=memT[dc][:, mt * T:(mt + 1) * T], in_=pa[:, 0:T], func=AF.Identity,
                                                 scale=mg[:, dc:dc + 1]), r=[pa, mg], w=[memT[dc]])
        wt = load_w(dr["w_mem_kv"][l], 0, 512)
        for h in range(4):
            pa = psr.next()
            for dc in range(8):
                k.mm(pa[0:64, 0:256], wt[:, dc, h * 64:(h + 1) * 64], memT[dc][:, 0:256], start=(dc == 0), stop=(dc == 7),
                     r=[wt, memT[dc]], w=[pa])
            k.A(lambda: nc.scalar.copy(out=kmT[:, h, :], in_=pa[0:64, 0:256]), r=[pa], w=[kmT])
        for mt in range(2):
            pa = psr.next()
            for dc in range(8):
                k.mm(pa[:, 0:256], memT[dc][:, mt * T:(mt + 1) * T], wt[:, dc, 256:512], start=(dc == 0), stop=(dc == 7),
                     r=[wt, memT[dc]], w=[pa])
            k.A(lambda: nc.scalar.copy(out=vm[:, mt, :], in_=pa[:, 0:256]), r=[pa], w=[vm])
        for s_ in ret_S + gla_S + rw_S + [ssd_S]:
            k.V(lambda: nc.vector.memset(s_[:, :], 0.0), w=[s_])
        k.V(lambda: nc.vector.memset(ssd_carry[:, :, :], 0.0), w=[ssd_carry])
        k.V(lambda: nc.vector.memset(rw_carry[:, :], 0.0), w=[rw_carry])

    def la_step(ks_ap, ks_tl, qs_ap, qs_tl, qi_ap, qi_tl, mask_ap, mask_tl, v_ap, v_tl, S, py_ap, py):
        psc = psr.next()
        k.mm(psc[:, 0:T], ks_ap, qs_ap, r=[ks_tl, qs_tl], w=[psc])
        sc = sq.next()
        k.V(lambda: nc.vector.tensor_tensor(out=sc[:, :], in0=psc[:, 0:T], in1=mask_ap, op=ALU.mult),
            r=[psc, mask_tl], w=[sc])
        k.mm(py_ap, sc[:, :], v_ap, start=True, stop=False, r=[sc, v_tl], w=[py])
        k.mm(py_ap, qi_ap, S[:, :], start=False, stop=True, r=[qi_tl, S], w=[py])

    def ret_block(l, blk):
        t0 = blk * BLK
        qT, kT = FM[0:4], FM[4:8]
        vt, gt = TK[0], TK[1]
        for c_base, dst, scale in ((C_RET_Q, qT, 1.0), (C_RET_K, kT, 0.125)):
            for h in range(4):
                def ev(pa, h=h, dst=dst, scale=scale):
                    ta, tb = FM[8], FM[9]
                    k.A(lambda: nc.scalar.activation(out=ta[0:64, :], in_=pa[0:64, 0:BLK], func=AF.Copy, scale=scale),
                        r=[pa], w=[ta])
                    pb = psr.next()
                    k.mm(pb[0:64, 0:BLK], C("rot", 0, 64), ta[0:64, :], r=[cst, ta], w=[pb])
                    k.V(lambda: nc.vector.tensor_tensor(out=tb[0:64, :], in0=pb[0:64, 0:BLK], in1=sinT[:, :], op=ALU.mult),
                        r=[pb, sinT], w=[tb])
                    k.V(lambda: nc.vector.tensor_tensor(out=ta[0:64, :], in0=ta[0:64, :], in1=cosT[:, :], op=ALU.mult),
                        r=[ta, cosT], w=[ta])
                    k.V(lambda: nc.vector.tensor_tensor(out=dst[h][0:64, :], in0=ta[0:64, :], in1=tb[0:64, :], op=ALU.add),
                        r=[ta, tb], w=[dst[h]])
                proj_fm(l, c_base + 64 * h, 64, ev)
        proj_tok(l, C_RET_V, 512, lambda j, g0, w_, pa: k.A(
            lambda: nc.scalar.copy(out=vt[:, j, g0:g0 + w_], in_=pa[:, 0:w_]), r=[pa], w=[vt]))
        proj_tok(l, C_RET_G, 512, lambda j, g0, w_, pa: k.A(
            lambda: nc.scalar.activation(out=gt[:, j, g0:g0 + w_], in_=pa[:, 0:w_], func=AF.Silu), r=[pa], w=[gt]))
        ko, _ = CCOL["retks"]
        for j in range(NJ):
            py = pacc[j % 2]
            for hg in range(2):
                hs = (2 * hg, 2 * hg + 1)
                qs_ = {h: qT[h][0:64, j * T:(j + 1) * T] for h in hs}
                ks_ = {h: kT[h][0:64, j * T:(j + 1) * T] for h in hs}
                vh = {h: vt[:, j, h * 128:(h + 1) * 128] for h in hs}
                qi, psc, sc, kt, pt, pds = {}, {}, {}, {}, {}, {}
                for h in hs:
                    psc[h] = psr.next()
                    k.mm(psc[h][:, 0:T], ks_[h], qs_[h], r=[kT[h], qT[h]], w=[psc[h]])
                for h in hs:
                    pt[h] = psr.next()
                    k.op("pe", lambda: nc.tensor.transpose(pt[h][:, 0:64], ks_[h], ident[0:64, 0:64]), r=[kT[h], cst], w=[pt[h]])
                for h in hs:
                    qi[h] = sq.next()
                    k.V(lambda: nc.vector.tensor_tensor(out=qi[h][0:64, :], in0=qs_[h],
                                                        in1=C("retqs", 0, 64)[:, h * T:(h + 1) * T], op=ALU.mult),
                        r=[qT[h], cst], w=[qi[h]])
                    sc[h] = sq.next()
                    k.V(lambda: nc.vector.tensor_tensor(out=sc[h][:, :], in0=psc[h][:, 0:T],
                                                        in1=C("retdec")[:, h * T:(h + 1) * T], op=ALU.mult),
                        r=[psc[h], cst], w=[sc[h]])
                    kt[h] = sq.next()
                    k.A(lambda: nc.scalar.activation(out=kt[h][:, 0:64], in_=pt[h][:, 0:64], func=AF.Identity,
                                                     scale=cst[:, ko + h:ko + h + 1]), r=[pt[h], cst], w=[kt[h]])
                for h in hs:
                    k.mm(py[:, h * 128:(h + 1) * 128], sc[h][:, :], vh[h], start=True, stop=False, r=[sc[h], vt], w=[py])
                    k.mm(py[:, h * 128:(h + 1) * 128], qi[h][0:64, :], ret_S[h][:, :], start=False, stop=True,
                         r=[qi[h], ret_S[h]], w=[py])
                for h in hs:
                    pds[h] = psr.next()
                    k.mm(pds[h][0:64, 0:128], kt[h][:, 0:64], vh[h], r=[kt[h], vt], w=[pds[h]])
                for h in hs:
                    k.V(lambda: nc.vector.scalar_tensor_tensor(out=ret_S[h][:, :], in0=ret_S[h][:, :], scalar=RET_SDEC[h],
                                                               in1=pds[h][0:64, 0:128], op0=ALU.mult, op1=ALU.add),
                        r=[ret_S[h], pds[h]], w=[ret_S[h]])
            st = stt.next()
            rstd_groups(py, lambda g: py[:, g * 128:(g + 1) * 128], 4, 128, 1e-6, st, 0)
            o = obr.next()
            for h in range(4):
                k.V(lambda: nc.vector.scalar_tensor_tensor(out=o[:, h * 128:(h + 1) * 128], in0=py[:, h * 128:(h + 1) * 128],
                                                           scalar=st[:, 8 + h:9 + h], in1=gt[:, j, h * 128:(h + 1) * 128],
                                                           op0=ALU.mult, op1=ALU.mult), r=[py, st, gt], w=[o])
            emit_out(0, l, blk, j, o, 512)

    def gla_block(l, blk):
        qT, kT = FM[0:4], FM[4:8]
        vt, gt = TK[0], TK[1]
        glow = FM[8]
        for h in range(4):
            proj_fm(l, C_GLA_Q + 64 * h, 64, lambda pa, h=h: k.A(
                lambda: nc.scalar.activation(out=qT[h][0:64, :], in_=pa[0:64, 0:BLK], func=AF.Copy, scale=0.125),
                r=[pa], w=[qT[h]]))
            proj_fm(l, C_GLA_K + 64 * h, 64, lambda pa, h=h: k.A(
                lambda: nc.scalar.copy(out=kT[h][0:64, :], in_=pa[0:64, 0:BLK]), r=[pa], w=[kT[h]]))
        proj_fm(l, C_GLA_GK, 16, lambda pa: k.A(
            lambda: nc.scalar.copy(out=glow[0:16, :], in_=pa[0:16, 0:BLK]), r=[pa], w=[glow]))
        proj_tok(l, C_GLA_V, 512, lambda j, g0, w_, pa: k.A(
            lambda: nc.scalar.copy(out=vt[:, j, g0:g0 + w_], in_=pa[:, 0:w_]), r=[pa], w=[vt]))
        proj_tok(l, C_GLA_G, 512, lambda j, g0, w_, pa: k.A(
            lambda: nc.scalar.activation(out=gt[:, j, g0:g0 + w_], in_=pa[:, 0:w_], func=AF.Silu), r=[pa], w=[gt]))
        for j in range(NJ):
            pl = psr.next()
            k.mm(pl[:, 0:256], glow[0:16, j * T:(j + 1) * T], gla_w2[:, :], start=True, stop=False, r=[glow, gla_w2], w=[pl])
            k.mm(pl[:, 0:256], C("ones", 0, 1), gla_b[:, :], start=False, stop=True, r=[cst, gla_b], w=[pl])
            lg = sq2.next()
            k.A(lambda: nc.scalar.activation(out=lg[:, :], in_=pl[:, 0:256], func=AF.Copy, scale=-1.0), r=[pl], w=[lg])
            softplus_ip(lg[:, :], lg, 256)
            k.V(lambda: nc.vector.tensor_scalar(out=lg[:, :], in0=lg[:, :], scalar1=-1.0 / 16.0, scalar2=None,
                                                op0=ALU.mult), r=[lg], w=[lg])
            py = pacc[j % 2]
            for hg in range(2):
                hs = (2 * hg, 2 * hg + 1)
                qs_ = {h: qT[h][0:64, j * T:(j + 1) * T] for h in hs}
                ks_ = {h: kT[h][0:64, j * T:(j + 1) * T] for h in hs}
                vh = {h: vt[:, j, h * 128:(h + 1) * 128] for h in hs}
                pc, sth, ekin, eqin, eq, psc, sc, kt, pds = {}, {}, {}, {}, {}, {}, {}, {}, {}
                for h in hs:
                    pc[h] = psr.next()
                    k.mm(pc[h][0:64, 0:T], lg[:, h * 64:(h + 1) * 64], C("maskT"), r=[lg, cst], w=[pc[h]])
                for h in hs:
                    st = sth[h] = stt.next()
                    p_ = pc[h]
                    k.V(lambda: nc.vector.tensor_copy(out=st[0:64, 0:1], in_=p_[0:64, 64:65]), r=[p_], w=[st])
                    k.V(lambda: nc.vector.tensor_scalar(out=st[0:64, 1:2], in0=p_[0:64, 64:65], scalar1=-1.0, scalar2=None,
                                                        op0=ALU.mult), r=[p_], w=[st])
                    ekin[h], eqin[h], eq[h] = sq.next(), sq.next(), sq.next()
                    k.A(lambda: nc.scalar.activation(out=ekin[h][0:64, :], in_=p_[0:64, 0:T], func=AF.Exp, scale=-1.0,
                                                     bias=st[0:64, 0:1]), r=[p_, st], w=[ekin[h]])
                    k.A(lambda: nc.scalar.activation(out=eqin[h][0:64, :], in_=p_[0:64, 0:T], func=AF.Exp, scale=1.0,
                                                     bias=st[0:64, 1:2]), r=[p_, st], w=[eqin[h]])
                    k.A(lambda: nc.scalar.activation(out=eq[h][0:64, :], in_=p_[0:64, 0:T], func=AF.Exp), r=[p_], w=[eq[h]])
                    k.A(lambda: nc.scalar.activation(out=st[0:64, 2:3], in_=p_[0:64, T - 1:T], func=AF.Exp), r=[p_], w=[st])
                    k.A(lambda: nc.scalar.activation(out=st[0:64, 3:4], in_=p_[0:64, T - 1:T], func=AF.Exp, scale=1.0,
                                                     bias=st[0:64, 1:2]), r=[p_, st], w=[st])
                for h in hs:
                    k.V(lambda: nc.vector.tensor_tensor(out=ekin[h][0:64, :], in0=ekin[h][0:64, :], in1=ks_[h], op=ALU.mult),
                        r=[ekin[h], kT[h]], w=[ekin[h]])
                    k.V(lambda: nc.vector.tensor_tensor(out=eqin[h][0:64, :], in0=eqin[h][0:64, :], in1=qs_[h], op=ALU.mult),
                        r=[eqin[h], qT[h]], w=[eqin[h]])
                    k.V(lambda: nc.vector.tensor_tensor(out=eq[h][0:64, :], in0=eq[h][0:64, :], in1=qs_[h], op=ALU.mult),
                        r=[eq[h], qT[h]], w=[eq[h]])
                for h in hs:
                    psc[h] = psr.next()
                    k.mm(psc[h][:, 0:T], ekin[h][0:64, :], eqin[h][0:64, :], r=[ekin[h], eqin[h]], w=[psc[h]])
                for h in hs:
                    kt[h] = sq.next()
                    tr(kt[h][:, 0:64], kt[h], ekin[h][0:64, :], ekin[h], 64, 128)
                for h in hs:
                    sc[h] = sq.next()
                    k.V(lambda: nc.vector.tensor_tensor(out=sc[h][:, :], in0=psc[h][:, 0:T], in1=C("maskT"), op=ALU.mult),
                        r=[psc[h], cst], w=[sc[h]])
                for h in hs:
                    k.mm(py[:, h * 128:(h + 1) * 128], sc[h][:, :], vh[h], start=True, stop=False, r=[sc[h], vt], w=[py])
                    k.mm(py[:, h * 128:(h + 1) * 128], eq[h][0:64, :], gla_S[h][:, :], start=False, stop=True,
                         r=[eq[h], gla_S[h]], w=[py])
                for h in hs:
                    pds[h] = psr.next()
                    k.mm(pds[h][0:64, 0:128], kt[h][:, 0:64], vh[h], r=[kt[h], vt], w=[pds[h]])
                for h in hs:
                    st = sth[h]
                    k.V(lambda: nc.vector.tensor_scalar(out=gla_S[h][:, :], in0=gla_S[h][:, :], scalar1=st[0:64, 2:3],
                                                        scalar2=None, op0=ALU.mult), r=[gla_S[h], st], w=[gla_S[h]])
                    k.V(lambda: nc.vector.scalar_tensor_tensor(out=gla_S[h][:, :], in0=pds[h][0:64, 0:128], scalar=st[0:64, 3:4],
                                                               in1=gla_S[h][:, :], op0=ALU.mult, op1=ALU.add),
                        r=[gla_S[h], pds[h], st], w=[gla_S[h]])
            st = stt.next()
            rstd_groups(py, lambda g: py[:, g * 128:(g + 1) * 128], 4, 128, 1e-6, st, 0)
            o = obr.next()
            for h in range(4):
                k.V(lambda: nc.vector.scalar_tensor_tensor(out=o[:, h * 128:(h + 1) * 128], in0=py[:, h * 128:(h + 1) * 128],
                                                           scalar=st[:, 8 + h:9 + h], in1=gt[:, j, h * 128:(h + 1) * 128],
                                                           op0=ALU.mult, op1=ALU.mult), r=[py, st, gt], w=[o])
                k.V(lambda: nc.vector.tensor_tensor(out=o[:, h * 128:(h + 1) * 128], in0=o[:, h * 128:(h + 1) * 128],
                                                    in1=gla_ng[:, :], op=ALU.mult), r=[o, gla_ng], w=[o])
            emit_out(1, l, blk, j, o, 512)

    def ssd_block(l, blk):
        cv = FM[0:8]
        zt, dtt = TK[0], TK[1]
        raw = FM[8]
        for c in range(8):
            def ev(pa, c=c):
                k.A(lambda: nc.scalar.copy(out=raw[:, :], in_=pa[:, 0:BLK]), r=[pa], w=[raw])
                acc = cv[c]
                k.V(lambda: nc.vector.tensor_scalar(out=acc[:, :], in0=raw[:, :], scalar1=ssd_cw[:, c, 3:4], scalar2=None,
                                                    op0=ALU.mult), r=[raw, ssd_cw], w=[acc])
                for d_ in (1, 2, 3):
                    k.V(lambda: nc.vector.scalar_tensor_tensor(out=acc[:, d_:BLK], in0=raw[:, 0:BLK - d_],
                                                               scalar=ssd_cw[:, c, 3 - d_:4 - d_], in1=acc[:, d_:BLK],
                                                               op0=ALU.mult, op1=ALU.add), r=[raw, ssd_cw, acc], w=[acc])
                    k.V(lambda: nc.vector.scalar_tensor_tensor(out=acc[:, 0:d_], in0=ssd_carry[:, c, 3 - d_:3],
                                                               scalar=ssd_cw[:, c, 3 - d_:4 - d_], in1=acc[:, 0:d_],
                                                               op0=ALU.mult, op1=ALU.add),
                        r=[ssd_carry, ssd_cw, acc], w=[acc])
                k.V(lambda: nc.vector.tensor_copy(out=ssd_carry[:, c, 0:3], in_=raw[:, BLK - 3:BLK]), r=[raw], w=[ssd_carry])
                k.A(lambda: nc.scalar.activation(out=acc[:, :], in_=acc[:, :], func=AF.Silu, bias=ssd_cb[:, c:c + 1],
                                                 scale=1.0), r=[acc, ssd_cb], w=[acc])
            proj_fm(l, C_SSD_XBC + 128 * c, 128, ev)
        proj_tok(l, C_SSD_Z, 512, lambda j, g0, w_, pa: k.A(
            lambda: nc.scalar.activation(out=zt[:, j, g0:g0 + w_], in_=pa[:, 0:w_], func=AF.Silu), r=[pa], w=[zt]))

        def ev_dt(j, g0, w_, pa):
            k.V(lambda: nc.vector.tensor_tensor(out=dtt[:, j, 0:8], in0=pa[:, 0:8], in1=ssd_sm[:, 0:8], op=ALU.add),
                r=[pa, ssd_sm], w=[dtt])
            softplus_ip(dtt[:, j, 0:8], dtt, 8)
        proj_tok(l, C_SSD_DT, 8, ev_dt)
        for j in range(NJ):
            xs = obr.next()
            for c in range(4):
                tr(xs[:, c * 128:(c + 1) * 128], xs, cv[c][:, j * T:(j + 1) * T], cv[c], 128, 128,
                   eng=("act" if c % 2 == 0 else "dve"))
            btok = [sq.next(), sq.next()]
            for g in range(2):
                tr(btok[g][:, :], btok[g], cv[4 + g][:, j * T:(j + 1) * T], cv[4 + g], 128, 128)
            st = stt.next()
            k.V(lambda: nc.vector.tensor_tensor(out=st[:, 0:8], in0=dtt[:, j, 0:8], in1=ssd_sm[:, 8:16], op=ALU.mult),
                r=[dtt, ssd_sm], w=[st])
            pq = psr.next()
            k.mm(pq[:, 0:8], C("maskT"), st[:, 0:8], r=[cst, st], w=[pq])
            k.mm(pq[:, 8:16], C("maskL"), st[:, 0:8], r=[cst, st], w=[pq])
            k.mm(pq[:, 16:24], C("ones"), st[:, 0:8], r=[cst, st], w=[pq])
            k.A(lambda: nc.scalar.activation(out=st[:, 8:32], in_=pq[:, 0:24], func=AF.Exp), r=[pq], w=[st])
            rhsM = [sq2.next() for _ in range(4)]
            for h in range(8):
                k.V(lambda: nc.vector.tensor_scalar(out=rhsM[h // 2][:, (h % 2) * T:(h % 2 + 1) * T], in0=C("maskT"),
                                                    scalar1=st[:, h:h + 1], scalar2=None, op0=ALU.mult),
                    r=[cst, st], w=[rhsM[h // 2]])
            v1 = sq2.next(), sq2.next()
            st2 = stt.next()
            k.V(lambda: nc.vector.tensor_tensor(out=st2[:, 0:8], in0=dtt[:, j, 0:8], in1=st[:, 16:24], op=ALU.mult),
                r=[dtt, st], w=[st2])
            for g in range(2):
                k.V(lambda: nc.vector.tensor_tensor(
                    out=v1[g][:, :].rearrange("p (h d) -> p h d", h=4),
                    in0=xs[:, g * 256:(g + 1) * 256].rearrange("p (h d) -> p h d", h=4),
                    in1=dtt[:, j, g * 4:(g + 1) * 4].unsqueeze(2).to_broadcast([128, 4, 64]), op=ALU.mult),
                    r=[xs, dtt], w=[v1[g]])
            k.V(lambda: nc.vector.tensor_tensor(
                out=junk[:, 0:512].rearrange("p (h d) -> p h d", h=8),
                in0=xs[:, :].rearrange("p (h d) -> p h d", h=8),
                in1=st2[:, 0:8].unsqueeze(2).to_broadcast([128, 8, 64]), op=ALU.mult), r=[xs, st2], w=[junk])
            pyi = pacc[0]
            pye = pacc[1]
            for g in range(2):
                bT = cv[4 + g][:, j * T:(j + 1) * T]
                cT = cv[6 + g][:, j * T:(j + 1) * T]
                psc = psr.next()
                k.mm(psc[:, 0:T], bT, cT, r=[cv[4 + g], cv[6 + g]], w=[psc])
                scg = sq.next()
                k.A(lambda: nc.scalar.copy(out=scg[:, :], in_=psc[:, 0:T]), r=[psc], w=[scg])
                pdd, exx = {}, {}
                for hh in range(4):
                    h = g * 4 + hh
                    pdd[hh] = pd = psr.next()
                    k.mm(pd[:, 0:T], C("maskL"), rhsM[h // 2][:, (h % 2) * T:(h % 2 + 1) * T], start=True, stop=False,
                         r=[cst, rhsM[h // 2]], w=[pd])
                    k.mm(pd[:, 0:T], ident, C("negm"), start=False, stop=True, r=[cst], w=[pd])
                for hh in range(4):
                    exx[hh] = ex = sq.next()
                    pd = pdd[hh]
                    k.A(lambda: nc.scalar.activation(out=ex[:, :], in_=pd[:, 0:T], func=AF.Exp), r=[pd], w=[ex])
                    k.V(lambda: nc.vector.tensor_tensor(out=ex[:, :], in0=ex[:, :], in1=scg[:, :], op=ALU.mult),
                        r=[ex, scg], w=[ex])
                for hh in range(4):
                    h = g * 4 + hh
                    ex = exx[hh]
                    k.mm(pyi[:, h * 64:(h + 1) * 64], ex[:, :], v1[g][:, hh * 64:(hh + 1) * 64], r=[ex, v1[g]], w=[pyi])
                k.mm(pye[:, g * 256:(g + 1) * 256], cT, ssd_S[:, g * 256:(g + 1) * 256], r=[cv[6 + g], ssd_S], w=[pye])
                pds = psr.next()
                k.mm(pds[:, 0:256], btok[g][:, :], junk[:, g * 256:(g + 1) * 256], r=[btok[g], junk], w=[pds])
                sg = ssd_S[:, g * 256:(g + 1) * 256].rearrange("p (h d) -> p h d", h=4)
                k.V(lambda: nc.vector.tensor_tensor(out=sg, in0=sg,
                                                    in1=st[:, 24 + g * 4:28 + g * 4].unsqueeze(2).to_broadcast([128, 4, 64]),
                                                    op=ALU.mult), r=[ssd_S, st], w=[ssd_S])
                k.V(lambda: nc.vector.tensor_tensor(out=ssd_S[:, g * 256:(g + 1) * 256], in0=ssd_S[:, g * 256:(g + 1) * 256],
                                                    in1=pds[:, 0:256], op=ALU.add), r=[ssd_S, pds], w=[ssd_S])
            o = obr.next()
            k.A(lambda: nc.scalar.copy(out=o[:, :], in_=pyi[:, :]), r=[pyi], w=[o])
            y3 = o[:, :].rearrange("p (h d) -> p h d", h=8)
            tmp = junk[:, 512:1024]
            k.V(lambda: nc.vector.tensor_tensor(out=tmp.rearrange("p (h d) -> p h d", h=8),
                                                in0=pye[:, :].rearrange("p (h d) -> p h d", h=8),
                                                in1=st[:, 8:16].unsqueeze(2).to_broadcast([128, 8, 64]), op=ALU.mult),
                r=[pye, st], w=[junk])
            k.V(lambda: nc.vector.tensor_tensor(out=o[:, :], in0=o[:, :], in1=tmp, op=ALU.add), r=[o, junk], w=[o])
            k.V(lambda: nc.vector.tensor_tensor(out=tmp.rearrange("p (h d) -> p h d", h=8),
                                                in0=xs[:, :].rearrange("p (h d) -> p h d", h=8),
                                                in1=ssd_sm[:, 16:24].unsqueeze(2).to_broadcast([128, 8, 64]), op=ALU.mult),
                r=[xs, ssd_sm], w=[junk])
            k.V(lambda: nc.vector.tensor_tensor(out=o[:, :], in0=o[:, :], in1=tmp, op=ALU.add), r=[o, junk], w=[o])
            k.V(lambda: nc.vector.tensor_tensor(out=o[:, :], in0=o[:, :], in1=zt[:, j, :], op=ALU.mult), r=[o, zt], w=[o])
            st3 = stt.next()
            rstd_groups(o, lambda g: o[:, g * 256:(g + 1) * 256], 2, 256, 1e-6, st3, 0)
            for g in range(2):
                k.V(lambda: nc.vector.scalar_tensor_tensor(out=o[:, g * 256:(g + 1) * 256], in0=o[:, g * 256:(g + 1) * 256],
                                                           scalar=st3[:, 4 + g:5 + g], in1=ssd_ng[:, g * 256:(g + 1) * 256],
                                                           op0=ALU.mult, op1=ALU.mult), r=[o, st3, ssd_ng], w=[o])
            emit_out(2, l, blk, j, o, 512)

    def mem_proj(l, blk):
        qT = FM[0:4]
        for h in range(4):
            proj_fm(l, C_MEM_Q + 64 * h, 64, lambda pa, h=h: k.A(
                lambda: nc.scalar.activation(out=qT[h][0:64, :], in_=pa[0:64, 0:BLK], func=AF.Copy, scale=0.125),
                r=[pa], w=[qT[h]]))

    def mem_block(l, blk, do_proj=True):
        qT = FM[0:4]
        if do_proj:
            mem_proj(l, blk)
        for j in range(NJ):
            st = stt.next()
            po = pacc[j % 2]
            o = obr.next()
            for hg in range(2):
                hs = (2 * hg, 2 * hg + 1)
                psc, pe_, pT = {}, {}, {}
                for h in hs:
                    psc[h] = psr.next()
                    k.mm(psc[h][:, 0:256], qT[h][0:64, j * T:(j + 1) * T], kmT[:, h, :], r=[qT[h], kmT], w=[psc[h]])
                for h in hs:
                    k.V(lambda: nc.vector.tensor_reduce(out=st[:, h:h + 1], in_=psc[h][:, 0:256], axis=AX.X, op=ALU.max),
                        r=[psc[h]], w=[st])
                    k.V(lambda: nc.vector.tensor_scalar(out=st[:, 4 + h:5 + h], in0=st[:, h:h + 1], scalar1=-1.0, scalar2=None,
                                                        op0=ALU.mult), r=[st], w=[st])
                    pe_[h] = sq2.next()
                    k.A(lambda: nc.scalar.activation(out=pe_[h][:, :], in_=psc[h][:, 0:256], func=AF.Exp,
                                                     bias=st[:, 4 + h:5 + h], scale=1.0, accum_out=st[:, 8 + h:9 + h]),
                        r=[psc[h], st], w=[pe_[h], st])
                for h in hs:
                    pT[h] = [sq.next(), sq.next()]
                    for mt in range(2):
                        tr(pT[h][mt][:, :], pT[h][mt], pe_[h][:, mt * T:(mt + 1) * T], pe_[h], 128, 128,
                           eng=("act" if mt == 0 else "dve"))
                for h in hs:
                    for mt in range(2):
                        k.mm(po[:, h * 64:(h + 1) * 64], pT[h][mt][:, :], vm[:, mt, h * 64:(h + 1) * 64], start=(mt == 0),
                             stop=(mt == 1), r=[pT[h][mt], vm], w=[po])
            k.V(lambda: nc.vector.reciprocal(out=st[:, 12:16], in_=st[:, 8:12]), r=[st], w=[st])
            k.V(lambda: nc.vector.tensor_tensor(out=o[:, 0:256].rearrange("p (h d) -> p h d", h=4),
                                                in0=po[:, 0:256].rearrange("p (h d) -> p h d", h=4),
                                                in1=st[:, 12:16].unsqueeze(2).to_broadcast([128, 4, 64]), op=ALU.mult),
                r=[po, st], w=[o])
            emit_out(4, l, blk, j, o, 256)

    RWD = F32R if (FAST and FAST_RW) else F32
    rwt = [k.sb([128, 128], F32 if i_ < 4 else RWD, name="rwt") for i_ in range(26)]
    rwx = [k.sb([128, 128], RWD, name="rwx") for _ in range(2)]
    rw_tmpS = k.sb([128, 64], name="rw_tmpS")

    def f32(ap):
        return ap.bitcast(F32) if ap.dtype == F32R else ap
    rw_AR = k.sb([128, 256], RWD, name="rw_AR")
    rw_bon = k.sb([128, NJ, 8], name="rw_bon")

    def rwkv_block(l, blk, before_epilogue=None):
        xr, xk, xv, ldt, at, kkt, tmpt, k2t, raw, bvt, prod = FM[0], FM[1], FM[2], FM[3], FM[4], FM[5], FM[6], FM[7], FM[8], FM[9], FM[10]
        waT = FM[12]
        gt, ytok, vtok = TK[0], TK[2], TK[3]

        def shift_ev(dst, cidx):
            def ev(pa):
                k.A(lambda: nc.scalar.copy(out=raw[:, :], in_=pa[:, 0:BLK]), r=[pa], w=[raw])
                k.V(lambda: nc.vector.tensor_scalar(out=dst[:, :], in0=raw[:, :], scalar1=rw_cols[:, 40 + cidx:41 + cidx],
                                                    scalar2=None, op0=ALU.mult), r=[raw, rw_cols], w=[dst])
                k.V(lambda: nc.vector.scalar_tensor_tensor(out=dst[:, 1:BLK], in0=raw[:, 0:BLK - 1],
                                                           scalar=rw_cols[:, cidx:cidx + 1], in1=dst[:, 1:BLK],
                                                           op0=ALU.mult, op1=ALU.add), r=[raw, rw_cols, dst], w=[dst])
                k.V(lambda: nc.vector.scalar_tensor_tensor(out=dst[:, 0:1], in0=rw_carry[:, cidx:cidx + 1],
                                                           scalar=rw_cols[:, cidx:cidx + 1], in1=dst[:, 0:1],
                                                           op0=ALU.mult, op1=ALU.add), r=[rw_carry, rw_cols, dst], w=[dst])
                k.V(lambda: nc.vector.tensor_copy(out=rw_carry[:, cidx:cidx + 1], in_=raw[:, BLK - 1:BLK]),
                    r=[raw], w=[rw_carry])
            return ev

        proj_tok(l, C_RWKV_G, 512, lambda j, g0, w_, pa: k.A(
            lambda: nc.scalar.activation(out=gt[:, j, g0:g0 + w_], in_=pa[:, 0:w_], func=AF.Silu), r=[pa], w=[gt]))
        proj_fm(l, C_RWKV_IN + 12 * 128, 128, shift_ev(waT, 12))
        k.A(lambda: nc.scalar.activation(out=waT[0:64, :], in_=waT[0:64, :], func=AF.Tanh), r=[waT], w=[waT])
        for p in range(4):
            proj_fm(l, C_RWKV_IN + p * 128, 128, shift_ev(xr, p))
            proj_fm(l, C_RWKV_IN + (4 + p) * 128, 128, shift_ev(xk, 4 + p))
            proj_fm(l, C_RWKV_IN + (8 + p) * 128, 128, shift_ev(xv, 8 + p))
            pz = psr.next()
            k.mm(pz[:, 0:BLK], rw_w2a2[0:64, p * 128:(p + 1) * 128], waT[0:64, :], r=[rw_w2a2, waT], w=[pz])
            k.A(lambda: nc.scalar.activation(out=ldt[:, :], in_=pz[:, 0:BLK], func=AF.Sigmoid, bias=rw_cols[:, 16 + p:17 + p],
                                             scale=1.0), r=[pz, rw_cols], w=[ldt])
            k.V(lambda: nc.vector.tensor_scalar(out=ldt[:, :], in0=ldt[:, :], scalar1=-math.exp(-0.5), scalar2=None,
                                                op0=ALU.mult), r=[ldt], w=[ldt])
            pa_ = psr.next()
            k.mm(pa_[:, 0:BLK], rw_w2a2[64:128, p * 128:(p + 1) * 128], waT[64:128, :], r=[rw_w2a2, waT], w=[pa_])
            k.A(lambda: nc.scalar.activation(out=at[:, :], in_=pa_[:, 0:BLK], func=AF.Sigmoid, bias=rw_cols[:, 20 + p:21 + p],
                                             scale=1.0), r=[pa_, rw_cols], w=[at])
            k.V(lambda: nc.vector.tensor_scalar(out=kkt[:, :], in0=xk[:, :], scalar1=rw_cols[:, 24 + p:25 + p], scalar2=None,
                                                op0=ALU.mult), r=[xk, rw_cols], w=[kkt])
            k.V(lambda: nc.vector.tensor_tensor(out=tmpt[:, :], in0=kkt[:, :], in1=kkt[:, :], op=ALU.mult), r=[kkt], w=[tmpt])
            pn = psr.next()
            k.mm(pn[:, 0:BLK], C("blk64"), tmpt[:, :], r=[cst, tmpt], w=[pn])
            k.A(lambda: nc.scalar.sqrt(out=tmpt[:, :], in_=pn[:, 0:BLK]), r=[pn], w=[tmpt])
            k.V(lambda: nc.vector.tensor_scalar(out=tmpt[:, :], in0=tmpt[:, :], scalar1=1e-12, scalar2=None, op0=ALU.max),
                r=[tmpt], w=[tmpt])
            k.V(lambda: nc.vector.reciprocal(out=tmpt[:, :], in_=tmpt[:, :]), r=[tmpt], w=[tmpt])
            k.V(lambda: nc.vector.tensor_tensor(out=kkt[:, :], in0=kkt[:, :], in1=tmpt[:, :], op=ALU.mult), r=[kkt, tmpt], w=[kkt])
            k.V(lambda: nc.vector.tensor_scalar(out=tmpt[:, :], in0=at[:, :], scalar1=rw_cols[:, 28 + p:29 + p], scalar2=-1.0,
                                                op0=ALU.mult, op1=ALU.mult), r=[at, rw_cols], w=[tmpt])
            k.V(lambda: nc.vector.tensor_scalar(out=tmpt[:, :], in0=tmpt[:, :], scalar1=rw_cols[:, 28 + p:29 + p], scalar2=-1.0,
                                                op0=ALU.add, op1=ALU.add), r=[tmpt, rw_cols], w=[tmpt])
            k.V(lambda: nc.vector.scalar_tensor_tensor(out=k2t[:, :], in0=tmpt[:, :], scalar=-1.0, in1=xk[:, :],
                                                       op0=ALU.mult, op1=ALU.mult), r=[tmpt, xk], w=[k2t])
            k.V(lambda: nc.vector.tensor_tensor(out=bvt[:, :], in0=kkt[:, :], in1=at[:, :], op=ALU.mult), r=[kkt, at], w=[bvt])
            k.V(lambda: nc.vector.scalar_tensor_tensor(out=prod[:, :], in0=xr[:, :], scalar=rw_cols[:, 32 + p:33 + p],
                                                       in1=k2t[:, :], op0=ALU.mult, op1=ALU.mult),
                r=[xr, rw_cols, k2t], w=[prod])
            for j in range(NJ):
                js = slice(j * T, (j + 1) * T)
                (cum, epos, eneg, eposx, Bt, Kt, Btok, Ktok, Vt, S0p) = rwt[0:10]
                pb = psr.next()
                k.mm(pb[:, 0:2], prod[:, js], C("headsel"), r=[prod, cst], w=[pb])
                k.A(lambda: nc.scalar.copy(out=rw_bon[:, j, 2 * p:2 * p + 2], in_=pb[:, 0:2]), r=[pb], w=[rw_bon])
                k.V(lambda: nc.vector.tensor_tensor_scan(out=cum[:, :], data0=C("ones"), data1=ldt[:, js], initial=0.0,
                                                         op0=ALU.mult, op1=ALU.add), r=[cst, ldt], w=[cum])
                st = stt.next()
                k.V(lambda: nc.vector.tensor_copy(out=st[:, 0:1], in_=cum[:, 63:64]), r=[cum], w=[st])
                k.V(lambda: nc.vector.tensor_scalar(out=st[:, 1:2], in0=cum[:, 63:64], scalar1=-1.0, scalar2=None,
                                                    op0=ALU.mult), r=[cum], w=[st])
                k.A(lambda: nc.scalar.activation(out=epos[:, :], in_=cum[:, :], func=AF.Exp, bias=st[:, 1:2], scale=1.0),
                    r=[cum, st], w=[epos])
                k.A(lambda: nc.scalar.activation(out=eneg[:, :], in_=cum[:, :], func=AF.Exp, bias=st[:, 0:1], scale=-1.0),
                    r=[cum, st], w=[eneg])
                k.V(lambda: nc.vector.tensor_tensor(out=eposx[:, :], in0=cum[:, :], in1=ldt[:, js], op=ALU.subtract),
                    r=[cum, ldt], w=[eposx])
                k.A(lambda: nc.scalar.activation(out=eposx[:, :], in_=eposx[:, :], func=AF.Exp, bias=st[:, 1:2], scale=1.0),
                    r=[eposx, st], w=[eposx])
                k.A(lambda: nc.scalar.activation(out=st[:, 2:3], in_=st[:, 0:1], func=AF.Exp), r=[st], w=[st])
                k.A(lambda: nc.scalar.activation(out=st[:, 3:4], in_=cum[:, T - 1:T], func=AF.Exp, bias=st[:, 1:2], scale=1.0),
                    r=[cum, st], w=[st])
                k.V(lambda: nc.vector.scalar_tensor_tensor(out=rw_AR[:, 0:T], in0=kkt[:, js], scalar=-1.0, in1=eposx[:, :],
                                                           op0=ALU.mult, op1=ALU.mult), r=[kkt, eposx], w=[rw_AR])
                k.V(lambda: nc.vector.tensor_tensor(out=rw_AR[:, T:2 * T], in0=xr[:, js], in1=epos[:, :], op=ALU.mult),
                    r=[xr, epos], w=[rw_AR])
                k.V(lambda: nc.vector.tensor_tensor(out=Bt[:, :], in0=bvt[:, js], in1=eneg[:, :], op=ALU.mult),
                    r=[bvt, eneg], w=[Bt])
                k.V(lambda: nc.vector.tensor_tensor(out=Kt[:, :], in0=k2t[:, js], in1=eneg[:, :], op=ALU.mult),
                    r=[k2t, eneg], w=[Kt])
                k.V(lambda: nc.vector.tensor_scalar(out=S0p[:, 0:64], in0=rw_S[p][:, :], scalar1=st[:, 2:3], scalar2=None,
                                                    op0=ALU.mult), r=[rw_S[p], st], w=[S0p])
                tr(Btok[:, :], Btok, f32(Bt[:, :]), Bt, 128, 128, eng="act")
                tr(Ktok[:, :], Ktok, f32(Kt[:, :]), Kt, 128, 128, eng="dve")
                tr(Vt[:, :], Vt, xv[:, js], xv, 128, 128, eng="act")
                k.A(lambda: nc.scalar.copy(out=vtok[:, j, p * 128:(p + 1) * 128], in_=f32(Vt[:, :])), r=[Vt], w=[vtok])
                HS = []
                for hh in range(2):
                    r0 = 64 * hh
                    d = dict(rs=slice(r0, r0 + 64), X=rwt[10 + 8 * hh], XT=rwt[11 + 8 * hh], Xn=rwt[12 + 8 * hh],
                             XTn=rwt[13 + 8 * hh], Z=rwt[14 + 8 * hh], Zn=rwt[15 + 8 * hh], Mbr=rwt[16 + 8 * hh],
                             Nak=rwt[17 + 8 * hh], Mkr=rwx[hh], hh=hh)
                    HS.append(d)
                for d in HS:
                    rs = d["rs"]
                    pN = psr.next()
                    k.mm(pN[:, 0:2 * T], Bt[rs, :], rw_AR[rs, :], r=[Bt, rw_AR], w=[pN], fast=FAST_RW)
                    pK = psr.next()
                    k.mm(pK[:, 0:2 * T], Kt[rs, :], rw_AR[rs, :], r=[Kt, rw_AR], w=[pK], fast=FAST_RW)
                    pX = psr.next()
                    k.mm(pX[:, 0:T], rw_AR[rs, 0:T], Bt[rs, :], r=[Bt, rw_AR], w=[pX], fast=FAST_RW)
                    k.V(lambda: nc.vector.tensor_tensor(out=d["X"][:, :], in0=pN[:, 0:T], in1=C("maskS"), op=ALU.mult),
                        r=[pN, cst], w=[d["X"]])
                    k.V(lambda: nc.vector.tensor_tensor(out=d["Mbr"][:, :], in0=pN[:, T:2 * T], in1=C("maskT"), op=ALU.mult),
                        r=[pN, cst], w=[d["Mbr"]])
                    k.V(lambda: nc.vector.tensor_tensor(out=d["Nak"][:, :], in0=pK[:, 0:T], in1=C("maskS"), op=ALU.mult),
                        r=[pK, cst], w=[d["Nak"]])
                    k.V(lambda: nc.vector.tensor_tensor(out=d["Mkr"][:, :], in0=pK[:, T:2 * T], in1=C("maskT"), op=ALU.mult),
                        r=[pK, cst], w=[d["Mkr"]])
                    k.V(lambda: nc.vector.tensor_tensor(out=d["XT"][:, :], in0=pX[:, 0:T], in1=C("maskL"), op=ALU.mult),
                        r=[pX, cst], w=[d["XT"]])
                for d in HS:
                    rs = d["rs"]
                    pW = psr.next()
                    k.mm(pW[:, 0:64], rw_AR[rs, 0:T], S0p[rs, 0:64], start=True, stop=False, r=[rw_AR, S0p], w=[pW], fast=FAST_RW)
                    k.mm(pW[:, 0:64], d["Nak"][:, :], Vt[:, rs], start=False, stop=True, r=[d["Nak"], Vt], w=[pW], fast=FAST_RW)
                    k.A(lambda: nc.scalar.copy(out=d["Z"][:, 0:64], in_=pW[:, 0:64]), r=[pW], w=[d["Z"]])
                for lev in range(7):
                    for d in HS:
                        X, XT, Xn, XTn, Z, Zn = d["X"], d["XT"], d["Xn"], d["XTn"], d["Z"], d["Zn"]
                        pZ = psr.next()
                        k.mm(pZ[:, 0:64], X[:, :], Z[:, 0:64], r=[X, Z], w=[pZ], fast=FAST_RW)
                        k.V(lambda: nc.vector.tensor_tensor(out=Zn[:, 0:64], in0=f32(Z[:, 0:64]), in1=pZ[:, 0:64], op=ALU.add),
                            r=[Z, pZ], w=[Zn])
                        d["Z"], d["Zn"] = Zn, Z
                        if lev < 6:
                            p1 = psr.next()
                            k.mm(p1[:, 0:T], XT[:, :], X[:, :], r=[XT, X], w=[p1], fast=FAST_RW)
                            p2 = psr.next()
                            k.mm(p2[:, 0:T], X[:, :], XT[:, :], r=[XT, X], w=[p2], fast=FAST_RW)
                            k.A(lambda: nc.scalar.copy(out=Xn[:, :], in_=p1[:, 0:T]), r=[p1], w=[Xn])
                            if d["hh"] == 0:
                                k.V(lambda: nc.vector.tensor_copy(out=XTn[:, :], in_=p2[:, 0:T]), r=[p2], w=[XTn])
                            else:
                                k.A(lambda: nc.scalar.copy(out=XTn[:, :], in_=p2[:, 0:T]), r=[p2], w=[XTn])
                            d["X"], d["Xn"] = Xn, X
                            d["XT"], d["XTn"] = XTn, XT
                for d in HS:
                    rs = d["rs"]
                    U = d["Z"]
                    pY = psr.next()
                    k.mm(pY[:, 0:64], rw_AR[rs, T:2 * T], S0p[rs, 0:64], start=True, stop=False, r=[rw_AR, S0p], w=[pY], fast=FAST_RW)
                    k.mm(pY[:, 0:64], d["Mbr"][:, :], U[:, 0:64], start=False, stop=False, r=[d["Mbr"], U], w=[pY], fast=FAST_RW)
                    k.mm(pY[:, 0:64], d["Mkr"][:, :], Vt[:, rs], start=False, stop=True, r=[d["Mkr"], Vt], w=[pY], fast=FAST_RW)
                    hcol = (2 * p + d["hh"]) * 64
                    k.A(lambda: nc.scalar.copy(out=ytok[:, j, hcol:hcol + 64], in_=pY[:, 0:64]), r=[pY], w=[ytok])
                    pS = psr.next()
                    k.mm(pS[:, 0:64], Btok[:, :], U[:, 0:64], start=True, stop=False, r=[Btok, U], w=[pS], fast=FAST_RW)
                    k.mm(pS[:, 0:64], Ktok[:, :], Vt[:, rs], start=False, stop=True, r=[Ktok, Vt], w=[pS], fast=FAST_RW)
                    k.V(lambda: nc.vector.tensor_tensor(out=rw_tmpS[rs, 0:64], in0=f32(S0p[rs, 0:64]), in1=pS[rs, 0:64], op=ALU.add),
                        r=[S0p, pS], w=[rw_tmpS])
                    k.V(lambda: nc.vector.tensor_scalar(out=rw_S[p][rs, :], in0=rw_tmpS[rs, 0:64], scalar1=st[rs, 3:4],
                                                        scalar2=None, op0=ALU.mult), r=[rw_tmpS, st], w=[rw_S[p]])
        if before_epilogue is not None:
            before_epilogue()
        for j in range(NJ):
            st = stt.next()
            y3 = ytok[:, j, :].rearrange("p (h d) -> p h d", h=8)
            k.V(lambda: nc.vector.tensor_reduce(out=st[:, 0:8], in_=y3, axis=AX.X, op=ALU.add), r=[ytok], w=[st])
            k.A(lambda: nc.scalar.activation(out=junk[:, 0:512], in_=ytok[:, j, :], func=AF.Square), r=[ytok], w=[junk])
            k.V(lambda: nc.vector.tensor_reduce(out=st[:, 8:16], in_=junk[:, 0:512].rearrange("p (h d) -> p h d", h=8),
                                                axis=AX.X, op=ALU.add), r=[junk], w=[st])
            k.V(lambda: nc.vector.tensor_scalar(out=st[:, 0:8], in0=st[:, 0:8], scalar1=1.0 / 64, scalar2=None, op0=ALU.mult),
                r=[st], w=[st])
            k.V(lambda: nc.vector.tensor_tensor(out=st[:, 16:24], in0=st[:, 0:8], in1=st[:, 0:8], op=ALU.mult), r=[st], w=[st])
            k.V(lambda: nc.vector.scalar_tensor_tensor(out=st[:, 8:16], in0=st[:, 8:16], scalar=1.0 / 64, in1=st[:, 16:24],
                                                       op0=ALU.mult, op1=ALU.subtract), r=[st], w=[st])
            k.V(lambda: nc.vector.tensor_scalar(out=st[:, 8:16], in0=st[:, 8:16], scalar1=64e-5, scalar2=None, op0=ALU.add),
                r=[st], w=[st])
            k.A(lambda: nc.scalar.sqrt(out=st[:, 8:16], in_=st[:, 8:16]), r=[st], w=[st])
            k.V(lambda: nc.vector.reciprocal(out=st[:, 24:32], in_=st[:, 8:16]), r=[st], w=[st])
            o = obr.next()
            o3 = o[:, :].rearrange("p (h d) -> p h d", h=8)
            k.V(lambda: nc.vector.tensor_tensor(out=o3, in0=y3, in1=st[:, 0:8].unsqueeze(2).to_broadcast([128, 8, 64]),
                                                op=ALU.subtract), r=[ytok, st], w=[o])
            k.V(lambda: nc.vector.tensor_tensor(out=o3, in0=o3, in1=st[:, 24:32].unsqueeze(2).to_broadcast([128, 8, 64]),
                                                op=ALU.mult), r=[o, st], w=[o])
            k.V(lambda: nc.vector.tensor_tensor(out=o[:, :], in0=o[:, :], in1=rw_lng[:, :], op=ALU.mult), r=[o, rw_lng], w=[o])
            k.V(lambda: nc.vector.tensor_tensor(out=o[:, :], in0=o[:, :], in1=rw_lnb[:, :], op=ALU.add), r=[o, rw_lnb], w=[o])
            k.V(lambda: nc.vector.tensor_tensor(out=junk[:, 0:512].rearrange("p (h d) -> p h d", h=8),
                                                in0=vtok[:, j, :].rearrange("p (h d) -> p h d", h=8),
                                                in1=rw_bon[:, j, :].unsqueeze(2).to_broadcast([128, 8, 64]), op=ALU.mult),
                r=[vtok, rw_bon], w=[junk])
            k.V(lambda: nc.vector.tensor_tensor(out=o[:, :], in0=o[:, :], in1=junk[:, 0:512], op=ALU.add), r=[o, junk], w=[o])
            k.V(lambda: nc.vector.tensor_tensor(out=o[:, :], in0=o[:, :], in1=gt[:, j, :], op=ALU.mult), r=[o, gt], w=[o])
            emit_out(3, l, blk, j, o, 512)

    gsb = obr
    fng = k.sb([128, D], name="fng")
    UP = ["w_up_ret", "w_up_gla", "w_up_ssd", "w_up_rwkv", "w_up_mem"]

    def merge_block(l, blk, last):
        t0 = blk * BLK
        mh = [TK[0], TK[1]]
        first = True
        for bi in range(5):
            if BR[bi] not in branches:
                continue
            nr = 2 if bi == 4 else 4
            for hf in range(2):
                wg = load_w(dr["w_in"][l], C_GATES + bi * 1024 + hf * 512, 512)
                wu = load_w(dr[UP[bi]][l], hf * 512, 512, rows=nr)
                for j in range(NJ):
                    pg = psr.next()
                    for dc in range(8):
                        k.mm(pg[:, :], hT[:, dc, j * T:(j + 1) * T], wg[:, dc, :], start=(dc == 0), stop=(dc == 7),
                             r=[hT, wg], w=[pg], fast=True)
                    gs = gsb.next()
                    k.A(lambda: nc.scalar.activation(out=gs[:, :], in_=pg[:, :], func=AF.Sigmoid), r=[pg], w=[gs])
                    pu = psr.next()
                    for c in range(nr):
                        k.mm(pu[:, :], oT[bi][:, c, j * T:(j + 1) * T], wu[:, c, :], start=(c == 0), stop=(c == nr - 1),
                             r=[oT[bi], wu], w=[pu], fast=True)
                    if first:
                        k.V(lambda: nc.vector.tensor_tensor(out=mh[hf][:, j, :], in0=gs[:, :], in1=pu[:, :], op=ALU.mult),
                            r=[gs, pu], w=[mh[hf]])
                    else:
                        k.V(lambda: nc.vector.tensor_tensor(out=gs[:, :], in0=gs[:, :], in1=pu[:, :], op=ALU.mult),
                            r=[gs, pu], w=[gs])
                        k.V(lambda: nc.vector.tensor_tensor(out=mh[hf][:, j, :], in0=mh[hf][:, j, :], in1=gs[:, :], op=ALU.add),
                            r=[gs, mh[hf]], w=[mh[hf]])
            first = False
        if debug is not None and l == dbg_layer:
            for j in range(NJ):
                for hf in range(2):
                    k.dma("pool", dbg_d[t0 + j * T:t0 + (j + 1) * T, 2304 + hf * 512:2304 + (hf + 1) * 512], mh[hf][:, j, :],
                          r=[mh[hf]])
        for j in range(NJ):
            for hf in range(2):
                for c in range(4):
                    tr(hT[:, hf * 4 + c, j * T:(j + 1) * T], hT, mh[hf][:, j, c * 128:(c + 1) * 128], mh[hf], 128, 128,
                       eng=("act" if c % 2 == 0 else "dve"))
        for hf in range(2):
            wo = load_w(dr["w_out"][l], hf * 512, 512)
            for j in range(NJ):
                po = psr.next()
                for dc in range(8):
                    k.mm(po[:, :], hT[:, dc, j * T:(j + 1) * T], wo[:, dc, :], start=(dc == 0), stop=(dc == 7),
                         r=[hT, wo], w=[po], fast=True)
                k.V(lambda: nc.vector.tensor_tensor(out=xb[:, j, hf * 512:(hf + 1) * 512], in0=xb[:, j, hf * 512:(hf + 1) * 512],
                                                    in1=po[:, :], op=ALU.add), r=[xb, po], w=[xb])
        if debug is not None and l == dbg_layer:
            for j in range(NJ):
                k.dma("pool", dbg_d[t0 + j * T:t0 + (j + 1) * T, 3328:4352], xb[:, j, :], r=[xb])
        if pipe:
            return
        if not last:
            k.dma("pool", x1_d[t0:t0 + BLK, :].rearrange("(j p) d -> p j d", p=128), xb[:, :, :], r=[xb], w=[x1_b[blk]])
        else:
            for j in range(NJ):
                st = stt.next()
                k.A(lambda: nc.scalar.activation(out=junk[:, :], in_=xb[:, j, :], func=AF.Square, accum_out=st[:, 0:1]),
                    r=[xb], w=[junk, st])
                k.V(lambda: nc.vector.tensor_scalar(out=st[:, 1:2], in0=st[:, 0:1], scalar1=1.0 / D, scalar2=1e-6,
                                                    op0=ALU.mult, op1=ALU.add), r=[st], w=[st])
                k.A(lambda: nc.scalar.sqrt(out=st[:, 1:2], in_=st[:, 1:2]), r=[st], w=[st])
                k.V(lambda: nc.vector.reciprocal(out=st[:, 2:3], in_=st[:, 1:2]), r=[st], w=[st])
                k.V(lambda: nc.vector.scalar_tensor_tensor(out=xb[:, j, :], in0=xb[:, j, :], scalar=st[:, 2:3], in1=fng[:, :],
                                                           op0=ALU.mult, op1=ALU.mult), r=[xb, st, fng], w=[xb])
            k.dma("pool", out_d[t0:t0 + BLK, :].rearrange("(j p) d -> p j d", p=128), xb[:, :, :], r=[xb])

    BR = ["ret", "gla", "ssd", "rwkv", "mem"]
    k.dma("sp", fng[:, :], bc(dr["final_norm_g"][0:1, :], D), w=[fng])

    def front(l, pos0):
        st = ss
        for j in range(NJ):
            k.A(lambda: nc.scalar.activation(out=junk[:, :], in_=xb[:, j, :], func=AF.Square,
                                             accum_out=st[:, j:j + 1]), r=[xb], w=[junk, st])
        k.V(lambda: nc.vector.tensor_scalar(out=st[:, 4:4 + NJ], in0=st[:, 0:NJ], scalar1=1.0 / D, scalar2=1e-6,
                                            op0=ALU.mult, op1=ALU.add), r=[st], w=[st])
        k.A(lambda: nc.scalar.sqrt(out=st[:, 4:4 + NJ], in_=st[:, 4:4 + NJ]), r=[st], w=[st])
        k.V(lambda: nc.vector.reciprocal(out=st[:, 8:8 + NJ], in_=st[:, 4:4 + NJ]), r=[st], w=[st])
        for j in range(NJ):
            k.V(lambda: nc.vector.tensor_scalar(out=xn[:, :], in0=xb[:, j, :], scalar1=st[:, 8 + j:9 + j],
                                                scalar2=None, op0=ALU.mult), r=[xb, st], w=[xn])
            for half in range(2):
                pa = psr.next()
                for q in range(4):
                    dc = half * 4 + q
                    k.op("pe", lambda: nc.tensor.transpose(pa[:, q * T:(q + 1) * T], xn[:, dc * T:(dc + 1) * T], ident),
                         r=[xn, cst], w=[pa])
                for q in range(4):
                    dc = half * 4 + q
                    k.A(lambda: nc.scalar.activation(out=hT[:, dc, j * T:(j + 1) * T], in_=pa[:, q * T:(q + 1) * T],
                                                     func=AF.Identity, scale=gcol[:, dc:dc + 1]), r=[pa, gcol], w=[hT])
        k.dma("sp", posi[:, :], dr["positions"][:, pos0:pos0 + BLK], w=[posi])
        k.V(lambda: nc.vector.tensor_copy(out=posf[:, :], in_=posi[:, :]), r=[posi], w=[posf])
        pa = psr.next()
        k.mm(pa[0:64, 0:BLK], C("invrow", 0, 1), posf[0:1, :], r=[cst, posf], w=[pa])
        for tab, shift in ((sinT, math.pi), (cosT, 1.5 * math.pi)):
            k.V(lambda: nc.vector.tensor_scalar(out=tab[:, :], in0=pa[0:64, 0:BLK], scalar1=shift, scalar2=None,
                                                op0=ALU.add), r=[pa], w=[tab])
            k.V(lambda: nc.vector.tensor_scalar(out=rope_qi[:, :], in0=tab[:, :], scalar1=1.0 / TWO_PI, scalar2=None,
                                                op0=ALU.mult), r=[tab], w=[rope_qi])
            k.V(lambda: nc.vector.tensor_copy(out=rope_qf[:, :], in_=rope_qi[:, :]), r=[rope_qi], w=[rope_qf])
            k.V(lambda: nc.vector.scalar_tensor_tensor(out=tab[:, :], in0=rope_qf[:, :], scalar=-TWO_PI, in1=tab[:, :],
                                                       op0=ALU.mult, op1=ALU.add), r=[rope_qf, tab], w=[tab])
            k.V(lambda: nc.vector.tensor_scalar(out=rope_qf[:, :], in0=tab[:, :], scalar1=0.0, scalar2=TWO_PI,
                                                op0=ALU.is_lt, op1=ALU.mult), r=[tab], w=[rope_qf])
            k.V(lambda: nc.vector.tensor_tensor(out=tab[:, :], in0=tab[:, :], in1=rope_qf[:, :], op=ALU.add),
                r=[tab, rope_qf], w=[tab])
            k.V(lambda: nc.vector.tensor_scalar(out=tab[:, :], in0=tab[:, :], scalar1=0.0, scalar2=TWO_PI,
                                                op0=ALU.max, op1=ALU.min), r=[tab], w=[tab])
            k.A(lambda: nc.scalar.activation(out=tab[:, :], in_=tab[:, :], func=AF.Sin, bias=pi_c[0:64, :], scale=1.0),
                r=[tab, pi_c], w=[tab])

    def branches_and_merge(l, blk, last):
        if "ret" in branches:
            ret_block(l, blk)
        if "gla" in branches:
            gla_block(l, blk)
        if "ssd" in branches:
            ssd_block(l, blk)
        hoist = ("rwkv" in branches) and ("mem" in branches)
        if "rwkv" in branches:
            rwkv_block(l, blk, before_epilogue=(lambda: mem_proj(l, blk)) if hoist else None)
        if "mem" in branches:
            mem_block(l, blk, do_proj=not hoist)
        if do_merge:
            merge_block(l, blk, last=last)

    if not pipe:
        for l in range(nlayers):
            load_params(l)
            for blk in range(nblk):
                t0 = blk * BLK
                src = dr["x"] if l == 0 else x1_d
                rb = [] if l == 0 else [x1_b[blk]]
                k.dma("sp", xb[:, :, :], src[t0:t0 + BLK, :].rearrange("(j p) d -> p j d", p=128), r=rb, w=[xb])
                front(l, t0)
                branches_and_merge(l, blk, last=(l == nlayers - 1))
    else:
        role = k.sb([128, 16], name="role")
        k.dma("sp", role[:, :], role_d[:, :], w=[role])
        load_params(0)
        k.V(lambda: nc.vector.memset(xb[:, :, :], 0.0), w=[xb])
        tmpX = [(TK[0], TK[1]), (TK[2], TK[3])]
        for it in range(nblk + 1):
            for s_ in range(4):
                ta = tmpX[s_ % 2]
                for hf in range(2):
                    eng = k.V if hf == 0 else k.G
                    e_ = nc.vector if hf == 0 else nc.gpsimd
                    eng(lambda: e_.tensor_scalar(out=ta[hf][:, :, :], in0=xb[:, :, hf * 512:(hf + 1) * 512],
                                                 scalar1=role[:, 1 + s_:2 + s_], scalar2=None, op0=ALU.mult),
                        r=[xb, role], w=[ta[hf]])
                    k.dma("pool", cin_d[s_ * BLK:(s_ + 1) * BLK, hf * 512:(hf + 1) * 512].rearrange("(j p) d -> p j d", p=128),
                          ta[hf][:, :, :], r=[ta[hf]], w=[cin_b])
            k.collective(cin_d.opt(), cout_d.opt(), r=[cin_b], w=[cout_b])
            ba = min(it, nblk - 1)
            k.dma("sp", xb[:, :, :], dr["x"][ba * BLK:(ba + 1) * BLK, :].rearrange("(j p) d -> p j d", p=128), w=[xb])
            k.V(lambda: nc.vector.tensor_scalar(out=xb[:, :, :], in0=xb[:, :, :], scalar1=role[:, 0:1], scalar2=None,
                                                op0=ALU.mult), r=[xb, role], w=[xb])
            for s_ in range(4):
                ta = tmpX[s_ % 2]
                for hf in range(2):
                    k.dma("sp", ta[hf][:, :, :],
                          cout_d[s_ * BLK:(s_ + 1) * BLK, hf * 512:(hf + 1) * 512].rearrange("(j p) d -> p j d", p=128),
                          r=[cout_b], w=[ta[hf]])
                    k.V(lambda: nc.vector.scalar_tensor_tensor(out=xb[:, :, hf * 512:(hf + 1) * 512], in0=ta[hf][:, :, :],
                                                               scalar=role[:, 5 + s_:6 + s_],
                                                               in1=xb[:, :, hf * 512:(hf + 1) * 512],
                                                               op0=ALU.mult, op1=ALU.add), r=[ta[hf], role, xb], w=[xb])
            front(0, it * BLK)
            branches_and_merge(0, it, last=False)
            if it == 0:
                for s_ in ret_S + gla_S + rw_S + [ssd_S]:
                    np_ = s_.t.shape[0]
                    k.V(lambda: nc.vector.tensor_scalar(out=s_[:, :], in0=s_[:, :], scalar1=role[0:np_, 9:10], scalar2=None,
                                                        op0=ALU.mult), r=[s_, role], w=[s_])
            ob = max(it - 1, 0)
            on = tmpX[1]
            for j in range(NJ):
                st = stt.next()
                k.A(lambda: nc.scalar.activation(out=junk[:, :], in_=xb[:, j, :], func=AF.Square, accum_out=st[:, 0:1]),
                    r=[xb], w=[junk, st])
                k.V(lambda: nc.vector.tensor_scalar(out=st[:, 1:2], in0=st[:, 0:1], scalar1=1.0 / D, scalar2=1e-6,
                                                    op0=ALU.mult, op1=ALU.add), r=[st], w=[st])
                k.A(lambda: nc.scalar.sqrt(out=st[:, 1:2], in_=st[:, 1:2]), r=[st], w=[st])
                k.V(lambda: nc.vector.reciprocal(out=st[:, 2:3], in_=st[:, 1:2]), r=[st], w=[st])
                for hf in range(2):
                    k.V(lambda: nc.vector.scalar_tensor_tensor(out=on[hf][:, j, :], in0=xb[:, j, hf * 512:(hf + 1) * 512],
                                                               scalar=st[:, 2:3], in1=fng[:, hf * 512:(hf + 1) * 512],
                                                               op0=ALU.mult, op1=ALU.mult), r=[xb, st, fng], w=[on[hf]])
            for hf in range(2):
                k.dma("pool", out_d[ob * BLK:(ob + 1) * BLK, hf * 512:(hf + 1) * 512].rearrange("(j p) d -> p j d", p=128),
                      on[hf][:, :, :], r=[on[hf]], w=[out_b[ob]])
    k.finish()
    return nc, k


NCORES = 4
PIPE = False


def make_in_maps(inputs, nblk=SEQ // BLK):
    ntok = nblk * BLK
    maps = []
    if not PIPE:
        params = {n: np.ascontiguousarray(np.asarray(inputs[n], np.float32).reshape(SHAPES[n])) for n in PARAM_NAMES}
        for c in range(4):
            m = {"x": np.ascontiguousarray(inputs["x"][c]), "mem": np.ascontiguousarray(inputs["mem"][c]),
                 "positions": np.ascontiguousarray(inputs["positions"][c:c + 1]).astype(np.int32), "cst": CST}
            m.update(params)
            maps.append(m)
        return maps
    per_stage = []
    for stg in range(2):
        p = {}
        for n in PARAM_NAMES:
            a = np.asarray(inputs[n], np.float32).reshape(SHAPES[n])
            if n != "final_norm_g":
                a = a[stg:stg + 1]
            p[n] = np.ascontiguousarray(a)
        per_stage.append(p)
    for c in range(8):
        b, stg = c % 4, c // 4
        pos = np.asarray(inputs["positions"][b], np.int32)[:ntok]
        if stg == 0:
            pos2 = np.concatenate([pos, pos[ntok - BLK:ntok]])
        else:
            pos2 = np.concatenate([np.zeros(BLK, np.int32), pos])
        role = np.zeros((128, 16), np.float32)
        if stg == 0:
            role[:, 0] = 1.0
            role[:, 1 + b] = 1.0
            role[:, 9] = 1.0
        else:
            role[:, 5 + b] = 1.0
        m = {"x": np.ascontiguousarray(inputs["x"][b][:ntok]), "mem": np.ascontiguousarray(inputs["mem"][b]),
             "positions": np.ascontiguousarray(pos2[None, :]), "cst": CST, "role": role}
        m.update(per_stage[stg])
        maps.append(m)
    return maps


def kernel(**inputs):
    if PIPE:
        nc, k = build(nlayers=1, pipe=True)
        maps = make_in_maps(inputs)
        res = run_bass_kernel_spmd(nc, maps, core_ids=list(range(8)))
        out = np.stack([np.asarray(res.results[4 + b]["out"]) for b in range(4)], axis=0)
    else:
        nc, k = build()
        maps = make_in_maps(inputs)
        res = run_bass_kernel_spmd(nc, maps, core_ids=list(range(4)))
        out = np.stack([np.asarray(res.results[b]["out"]) for b in range(4)], axis=0)
    return out.astype(np.float32)
```
